# Optimizing a Trainium2 kernel written in Bass

```python
import math
import jax
import jax.numpy as jnp
from jax import lax
import numpy as np

D_MODEL = 1024
BATCH = 16
SEQ = 2048
DEPTH = 2

GRID_W = 64
CTX_LEN = 256
HEAD_DIM = 64
D_MIX = D_MODEL
RWKV_WIDTH = D_MIX // 4
RWKV_HEADS = RWKV_WIDTH // HEAD_DIM
RWKV_LORA_W = 64
RWKV_LORA_A = 64
RWKV_LORA_G = 128
RWKV_IN = 3 * RWKV_WIDTH + RWKV_LORA_W + RWKV_LORA_A + RWKV_LORA_G
RWKV_SPLITS = (RWKV_WIDTH, 2 * RWKV_WIDTH, 3 * RWKV_WIDTH, 3 * RWKV_WIDTH + RWKV_LORA_W, 3 * RWKV_WIDTH + RWKV_LORA_W + RWKV_LORA_A)
CONV_W = 3
GN_EPS = 64e-5
S5_WIDTH = D_MIX // 4
S5_CH = 16
S5_GROUPS = S5_WIDTH // S5_CH
S5_STATE = 64
ATT_WIDTH = D_MIX - RWKV_WIDTH - S5_WIDTH
ATT_HEADS = ATT_WIDTH // HEAD_DIM
ATT_KV_HEADS = 2
ATT_GQ = ATT_HEADS // ATT_KV_HEADS
WINDOW = 128
ATT_BLOCK = 128
ROPE_BASE = 10000.0
IN_SPLITS = (RWKV_IN, RWKV_IN + S5_WIDTH, RWKV_IN + S5_WIDTH + ATT_WIDTH, RWKV_IN + S5_WIDTH + ATT_WIDTH + ATT_KV_HEADS * HEAD_DIM)
D_IN = IN_SPLITS[-1] + ATT_KV_HEADS * HEAD_DIM
N_GROUPS = 4
EXPERTS_PER_GROUP = 8
N_EXPERTS = N_GROUPS * EXPERTS_PER_GROUP
TOP_K_EXPERT = 2
D_EXPERT = 512
MOE_BLOCK = 128
N_MOD = 6
DEEPNORM_ALPHA = (2.0 * DEPTH) ** 0.25
DEEPNORM_BETA = (8.0 * DEPTH) ** -0.25
LN_EPS = 1e-5
F32 = jnp.float32

kernel_name = 'hybrid_rwkv7_s5_swa_hmoe_dit_block'


def layer_norm(x, g, b):
    xf = x.astype(F32)
    mu = jnp.mean(xf, axis=-1, keepdims=True)
    var = jnp.mean(jnp.square(xf - mu), axis=-1, keepdims=True)
    return ((xf - mu) * lax.rsqrt(var + LN_EPS) * g + b).astype(x.dtype)


def centred_conv(u, w):
    half = CONV_W // 2
    L = u.shape[1]
    up = jnp.pad(u, ((0, 0), (half, half), (0, 0)))
    return sum(up[:, j:j + L] * w[j] for j in range(CONV_W))


def rwkv_features(p, conv_w, w0, w2, a0, a2, k_k, k_a):
    p = centred_conv(p, conv_w)
    r, k, v, w_lo, a_lo, g_lo = jnp.split(p, RWKV_SPLITS, axis=-1)
    B, L = r.shape[:2]
    heads = lambda t: t.reshape(B, L, RWKV_HEADS, HEAD_DIM)
    kk = heads(k * k_k).astype(F32)
    kk = kk * lax.rsqrt(jnp.sum(kk * kk, axis=-1, keepdims=True) + 1e-12)
    dirs = []
    for d in range(2):
        w_log = -jax.nn.softplus(-(w0[d] + jnp.tanh(w_lo) @ w2[d])) - 0.5
        decay = jnp.exp(-jnp.exp(w_log))
        a = jax.nn.sigmoid(a0[d] + a_lo @ a2[d])
        k_d = k * (1.0 + (a - 1.0) * k_a)
        dirs.append((heads(decay), heads(k_d), heads(a)))
    return heads(r), heads(v), kk, g_lo, dirs


def rwkv_scan(r, decay, k, v, kk, kk_a, s0, reverse, emit):
    xs = tuple(jnp.moveaxis(t.astype(F32), 1, 0) for t in (r, decay, k, v, kk, kk_a))

    def step(s, inp):
        r_t, w_t, k_t, v_t, kk_t, b_t = inp
        s_kk = jnp.einsum('bhvk,bhk->bhv', s, kk_t)
        s = s * w_t[:, :, None, :] - s_kk[..., None] * b_t[:, :, None, :] + v_t[..., None] * k_t[:, :, None, :]
        return s, (jnp.einsum('bhvk,bhk->bhv', s, r_t) if emit else None)

    s_final, ys = lax.scan(step, s0, xs, reverse=reverse)
    return s_final, (jnp.moveaxis(ys, 0, 1) if emit else None)


def rwkv_output(y, feats, g2, r_k, gn_w, gn_b):
    r, v, kk, g_lo, dirs = feats
    B, L = y.shape[:2]
    mu = jnp.mean(y, axis=-1, keepdims=True)
    var = jnp.mean(jnp.square(y - mu), axis=-1, keepdims=True)
    yn = ((y - mu) * lax.rsqrt(var + GN_EPS)).reshape(B, L, RWKV_WIDTH) * gn_w + gn_b
    k_sum = dirs[0][1] + dirs[1][1]
    bonus = jnp.sum(r * k_sum * r_k, axis=-1, keepdims=True) * v
    gate = jax.nn.sigmoid(g_lo) @ g2
    return (yn.astype(v.dtype) + bonus.reshape(B, L, RWKV_WIDTH)) * gate


def rwkv_mixer(p_ctx, p_lat, emit_ctx, conv_w, w0, w2, a0, a2, g2, k_k, k_a, r_k, gn_w, gn_b):
    fc = rwkv_features(p_ctx, conv_w, w0, w2, a0, a2, k_k, k_a)
    fl = rwkv_features(p_lat, conv_w, w0, w2, a0, a2, k_k, k_a)
    B = p_lat.shape[0]
    s0 = jnp.zeros((B, RWKV_HEADS, HEAD_DIM, HEAD_DIM), F32)
    y_lat, y_ctx = [], []
    for d, rev in enumerate((False, True)):
        rc, vc, kkc, _, dc = fc
        rl, vl, kkl, _, dl = fl
        s_ctx, yc = rwkv_scan(rc, dc[d][0], dc[d][1], vc, kkc, kkc * dc[d][2], s0, rev, emit_ctx)
        _, yl = rwkv_scan(rl, dl[d][0], dl[d][1], vl, kkl, kkl * dl[d][2], s_ctx, rev, True)
        y_lat.append(yl)
        y_ctx.append(yc)
    out_lat = rwkv_output(y_lat[0] + y_lat[1], fl, g2, r_k, gn_w, gn_b)
    out_ctx = rwkv_output(y_ctx[0] + y_ctx[1], fc, g2, r_k, gn_w, gn_b) if emit_ctx else None
    return out_lat, out_ctx


def s5_discretise(lam_re, lam_im, log_dt):
    dt = jnp.exp(log_dt)[:, None]
    mag = jnp.exp(lam_re * dt)
    ab_re = mag * jnp.cos(lam_im * dt)
    ab_im = mag * jnp.sin(lam_im * dt)
    nr, ni = ab_re - 1.0, ab_im
    den = lam_re * lam_re + lam_im * lam_im
    coef_re = (nr * lam_re + ni * lam_im) / den
    coef_im = (ni * lam_re - nr * lam_im) / den
    return ab_re, ab_im, coef_re, coef_im


def s5_scan(u, ab_re, ab_im, coef_re, coef_im, b_re, b_im, h0_re, h0_im):
    b_re, b_im = b_re.astype(F32), b_im.astype(F32)
    bb_re = coef_re[..., None] * b_re - coef_im[..., None] * b_im
    bb_im = coef_re[..., None] * b_im + coef_im[..., None] * b_re
    bu_re = jnp.einsum('gpc,blgc->blgp', bb_re, u)
    bu_im = jnp.einsum('gpc,blgc->blgp', bb_im, u)
    bu_re = bu_re.at[:, 0].add(ab_re * h0_re - ab_im * h0_im)
    bu_im = bu_im.at[:, 0].add(ab_re * h0_im + ab_im * h0_re)
    L = u.shape[1]
    a_re = jnp.broadcast_to(ab_re, (1, L) + ab_re.shape)
    a_im = jnp.broadcast_to(ab_im, (1, L) + ab_im.shape)

    def combine(e1, e2):
        a1r, a1i, b1r, b1i = e1
        a2r, a2i, b2r, b2i = e2
        return (a2r * a1r - a2i * a1i, a2r * a1i + a2i * a1r,
                a2r * b1r - a2i * b1i + b2r, a2r * b1i + a2i * b1r + b2i)

    _, _, h_re, h_im = lax.associative_scan(combine, (a_re, a_im, bu_re, bu_im), axis=1)
    return h_re, h_im


def s5_readout(h_re, h_im, c_re, c_im):
    y = jnp.einsum('gcp,blgp->blgc', c_re.astype(F32), h_re) - jnp.einsum('gcp,blgp->blgc', c_im.astype(F32), h_im)
    return y.reshape(y.shape[0], y.shape[1], S5_WIDTH)


def s5_glu(y, glu_w, glu_b):
    z = jax.nn.gelu(y)
    return z * jax.nn.sigmoid(z @ glu_w + glu_b)


def s5_mixer(u_ctx, u_lat, emit_ctx, lam_re, lam_im, log_dt, b_re, b_im, c_re, c_im, d_skip, glu_w, glu_b):
    B, C = u_ctx.shape[:2]
    L = u_lat.shape[1]
    uc = u_ctx.astype(F32).reshape(B, C, S5_GROUPS, S5_CH)
    ul = u_lat.astype(F32).reshape(B, L, S5_GROUPS, S5_CH)
    h0 = jnp.zeros((B, S5_GROUPS, S5_STATE), F32)
    y_lat = d_skip * u_lat.astype(F32)
    y_ctx = d_skip * u_ctx.astype(F32) if emit_ctx else None
    for d in range(2):
        order = (lambda t: jnp.flip(t, axis=1)) if d == 1 else (lambda t: t)
        disc = s5_discretise(lam_re[d].astype(F32), lam_im[d].astype(F32), log_dt[d].astype(F32))
        hc_re, hc_im = s5_scan(order(uc), *disc, b_re, b_im, h0, h0)
        hl_re, hl_im = s5_scan(order(ul), *disc, b_re, b_im, hc_re[:, -1], hc_im[:, -1])
        y_lat = y_lat + order(s5_readout(hl_re, hl_im, c_re, c_im))
        if emit_ctx:
            y_ctx = y_ctx + order(s5_readout(hc_re, hc_im, c_re, c_im))
    out_lat = s5_glu(y_lat, glu_w, glu_b).astype(u_lat.dtype)
    out_ctx = s5_glu(y_ctx, glu_w, glu_b).astype(u_ctx.dtype) if emit_ctx else None
    return out_lat, out_ctx


def axial_rope_tables(L):
    rows = L // GRID_W
    row_id, col_id = jnp.meshgrid(jnp.arange(rows, dtype=F32), jnp.arange(GRID_W, dtype=F32), indexing='ij')
    n_freq = HEAD_DIM // 4
    inv_freq = ROPE_BASE ** (-jnp.arange(n_freq, dtype=F32) / n_freq)
    ang = jnp.concatenate([row_id.reshape(-1, 1) * inv_freq, col_id.reshape(-1, 1) * inv_freq], axis=-1)
    return jnp.cos(ang), jnp.sin(ang)


def apply_rope(t, cos, sin):
    extra = t.ndim - 3
    cos = cos.reshape(cos.shape[:1] + (1,) * extra + cos.shape[1:])
    sin = sin.reshape(sin.shape[:1] + (1,) * extra + sin.shape[1:])
    t1, t2 = jnp.split(t, 2, axis=-1)
    return jnp.concatenate([t1 * cos - t2 * sin, t2 * cos + t1 * sin], axis=-1).astype(t.dtype)


def softmax_with_sink(scores, sink):
    s_sink = jnp.broadcast_to(sink[None, :, :, None, None], scores.shape[:-1] + (1,))
    p = jax.nn.softmax(jnp.concatenate([scores, s_sink], axis=-1), axis=-1)
    return p[..., :-1]


def attention_mixer(q_ctx, k_ctx, v_ctx, q_lat, k_lat, v_lat, sink, emit_ctx):
    B, L, _ = q_lat.shape
    C = k_ctx.shape[1]
    nb = L // ATT_BLOCK
    scale = HEAD_DIM ** -0.5
    cos, sin = axial_rope_tables(L)
    q = apply_rope(q_lat.reshape(B, L, ATT_KV_HEADS, ATT_GQ, HEAD_DIM), cos, sin)
    k = apply_rope(k_lat.reshape(B, L, ATT_KV_HEADS, HEAD_DIM), cos, sin)
    v = v_lat.reshape(B, L, ATT_KV_HEADS, HEAD_DIM)
    kc = k_ctx.reshape(B, C, ATT_KV_HEADS, HEAD_DIM)
    vc = v_ctx.reshape(B, C, ATT_KV_HEADS, HEAD_DIM)
    sink = sink.reshape(ATT_KV_HEADS, ATT_GQ).astype(F32)

    def neighbours(t):
        tp = jnp.pad(t, ((0, 0), (ATT_BLOCK, ATT_BLOCK), (0, 0), (0, 0)))
        tp = tp.reshape(B, nb + 2, ATT_BLOCK, ATT_KV_HEADS, HEAD_DIM)
        return jnp.moveaxis(jnp.concatenate([tp[:, :-2], tp[:, 1:-1], tp[:, 2:]], axis=2), 1, 0)

    qb = jnp.moveaxis(q.reshape(B, nb, ATT_BLOCK, ATT_KV_HEADS, ATT_GQ, HEAD_DIM), 1, 0)
    kb, vb = neighbours(k), neighbours(v)
    blk = jnp.arange(nb)[:, None, None] * ATT_BLOCK
    q_pos = blk + jnp.arange(ATT_BLOCK)[None, :, None]
    k_pos = blk - ATT_BLOCK + jnp.arange(3 * ATT_BLOCK)[None, None, :]
    band_mask = (jnp.abs(q_pos - k_pos) <= WINDOW) & (k_pos >= 0) & (k_pos < L)
    n_band = 3 * ATT_BLOCK

    def attend_block(inp):
        q_blk, k_blk, v_blk, m = inp
        s_lat = jnp.einsum('bqhgd,bkhd->bhgqk', q_blk, k_blk, preferred_element_type=F32) * scale
        s_lat = jnp.where(m, s_lat, -jnp.inf)
        s_ctx = jnp.einsum('bqhgd,bchd->bhgqc', q_blk, kc, preferred_element_type=F32) * scale
        p = softmax_with_sink(jnp.concatenate([s_lat, s_ctx], axis=-1), sink)
        o = jnp.einsum('bhgqk,bkhd->bqhgd', p[..., :n_band].astype(v_blk.dtype), v_blk)
        return o + jnp.einsum('bhgqc,bchd->bqhgd', p[..., n_band:].astype(vc.dtype), vc)

    ob = lax.map(attend_block, (qb, kb, vb, band_mask))
    out_lat = jnp.moveaxis(ob, 0, 1).reshape(B, L, ATT_WIDTH).astype(q_lat.dtype)
    out_ctx = None
    if emit_ctx:
        qc = q_ctx.reshape(B, C, ATT_KV_HEADS, ATT_GQ, HEAD_DIM)
        s_cc = jnp.einsum('bqhgd,bchd->bhgqc', qc, kc, preferred_element_type=F32) * scale
        p_cc = softmax_with_sink(s_cc, sink)
        out_ctx = jnp.einsum('bhgqc,bchd->bqhgd', p_cc.astype(vc.dtype), vc).reshape(B, C, ATT_WIDTH).astype(q_ctx.dtype)
    return out_lat, out_ctx


def routed_experts(h, experts, weights, w_gate, w_up, w_down):
    T, D = h.shape
    A = T * TOP_K_EXPERT
    P = -(-A // MOE_BLOCK) * MOE_BLOCK + N_EXPERTS * MOE_BLOCK
    n_blk = P // MOE_BLOCK
    flat_e = experts.reshape(-1)
    flat_tok = jnp.repeat(jnp.arange(T, dtype=jnp.int32), TOP_K_EXPERT)
    flat_w = weights.reshape(-1)
    order = jnp.argsort(flat_e)
    se = flat_e[order]
    counts = jnp.zeros((N_EXPERTS,), jnp.int32).at[flat_e].add(1)
    start = jnp.cumsum(counts) - counts
    padded = (counts + MOE_BLOCK - 1) // MOE_BLOCK * MOE_BLOCK
    pend = jnp.cumsum(padded)
    pstart = pend - padded
    dest = pstart[se] + (jnp.arange(A, dtype=jnp.int32) - start[se])
    tok_buf = jnp.zeros((P,), jnp.int32).at[dest].set(flat_tok[order])
    w_buf = jnp.zeros((P,), F32).at[dest].set(flat_w[order])
    blk_exp = jnp.minimum(jnp.searchsorted(pend, jnp.arange(n_blk) * MOE_BLOCK, side='right'), N_EXPERTS - 1)
    xb = h[tok_buf].reshape(n_blk, MOE_BLOCK, D)

    def run_block(inp):
        x_blk, e = inp
        return (jax.nn.silu(x_blk @ w_gate[e]) * (x_blk @ w_up[e])) @ w_down[e]

    yb = lax.map(run_block, (xb, blk_exp)).reshape(P, D)
    return jnp.zeros_like(h).at[tok_buf].add(yb * w_buf[:, None].astype(h.dtype))


def hier_moe(h, rg_w, rg_b, re_w, re_b, w_gate, w_up, w_down):
    T = h.shape[0]
    g_logits = (h @ rg_w + rg_b).astype(F32)
    g_idx = jnp.argmax(g_logits, axis=-1)
    g_prob = jnp.take_along_axis(jax.nn.softmax(g_logits, axis=-1), g_idx[:, None], axis=-1)
    e_logits = (h @ re_w + re_b).astype(F32).reshape(T, N_GROUPS, EXPERTS_PER_GROUP)
    e_logits = jnp.take_along_axis(e_logits, g_idx[:, None, None], axis=1)[:, 0]
    top_logits, top_idx = lax.top_k(e_logits, TOP_K_EXPERT)
    weights = g_prob * jax.nn.softmax(top_logits, axis=-1)
    experts = g_idx[:, None] * EXPERTS_PER_GROUP + top_idx
    return routed_experts(h, experts, weights, w_gate, w_up, w_down)


def trunk_layer(x, xc, c, c_ctx, last, w_mod, b_mod, w_in, rwkv_conv, rwkv_w0, rwkv_w2, rwkv_a0, rwkv_a2, rwkv_g2,
                rwkv_k_k, rwkv_k_a, rwkv_r_k, rwkv_gn_w, rwkv_gn_b, s5_lam_re, s5_lam_im, s5_log_dt, s5_b_re, s5_b_im,
                s5_c_re, s5_c_im, s5_d, s5_glu_w, s5_glu_b, attn_sink, w_out, ln1_g, ln1_b, ln2_g, ln2_b,
                router_group_w, router_group_b, router_expert_w, router_expert_b, expert_w_gate, expert_w_up, expert_w_down):
    emit_ctx = not last
    B, L, D = x.shape
    mod = jax.nn.silu(c) @ w_mod + b_mod
    mod_c = jax.nn.silu(c_ctx) @ w_mod + b_mod
    sh1, sc1, gt1, sh2, sc2, gt2 = jnp.split(mod[:, None, :], N_MOD, axis=-1)
    csh1, csc1, cgt1, csh2, csc2, cgt2 = jnp.split(mod_c, N_MOD, axis=-1)

    proj = (x * (1.0 + sc1) + sh1) @ w_in
    proj_c = (xc * (1.0 + csc1) + csh1) @ w_in
    pr, ps, pq, pk, pv = jnp.split(proj, IN_SPLITS, axis=-1)
    cr, cs, cq, ck, cv = jnp.split(proj_c, IN_SPLITS, axis=-1)
    yr, yr_c = rwkv_mixer(cr, pr, emit_ctx, rwkv_conv, rwkv_w0, rwkv_w2, rwkv_a0, rwkv_a2, rwkv_g2,
                          rwkv_k_k, rwkv_k_a, rwkv_r_k, rwkv_gn_w, rwkv_gn_b)
    ys, ys_c = s5_mixer(cs, ps, emit_ctx, s5_lam_re, s5_lam_im, s5_log_dt, s5_b_re, s5_b_im, s5_c_re, s5_c_im,
                        s5_d, s5_glu_w, s5_glu_b)
    ya, ya_c = attention_mixer(cq, ck, cv, pq, pk, pv, attn_sink, emit_ctx)
    mix = jnp.concatenate([yr, ys, ya], axis=-1) @ w_out
    x = layer_norm(DEEPNORM_ALPHA * x + gt1 * mix, ln1_g, ln1_b)
    if emit_ctx:
        mix_c = jnp.concatenate([yr_c, ys_c, ya_c], axis=-1) @ w_out
        xc = layer_norm(DEEPNORM_ALPHA * xc + cgt1 * mix_c, ln1_g, ln1_b)

    moe_params = (router_group_w, router_group_b, router_expert_w, router_expert_b, expert_w_gate, expert_w_up, expert_w_down)
    h = x * (1.0 + sc2) + sh2
    if emit_ctx:
        hc = xc * (1.0 + csc2) + csh2
        y = hier_moe(jnp.concatenate([h.reshape(-1, D), hc.reshape(-1, D)], axis=0), *moe_params)
        y_lat = y[:B * L].reshape(B, L, D)
        y_ctx = y[B * L:].reshape(xc.shape)
        xc = layer_norm(DEEPNORM_ALPHA * xc + cgt2 * y_ctx, ln2_g, ln2_b)
    else:
        y_lat = hier_moe(h.reshape(-1, D), *moe_params).reshape(B, L, D)
        xc = None
    x = layer_norm(DEEPNORM_ALPHA * x + gt2 * y_lat, ln2_g, ln2_b)
    return x, xc


def setup_inputs(seed: int = 0) -> dict:
    key = jax.random.key(seed)
    ks = iter(jax.random.split(key, 48))
    nrm = lambda shape, s: s * jax.random.normal(next(ks), shape, F32)
    Dp, D = DEPTH, D_MODEL
    ratio = jnp.arange(RWKV_WIDTH, dtype=F32) / (RWKV_WIDTH - 1)
    n_idx = jnp.arange(S5_STATE, dtype=F32)
    side = jnp.array([0.25, 0.5, 0.25], F32)[:, None]
    return {
        'x': nrm((BATCH, SEQ, D), 1.0),
        'c': nrm((BATCH, D), 1.0),
        'ctx': nrm((BATCH, CTX_LEN, D), 1.0),
        'c_ctx': nrm((D,), 1.0),
        'w_mod': nrm((Dp, D, N_MOD * D), 0.5 * D ** -0.5),
        'b_mod': nrm((Dp, N_MOD * D), 0.01),
        'w_in': nrm((Dp, D, D_IN), D ** -0.5),
        'rwkv_conv': side + nrm((Dp, CONV_W, RWKV_IN), 0.05),
        'rwkv_w0': -6.0 + 5.0 * ratio ** 0.85 + nrm((Dp, 2, RWKV_WIDTH), 0.1),
        'rwkv_w2': nrm((Dp, 2, RWKV_LORA_W, RWKV_WIDTH), 0.1),
        'rwkv_a0': nrm((Dp, 2, RWKV_WIDTH), 0.1),
        'rwkv_a2': nrm((Dp, 2, RWKV_LORA_A, RWKV_WIDTH), 0.1),
        'rwkv_g2': nrm((Dp, RWKV_LORA_G, RWKV_WIDTH), RWKV_LORA_G ** -0.5),
        'rwkv_k_k': 0.85 + nrm((Dp, RWKV_WIDTH), 0.02),
        'rwkv_k_a': 1.0 + nrm((Dp, RWKV_WIDTH), 0.02),
        'rwkv_r_k': nrm((Dp, RWKV_HEADS, HEAD_DIM), 0.1),
        'rwkv_gn_w': 1.0 + nrm((Dp, RWKV_WIDTH), 0.02),
        'rwkv_gn_b': nrm((Dp, RWKV_WIDTH), 0.01),
        's5_lam_re': -0.5 + nrm((Dp, 2, S5_GROUPS, S5_STATE), 0.01),
        's5_lam_im': math.pi * n_idx + nrm((Dp, 2, S5_GROUPS, S5_STATE), 0.01),
        's5_log_dt': jax.random.uniform(next(ks), (Dp, 2, S5_GROUPS), F32, math.log(1e-3), math.log(1e-1)),
        's5_b_re': nrm((Dp, S5_GROUPS, S5_STATE, S5_CH), (2 * S5_CH) ** -0.5),
        's5_b_im': nrm((Dp, S5_GROUPS, S5_STATE, S5_CH), (2 * S5_CH) ** -0.5),
        's5_c_re': nrm((Dp, S5_GROUPS, S5_CH, S5_STATE), 0.5),
        's5_c_im': nrm((Dp, S5_GROUPS, S5_CH, S5_STATE), 0.5),
        's5_d': nrm((Dp, S5_WIDTH), 1.0),
        's5_glu_w': nrm((Dp, S5_WIDTH, S5_WIDTH), S5_WIDTH ** -0.5),
        's5_glu_b': nrm((Dp, S5_WIDTH), 0.01),
        'attn_sink': nrm((Dp, ATT_HEADS), 0.5),
        'w_out': nrm((Dp, D_MIX, D), DEEPNORM_BETA * D_MIX ** -0.5),
        'ln1_g': 1.0 + nrm((Dp, D), 0.05),
        'ln1_b': nrm((Dp, D), 0.01),
        'ln2_g': 1.0 + nrm((Dp, D), 0.05),
        'ln2_b': nrm((Dp, D), 0.01),
        'router_group_w': nrm((Dp, D, N_GROUPS), D ** -0.5),
        'router_group_b': nrm((Dp, N_GROUPS), 0.01),
        'router_expert_w': nrm((Dp, D, N_EXPERTS), D ** -0.5),
        'router_expert_b': nrm((Dp, N_EXPERTS), 0.01),
        'expert_w_gate': nrm((Dp, N_EXPERTS, D, D_EXPERT), D ** -0.5),
        'expert_w_up': nrm((Dp, N_EXPERTS, D, D_EXPERT), D ** -0.5),
        'expert_w_down': nrm((Dp, N_EXPERTS, D_EXPERT, D), DEEPNORM_BETA * D_EXPERT ** -0.5),
    }


def reference(x, c, ctx, c_ctx, w_mod, b_mod, w_in, rwkv_conv, rwkv_w0, rwkv_w2, rwkv_a0, rwkv_a2, rwkv_g2,
              rwkv_k_k, rwkv_k_a, rwkv_r_k, rwkv_gn_w, rwkv_gn_b, s5_lam_re, s5_lam_im, s5_log_dt, s5_b_re, s5_b_im,
              s5_c_re, s5_c_im, s5_d, s5_glu_w, s5_glu_b, attn_sink, w_out, ln1_g, ln1_b, ln2_g, ln2_b,
              router_group_w, router_group_b, router_expert_w, router_expert_b, expert_w_gate, expert_w_up, expert_w_down):
    stacked = (w_mod, b_mod, w_in, rwkv_conv, rwkv_w0, rwkv_w2, rwkv_a0, rwkv_a2, rwkv_g2, rwkv_k_k, rwkv_k_a,
               rwkv_r_k, rwkv_gn_w, rwkv_gn_b, s5_lam_re, s5_lam_im, s5_log_dt, s5_b_re, s5_b_im, s5_c_re, s5_c_im,
               s5_d, s5_glu_w, s5_glu_b, attn_sink, w_out, ln1_g, ln1_b, ln2_g, ln2_b, router_group_w,
               router_group_b, router_expert_w, router_expert_b, expert_w_gate, expert_w_up, expert_w_down)
    xc = ctx
    for i in range(DEPTH):
        layer_params = [p[i] for p in stacked]
        x, xc = trunk_layer(x, xc, c, c_ctx, i == DEPTH - 1, *layer_params)
    return x
```

```python
import contextlib
import math
import numpy as np
import concourse.bass as bass
import concourse.mybir as mybir
from concourse.bass_utils import run_bass_kernel_spmd

F32 = mybir.dt.float32
BF16 = mybir.dt.bfloat16
AF = mybir.ActivationFunctionType
ALU = mybir.AluOpType
AX = mybir.AxisListType

D = 1024
DEPTH = 2
HD = 64
RW = 256
NH = 4
S5W = 256
S5G = 16
S5P = 64
S5C = 16
AW = 512
AH = 8
AKV = 2
AG = 4
DIN = 2048
NEXP = 32
DE = 512
ALPHA = (2.0 * DEPTH) ** 0.25
LN_EPS = 1e-5
GN_EPS = 64e-5
GRID_W = 64
SEM_LIMIT = 20000
RWKV_STAGGER = 0
I32 = mybir.dt.int32
EB = 512


class Tr:
    __slots__ = ("w", "r")

    def __init__(self):
        self.w = None
        self.r = {}


class KB:
    def __init__(self, nc):
        self.nc = nc
        self.es = contextlib.ExitStack()
        self.eng = {"pe": nc.tensor, "dve": nc.vector, "act": nc.scalar, "pool": nc.gpsimd, "sp": nc.sync}
        self.sems = {}
        self.cnt = {}
        self.phase = {k: 0 for k in self.eng}
        self.waited = {k: {} for k in self.eng}
        self.ndq = 8
        self.dq_next = {}
        self.n_inst = 0
        self._uid = 0
        self.psring = []
        self.nw = {}
        self.bregs = {}
        self.ps_i = 0

    def sbuf(self, st, name, shape, dt=F32):
        self._uid += 1
        return st.enter_context(self.nc.sbuf_tensor("%s_%d" % (name, self._uid), list(shape), dt))

    def dram(self, name, shape, dt=F32, kind="Internal"):
        return self.nc.dram_tensor(name, list(shape), dt, kind=kind).ap()

    def init_psum(self):
        for i in range(8):
            t = self.es.enter_context(self.nc.psum_tensor("psr%d" % i, [128, 512], F32))
            self.psring.append((t, Tr()))

    def ps(self):
        t = self.psring[self.ps_i]
        self.ps_i = (self.ps_i + 1) % 8
        return t

    def _sem(self, key):
        if key not in self.sems:
            self._uid += 1
            self.sems[key] = self.es.enter_context(self.nc.semaphore("s%d" % self._uid))
            self.cnt[key] = 0
        return self.sems[key]

    def _cur_key(self, e):
        key = (e, self.phase[e])
        self._sem(key)
        if self.cnt[key] >= SEM_LIMIT:
            self.phase[e] += 1
            key = (e, self.phase[e])
            self._sem(key)
        return key

    def _wait(self, e, deps):
        best = {}
        for d in deps:
            if d is None:
                continue
            key, n = d
            if best.get(key, 0) < n:
                best[key] = n
        for key, n in best.items():
            if key[0] == e and e == "pe":
                continue
            if self.waited[e].get(key, 0) >= n:
                continue
            self.eng[e].wait_ge(self._sem(key), n)
            self.waited[e][key] = n
            self.nw[e] = self.nw.get(e, 0) + 1
            if self.nw[e] >= 2:
                self.eng[e].nop()
                self.nw[e] = 0

    @staticmethod
    def _deps(reads, writes):
        deps = []
        for t in reads:
            deps.append(t.w)
        for t in writes:
            deps.append(t.w)
            for kk, n in t.r.items():
                deps.append((kk, n))
        return deps

    @staticmethod
    def _mark(reads, writes, me):
        key, n = me
        for t in reads:
            t.r[key] = n
        for t in writes:
            t.w = me
            t.r = {}

    def op(self, e, fn, reads=(), writes=()):
        self._wait(e, self._deps(reads, writes))
        key = self._cur_key(e)
        ins = fn(self.eng[e])
        self.nw[e] = 0
        ins.then_inc(self.sems[key], 1)
        self.cnt[key] += 1
        me = (key, self.cnt[key])
        self._mark(reads, writes, me)
        self.n_inst += 1
        return me

    def dma(self, out, in_, reads=(), writes=(), q="sp", **kw):
        i = self.dq_next.get(q, 0)
        self.dq_next[q] = (i + 1) % self.ndq
        key = ("dq" + q, i)
        self._sem(key)
        deps = self._deps(reads, writes)
        if self.cnt[key] > 0:
            deps.append((key, self.cnt[key]))
        self._wait(q, deps)
        ins = self.eng[q].dma_start(out=out, in_=in_, **kw)
        self.nw[q] = 0
        ins.then_inc(self.sems[key], 16)
        self.cnt[key] += 16
        me = (key, self.cnt[key])
        self._mark(reads, writes, me)
        self.n_inst += 1
        return me

    def idma(self, out, in_, in_off=None, out_off=None, bound=0, reads=(), writes=()):
        q = "pool"
        i = self.dq_next.get("ind", 0)
        self.dq_next["ind"] = (i + 1) % self.ndq
        key = ("dqind", i)
        self._sem(key)
        deps = self._deps(reads, writes)
        if self.cnt[key] > 0:
            deps.append((key, self.cnt[key]))
        self._wait(q, deps)
        if bound not in self.bregs:
            rg = self.nc.gpsimd.alloc_register("bnd%d" % len(self.bregs))
            self.nc.gpsimd.reg_mov(rg, int(bound))
            self.bregs[bound] = rg
        ins = self.nc.gpsimd.indirect_dma_start(
            out=out, out_offset=(bass.IndirectOffsetOnAxis(ap=out_off, axis=0) if out_off is not None else None),
            in_=in_, in_offset=(bass.IndirectOffsetOnAxis(ap=in_off, axis=0) if in_off is not None else None),
            bounds_check=self.bregs[bound], oob_is_err=False)
        self.nw[q] = 0
        ins.then_inc(self.sems[key], 16)
        self.cnt[key] += 16
        me = (key, self.cnt[key])
        self._mark(reads, writes, me)
        self.n_inst += 1
        return me

    def barrier(self):
        allk = [(key, c) for key, c in self.cnt.items() if c > 0]
        for e in self.eng:
            self._wait(e, allk)

    def close(self):
        self.es.close()


class Cfg:
    def __init__(self, NB=2, C=256, L=2048, layers=(0, 1), dbg=False, stages=None):
        self.NB, self.C, self.L = NB, C, L
        self.T = C + L
        self.layers = layers
        self.dbg = dbg
        self.stages = stages


def tiles(t0, t1, w):
    out = []
    t = t0
    while t < t1:
        ww = min(w, t1 - t)
        out.append((t, ww))
        t += ww
    return out


def host_consts(cfg):
    L = cfg.L
    c = {}
    c["ident"] = np.eye(128, dtype=np.float32)
    rows = L // GRID_W
    rid, cid = np.meshgrid(np.arange(rows, dtype=np.float32), np.arange(GRID_W, dtype=np.float32), indexing="ij")
    nf = HD // 4
    inv = (10000.0 ** (-np.arange(nf, dtype=np.float32) / nf)).astype(np.float32)
    ang = np.concatenate([rid.reshape(-1, 1) * inv, cid.reshape(-1, 1) * inv], axis=-1).astype(np.float32)
    cos = np.cos(ang).astype(np.float32).T
    sin = np.sin(ang).astype(np.float32).T
    c["rope_cos"] = np.ascontiguousarray(np.concatenate([cos, cos, cos, cos], axis=0))
    c["rope_sin"] = np.ascontiguousarray(np.concatenate([-sin, sin, -sin, sin], axis=0))
    sel = np.zeros((3, 3, 128), np.float32)
    for r in range(3):
        sel[r, r, :] = 1.0
    c["sel3"] = sel.reshape(3, 3 * 128)
    s = np.arange(128)[:, None]
    t = np.arange(128)[None, :]
    cw = -math.exp(-0.5)
    c["tri"] = np.stack([
        np.where(s <= t, cw, 0.0), np.where(s < t, cw, 0.0),
        np.where(s >= t, cw, 0.0), np.where(s > t, cw, 0.0),
    ]).astype(np.float32).transpose(1, 0, 2).reshape(128, 4 * 128)
    c["msk"] = np.stack([s < t, s <= t, s > t, s >= t]).astype(np.float32).transpose(1, 0, 2).reshape(128, 4 * 128)
    c["iotap"] = np.arange(128, dtype=np.float32).reshape(128, 1)
    c["blkoff"] = np.ascontiguousarray(np.broadcast_to((np.arange(128, dtype=np.float32) * EB)[None, :], (128, 128)))
    bi = np.arange(128)
    bmk = lambda B: (bi[:, None] // B == bi[None, :] // B).astype(np.float32)
    c["bmsk"] = np.stack([bmk(16), bmk(32) - bmk(16), bmk(64) - bmk(32), bmk(128) - bmk(64)]).transpose(1, 0, 2).reshape(128, 512)
    return c


CONST_SHAPES = lambda cfg: {
    "ident": [128, 128], "rope_cos": [128, cfg.L], "rope_sin": [128, cfg.L], "sel3": [3, 384],
    "tri": [128, 512], "msk": [128, 512], "bmsk": [128, 512], "iotap": [128, 1], "blkoff": [128, 128],
}

PARAM_SHAPES = {
    "w_mod": [DEPTH, D, 6 * D], "b_mod": [DEPTH, 6 * D], "w_in": [DEPTH, D, DIN], "rwkv_conv": [DEPTH, 3, 1024],
    "rwkv_w0": [DEPTH, 2, RW], "rwkv_w2": [DEPTH, 2, 64, RW], "rwkv_a0": [DEPTH, 2, RW], "rwkv_a2": [DEPTH, 2, 64, RW],
    "rwkv_g2": [DEPTH, 128, RW], "rwkv_k_k": [DEPTH, RW], "rwkv_k_a": [DEPTH, RW], "rwkv_r_k": [DEPTH, NH, HD],
    "rwkv_gn_w": [DEPTH, RW], "rwkv_gn_b": [DEPTH, RW],
    "s5_lam_re": [DEPTH, 2, S5G, S5P], "s5_lam_im": [DEPTH, 2, S5G, S5P], "s5_log_dt": [DEPTH, 2, S5G],
    "s5_b_re": [DEPTH, S5G, S5P, S5C], "s5_b_im": [DEPTH, S5G, S5P, S5C], "s5_c_re": [DEPTH, S5G, S5C, S5P],
    "s5_c_im": [DEPTH, S5G, S5C, S5P], "s5_d": [DEPTH, S5W], "s5_glu_w": [DEPTH, S5W, S5W], "s5_glu_b": [DEPTH, S5W],
    "attn_sink": [DEPTH, AH], "w_out": [DEPTH, D, D], "ln1_g": [DEPTH, D], "ln1_b": [DEPTH, D], "ln2_g": [DEPTH, D],
    "ln2_b": [DEPTH, D], "router_group_w": [DEPTH, D, 4], "router_group_b": [DEPTH, 4],
    "router_expert_w": [DEPTH, D, NEXP], "router_expert_b": [DEPTH, NEXP],
    "ewg_a": [DEPTH * NEXP * 128, 2048], "ewg_b": [DEPTH * NEXP * 128, 2048], "ewu_a": [DEPTH * NEXP * 128, 2048],
    "ewu_b": [DEPTH * NEXP * 128, 2048], "ewd_a": [DEPTH * NEXP * 128, 2048], "ewd_b": [DEPTH * NEXP * 128, 2048],
}


class State:
    pass


def R3(ap, pat, **kw):
    return ap.rearrange(pat, **kw)


def stage_mod(S, l):
    k, cfg = S.k, S.cfg
    R = cfg.NB + 1
    st = contextlib.ExitStack()
    cc = k.sbuf(st, "cc", [R, D]); t_cc = Tr()
    k.dma(cc[:], S.inp["cc"][:, :], writes=[t_cc])
    k.op("act", lambda e: e.activation(out=cc[:], in_=cc[:], func=AF.Silu), reads=[t_cc], writes=[t_cc])
    siluT = k.sbuf(st, "siluT", [128, 8, R]); t_sT = Tr()
    ps, tps = k.ps()
    for kc in range(8):
        k.op("pe", lambda e: e.transpose(out=ps[:, kc * R:(kc + 1) * R], in_=cc[:, kc * 128:(kc + 1) * 128],
                                         identity=S.ident[0:R, 0:R]), reads=[t_cc, S.t_const], writes=[tps])
    k.op("dve", lambda e: e.tensor_copy(out=siluT[:].rearrange("p a b -> p (a b)"), in_=ps[:, 0:8 * R]),
         reads=[tps], writes=[t_sT])
    bmod = k.sbuf(st, "bmod", [1, 6 * D]); t_bm = Tr()
    k.dma(bmod[:], S.inp["b_mod"][l:l + 1, :], writes=[t_bm])
    ones = k.sbuf(st, "ones", [1, 128]); t_on = Tr()
    k.op("dve", lambda e: e.memset(ones[:], 1.0), writes=[t_on])
    gaterow = k.sbuf(st, "gaterow", [R, 2, D]); t_gr = Tr()
    gateb = k.sbuf(st, "gateb", [128, 2, R, D]); t_gateb = Tr()
    wms = [(k.sbuf(st, "wm", [128, 8, 1024]), Tr()) for _ in range(2)]
    psm, tpsm = k.ps()
    wsrc = S.inp["w_mod"]
    for g in range(6):
        wm, twm = wms[g % 2]
        for kc in range(8):
            k.dma(wm[:, kc, :], wsrc[l, kc * 128:(kc + 1) * 128, g * 1024:(g + 1) * 1024], writes=[twm],
                  q=("sp" if kc % 2 == 0 else "act"))
        for oc in range(8):
            col = (g * 8 + oc) * R
            for kc in range(8):
                k.op("pe", lambda e: e.matmul(psm[:, col:col + R], wm[:, kc, oc * 128:(oc + 1) * 128], siluT[:, kc, :],
                                              start=(kc == 0), stop=False), reads=[twm, t_sT], writes=[tpsm])
            k.op("pe", lambda e: e.matmul(psm[:, col:col + R], bmod[0:1, (g * 8 + oc) * 128:(g * 8 + oc + 1) * 128],
                                          ones[0:1, 0:R], start=False, stop=True), reads=[t_bm, t_on], writes=[tpsm])
        if g in (2, 5):
            gi = 0 if g == 2 else 1
            for half in range(2):
                pg, tpg = k.ps()
                for kc in range(8):
                    k.op("pe", lambda e: e.matmul(pg[0:R, 0:512], siluT[:, kc, :], wm[:, kc, half * 512:(half + 1) * 512],
                                                  start=(kc == 0), stop=False), reads=[twm, t_sT], writes=[tpg])
                k.op("pe", lambda e: e.matmul(pg[0:R, 0:512], ones[0:1, 0:R],
                                              bmod[0:1, g * 1024 + half * 512:g * 1024 + (half + 1) * 512],
                                              start=False, stop=True), reads=[t_bm, t_on], writes=[tpg])
                k.op("act", lambda e: e.copy(out=gaterow[:, gi, half * 512:(half + 1) * 512], in_=pg[0:R, 0:512]),
                     reads=[tpg], writes=[t_gr])
    k.op("dve", lambda e: e.tensor_copy(out=S.mod[:].rearrange("p a b -> p (a b)"), in_=psm[:, 0:48 * R]),
         reads=[tpsm], writes=[S.t_mod])
    for base in (8, 32):
        k.op("dve", lambda e: e.tensor_scalar_add(out=S.mod[:, base:base + 8, :], in0=S.mod[:, base:base + 8, :],
                                                  scalar1=1.0), reads=[S.t_mod], writes=[S.t_mod])
    for gi in range(2):
        for r in range(R):
            for half in range(2):
                pg, tpg = k.ps()
                k.op("pe", lambda e: e.matmul(pg[:, 0:512], S.sel3[0:R, r * 128:(r + 1) * 128],
                                              gaterow[0:R, gi, half * 512:(half + 1) * 512], start=True, stop=True),
                     reads=[t_gr, S.t_const], writes=[tpg])
                k.op("act", lambda e: e.copy(out=gateb[:, gi, r, half * 512:(half + 1) * 512], in_=pg[:, 0:512]),
                     reads=[tpg], writes=[t_gateb])
    k.dma(S.GBD[:, :], gateb[:].rearrange("p a b c -> p (a b c)"), reads=[t_gateb], writes=[S.t_GBD])
    k.barrier()
    st.close()


def stage_inproj(S, l):
    k, cfg = S.k, S.cfg
    NB, C, L, T = cfg.NB, cfg.C, cfg.L, cfg.T
    st = contextlib.ExitStack()
    w = k.sbuf(st, "win", [128, 8, DIN], BF16); tw = Tr()
    ws = k.sbuf(st, "wsw", [128, 8, 640], BF16); tws = Tr()
    win = S.inp["w_in"]
    for kc in range(8):
        k.dma(w[:, kc, :], win[l, kc * 128:(kc + 1) * 128, :], writes=[tw], q="pool")
        src = win[l, kc * 128:(kc + 1) * 128, 1280:1920].rearrange("p (h two j) -> p h two j", two=2, j=32)
        dst = ws[:, kc, :].rearrange("p (h two j) -> p h two j", two=2, j=32)
        for half in range(2):
            k.dma(dst[:, :, 1 - half, :], src[:, :, half, :], writes=[tws], q="pool")
    cosT = k.sbuf(st, "cosT", [128, L]); sinT = k.sbuf(st, "sinT", [128, L]); t_rope = Tr()
    k.dma(cosT[:], S.inp["rope_cos"][:, :], writes=[t_rope])
    k.dma(sinT[:], S.inp["rope_sin"][:, :], writes=[t_rope])
    xins = [[(k.sbuf(st, "xin", [128, D]), Tr()) for _ in range(4)] for _ in range(2)]
    xms = [(k.sbuf(st, "xm", [128, 8, 512], BF16), Tr()) for _ in range(2)]
    stg = [(k.sbuf(st, "stg", [128, 512]), Tr()) for _ in range(4)]
    stgb = [(k.sbuf(st, "stgb", [128, 512], BF16), Tr()) for _ in range(4)]
    tmpa = [(k.sbuf(st, "tmpa", [128, 512]), Tr()) for _ in range(2)]
    tmpb = [(k.sbuf(st, "tmpb", [128, 512]), Tr()) for _ in range(2)]
    it = 0
    si = 0
    for b in range(NB):
        for (t0, wd) in tiles(0, C, 512) + tiles(C, T, 512):
            is_ctx = t0 < C
            r = NB if is_ctx else b
            tl = t0 - C
            nb = wd // 128
            xin = xins[it % 2]
            xm, txm = xms[it % 2]
            it += 1
            for tb in range(nb):
                k.dma(xin[tb][0][:], S.XR[b, t0 + tb * 128:t0 + (tb + 1) * 128, :], reads=[S.t_XR[b]], writes=[xin[tb][1]],
                      q=("sp" if tb % 2 == 0 else "act"))
            for fc in range(8):
                ps, tps = k.ps()
                for tb in range(nb):
                    k.op("pe", lambda e: e.transpose(out=ps[:, tb * 128:(tb + 1) * 128],
                                                     in_=xin[tb][0][:, fc * 128:(fc + 1) * 128], identity=S.ident[:, :]),
                         reads=[xin[tb][1], S.t_const], writes=[tps])
                k.op("act", lambda e: e.activation(out=xm[:, fc, 0:wd], in_=ps[:, 0:wd], func=AF.Identity,
                                                   scale=S.mod[:, 8 + fc, r:r + 1], bias=S.mod[:, fc, r:r + 1]),
                     reads=[tps, S.t_mod], writes=[txm])

            def proj(wt, twt, c0):
                ps, tps = k.ps()
                for kc in range(8):
                    k.op("pe", lambda e: e.matmul(ps[:, 0:wd], wt[:, kc, c0:c0 + 128], xm[:, kc, 0:wd],
                                                  start=(kc == 0), stop=(kc == 7)), reads=[twt, txm], writes=[tps])
                return ps, tps

            for oc in range(10):
                ps, tps = proj(w, tw, oc * 128)
                sg, tsg = stg[si % 4]
                si += 1
                if oc % 2 == 0:
                    k.op("dve", lambda e: e.tensor_copy(out=sg[:, 0:wd], in_=ps[:, 0:wd]), reads=[tps], writes=[tsg])
                else:
                    k.op("act", lambda e: e.copy(out=sg[:, 0:wd], in_=ps[:, 0:wd]), reads=[tps], writes=[tsg])
                if oc < 8:
                    k.dma(S.PR[b, oc * 128:(oc + 1) * 128, t0:t0 + wd], sg[:, 0:wd], reads=[tsg], writes=[S.t_PR[b]])
                else:
                    k.dma(S.PS[b, (oc - 8) * 128:(oc - 7) * 128, t0:t0 + wd], sg[:, 0:wd], reads=[tsg], writes=[S.t_PS[b]])
            for qc in range(5):
                ps, tps = proj(w, tw, 1280 + qc * 128)
                sb_, tsb = stgb[si % 4]
                si += 1
                if is_ctx:
                    k.op("act", lambda e: e.copy(out=sb_[:, 0:wd], in_=ps[:, 0:wd]), reads=[tps], writes=[tsb])
                else:
                    ps2, tps2 = proj(ws, tws, qc * 128)
                    ta, tta = tmpa[si % 2]
                    tb_, ttb = tmpb[si % 2]
                    k.op("dve", lambda e: e.tensor_tensor(out=ta[:, 0:wd], in0=ps[:, 0:wd], in1=cosT[:, tl:tl + wd],
                                                          op=ALU.mult), reads=[tps, t_rope], writes=[tta])
                    k.op("dve", lambda e: e.tensor_tensor(out=tb_[:, 0:wd], in0=ps2[:, 0:wd], in1=sinT[:, tl:tl + wd],
                                                          op=ALU.mult), reads=[tps2, t_rope], writes=[ttb])
                    k.op("pool", lambda e: e.tensor_tensor(out=sb_[:, 0:wd], in0=ta[:, 0:wd], in1=tb_[:, 0:wd],
                                                           op=ALU.add), reads=[tta, ttb], writes=[tsb])
                if qc < 4:
                    k.dma(S.QT[b, qc * 128:(qc + 1) * 128, t0:t0 + wd], sb_[:, 0:wd], reads=[tsb], writes=[S.t_QT[b]])
                else:
                    k.dma(S.KT[b, :, t0:t0 + wd], sb_[:, 0:wd], reads=[tsb], writes=[S.t_KT[b]])
            for tb in range(nb):
                ps, tps = k.ps()
                for kc in range(8):
                    k.op("pe", lambda e: e.matmul(ps[:, 0:128], xm[:, kc, tb * 128:(tb + 1) * 128], w[:, kc, 1920:2048],
                                                  start=(kc == 0), stop=(kc == 7)), reads=[tw, txm], writes=[tps])
                sb_, tsb = stgb[si % 4]
                si += 1
                k.op("act", lambda e: e.copy(out=sb_[:, 0:128], in_=ps[:, 0:128]), reads=[tps], writes=[tsb])
                k.dma(S.V[b, t0 + tb * 128:t0 + (tb + 1) * 128, :], sb_[:, 0:128], reads=[tsb], writes=[S.t_V[b]])
    k.barrier()
    st.close()


def build(cfg):
    nc = bass.Bass("TRN2", target_bir_lowering=False)
    k = KB(nc)
    S = State()
    S.k, S.cfg = k, cfg
    NB, C, L, T = cfg.NB, cfg.C, cfg.L, cfg.T
    S.inp = {}
    for name, shp in PARAM_SHAPES.items():
        S.inp[name] = nc.dram_tensor(name, shp, F32, kind="ExternalInput").ap()
    for name, shp in CONST_SHAPES(cfg).items():
        S.inp[name] = nc.dram_tensor(name, shp, F32, kind="ExternalInput").ap()
    S.inp["cc"] = nc.dram_tensor("cc", [NB + 1, D], F32, kind="ExternalInput").ap()
    S.inp["xall"] = nc.dram_tensor("xall", [NB, T, D], F32, kind="ExternalInput").ap()
    S.out = nc.dram_tensor("out", [NB, L, D], F32, kind="ExternalOutput").ap()
    S.t_out = Tr()
    skind = "ExternalOutput" if cfg.dbg else "Internal"
    S.XR = k.dram("XR", [NB, T, D], F32, skind)
    S.PR = k.dram("PR", [NB, 1024, T], F32, skind)
    S.PS = k.dram("PS", [NB, 256, T], F32, skind)
    S.QT = k.dram("QT", [NB, 512, T], BF16, skind)
    S.KT = k.dram("KT", [NB, 128, T], BF16, skind)
    S.V = k.dram("V", [NB, T, 128], BF16, skind)
    S.YT = k.dram("YT", [NB, 1024, T], BF16, skind)
    S.PC = k.dram("PC", [NB, 1024, T], F32, skind)
    S.YD = k.dram("YD", [NB, T, 260], F32, skind)
    S.YS = k.dram("YS", [NB, 2, 256, T], F32, skind)
    _tn = NB * T
    _pmax = ((2 * _tn + EB - 1) // EB + NEXP) * EB
    S.HS = k.dram("HS", [_tn, D], BF16, skind); S.t_HS = Tr()
    S.HSORT = k.dram("HSORT", [_pmax, D], BF16, skind); S.t_HSORT = Tr()
    S.YB = k.dram("YB", [_pmax, D], BF16, skind); S.t_YB = Tr()
    for nm in ("XR", "PR", "PS", "QT", "KT", "V", "YT", "YD", "YS", "PC", "YTa", "YTs"):
        setattr(S, "t_" + nm, [Tr() for _ in range(NB)])
    k.init_psum()
    es = k.es
    S.t_const = Tr()
    S.ident = k.sbuf(es, "ident", [128, 128])
    S.identb = k.sbuf(es, "identb", [128, 128], BF16)
    S.sel3 = k.sbuf(es, "sel3", [3, 384])
    k.dma(S.ident[:], S.inp["ident"][:, :], writes=[S.t_const])
    k.dma(S.identb[:], S.inp["ident"][:, :], writes=[S.t_const], q="pool")
    k.dma(S.sel3[:], S.inp["sel3"][:, :], writes=[S.t_const])
    S.mod = k.sbuf(es, "mod", [128, 48, NB + 1]); S.t_mod = Tr()
    S.GBD = k.dram("GBD", [128, 2 * (NB + 1) * D], F32, "Internal"); S.t_GBD = Tr()
    for b in range(NB):
        for (t0, wd) in tiles(0, T, 512):
            k.dma(S.XR[b, t0:t0 + wd, :], S.inp["xall"][b, t0:t0 + wd, :], writes=[S.t_XR[b]])
    if getattr(cfg, "inject_yt", False):
        ytin = nc.dram_tensor("ytin", [NB, 1024, T], BF16, kind="ExternalInput").ap()
        for b in range(NB):
            k.dma(S.YT[b], ytin[b], writes=[S.t_YT[b]])
    zst = contextlib.ExitStack()
    if stages_has_moe(cfg):
        zt = k.sbuf(zst, "zt", [128, 4, D], BF16); tzt = Tr()
        k.op("pool", lambda e: e.memset(zt[:], 0.0), writes=[tzt])
        for r0 in range(0, _pmax, 512):
            k.dma(S.HSORT[r0:r0 + 512, :].rearrange("(n p) d -> p n d", p=128), zt[:], reads=[tzt], writes=[S.t_HSORT],
                  q=("sp" if (r0 // 512) % 2 == 0 else "act"))
    k.barrier()
    zst.close()
    stages = cfg.stages
    for l in cfg.layers:
        last = (l == DEPTH - 1)
        if stages is None or "mod" in stages:
            stage_mod(S, l)
        if stages is None or "inproj" in stages:
            stage_inproj(S, l)
        if stages is None or "rwkv" in stages:
            stage_rwkv(S, l, last)
        if stages is None or ("s5" in stages and "attn" in stages):
            st_a = contextlib.ExitStack()
            ags = [attn_stream(S, l, last, st_a, bs=[b_]) for b_ in range(NB)]
            for ag_ in ags:
                next(ag_)
            ag = multi_stream(ags)
            stage_s5(S, l, last, side=ag)
            for _ in ag:
                pass
            k.barrier()
            st_a.close()
        else:
            if "s5" in stages:
                stage_s5(S, l, last)
            if "attn" in stages:
                stage_attn(S, l, last)
        if stages is None or "outln" in stages:
            stage_outln(S, l, last)
        if stages is None or "moe" in stages:
            stage_moe2(S, l, last)
    k.barrier()
    k.close()
    return nc


_EXPERT_CACHE = {}


def relayout_experts(inputs):
    key = id(inputs["expert_w_gate"])
    if key in _EXPERT_CACHE:
        return _EXPERT_CACHE[key]
    out = {}
    for nm, src, nchunk, width in (("ewg", "expert_w_gate", 8, DE), ("ewu", "expert_w_up", 8, DE), ("ewd", "expert_w_down", 4, D)):
        w = np.asarray(inputs[src], dtype=np.float32).reshape(DEPTH, NEXP, nchunk, 128, width)
        w = w.transpose(0, 1, 3, 2, 4)
        h = nchunk // 2
        out[nm + "_a"] = np.ascontiguousarray(w[:, :, :, :h, :]).reshape(DEPTH * NEXP * 128, 2048)
        out[nm + "_b"] = np.ascontiguousarray(w[:, :, :, h:, :]).reshape(DEPTH * NEXP * 128, 2048)
    _EXPERT_CACHE.clear()
    _EXPERT_CACHE[key] = out
    return out


def stages_has_moe(cfg):
    return (cfg.stages is None or "moe" in cfg.stages) and getattr(cfg, "sparse", True)


def multi_stream(gens):
    gens = list(gens)
    while gens:
        for g_ in list(gens):
            try:
                next(g_)
            except StopIteration:
                gens.remove(g_)
        yield


def make_in_maps(cfg, inputs, n_cores):
    consts = host_consts(cfg)
    NB = cfg.NB
    maps = []
    for ci in range(n_cores):
        m = {}
        for name in PARAM_SHAPES:
            if name.startswith("ew"):
                continue
            m[name] = np.ascontiguousarray(inputs[name], dtype=np.float32).reshape(PARAM_SHAPES[name])
        m.update(relayout_experts(inputs))
        m.update(consts)
        bs = slice(ci * NB, (ci + 1) * NB)
        m["cc"] = np.ascontiguousarray(np.concatenate([inputs["c"][bs], inputs["c_ctx"][None, :]], axis=0), dtype=np.float32)
        m["xall"] = np.ascontiguousarray(np.concatenate([inputs["ctx"][bs], inputs["x"][bs]], axis=1), dtype=np.float32)
        maps.append(m)
    return maps


def kernel(**inputs):
    cfg = Cfg()
    n = 8
    nc = build(cfg)
    maps = make_in_maps(cfg, inputs, n)
    res = run_bass_kernel_spmd(nc, maps, core_ids=list(range(n)))
    return np.concatenate([np.asarray(r["out"], dtype=np.float32) for r in res.results], axis=0)


def stage_attn(S, l, last):
    st = contextlib.ExitStack()
    for _ in attn_stream(S, l, last, st):
        pass
    S.k.barrier()
    st.close()


def attn_stream(S, l, last, st, bs=None):
    k, cfg = S.k, S.cfg
    NB, C, L, T = cfg.NB, cfg.C, cfg.L, cfg.T
    ncb = C // 128
    nkb = T // 128
    mskb = k.sbuf(st, "mskb", [128, 4, 128], BF16); t_msk = Tr()
    k.dma(mskb[:].rearrange("p a b -> p (a b)"), S.inp["msk"][:, :], writes=[t_msk], q="pool")
    esk = k.sbuf(st, "esk", [128, AH]); t_esk = Tr()
    k.dma(esk[64:65, :], S.inp["attn_sink"][l:l + 1, :], writes=[t_esk])
    k.op("act", lambda e: e.activation(out=esk[64:65, :], in_=esk[64:65, :], func=AF.Exp), reads=[t_esk], writes=[t_esk])
    onesr = k.sbuf(st, "onesr", [128, 64]); t_on = Tr()
    k.op("dve", lambda e: e.memset(onesr[:], 1.0), writes=[t_on])
    kT = k.sbuf(st, "kT", [64, T], BF16); t_kT = Tr()
    qTs = [(k.sbuf(st, "qT", [64, AG, 128], BF16), Tr()) for _ in range(2)]
    va = k.sbuf(st, "va", [128, nkb, 65], BF16); t_va = Tr()
    pts = [(k.sbuf(st, "pt", [128, 512], BF16), Tr()) for _ in range(3)]
    dens = [(k.sbuf(st, "den", [128, 512]), Tr()) for _ in range(1)]
    rbs = [(k.sbuf(st, "rb", [64, 512]), Tr()) for _ in range(1)]
    obs = [(k.sbuf(st, "ob", [64, 512], BF16), Tr()) for _ in range(2)]
    pi = 0
    oi = 0
    yield
    for b in (range(NB) if bs is None else bs):
        for kv in range(AKV):
            k.dma(kT[:], S.KT[b, kv * 64:(kv + 1) * 64, :], reads=[S.t_KT[b]], writes=[t_kT])
            k.op("dve", lambda e: e.memset(va[:, :, 64:65], 1.0), writes=[t_va])
            k.dma(va[:, :, 0:64], S.V[b, :, kv * 64:(kv + 1) * 64].rearrange("(n p) d -> p n d", p=128), reads=[S.t_V[b]],
                  writes=[t_va])
            qblocks = list(range(ncb, nkb)) if last else list(range(nkb))
            for qb in qblocks:
                if qb < ncb:
                    kbl = [(j, None) for j in range(ncb)]
                else:
                    kbl = [(j, None) for j in range(ncb)]
                    if qb - 1 >= ncb:
                        kbl.append((qb - 1, 3))
                    kbl.append((qb, None))
                    if qb + 1 < nkb:
                        kbl.append((qb + 1, 1))
                qT, t_qT = qTs[oi % 2]
                k.dma(qT[:], S.QT[b, kv * 256:(kv + 1) * 256, qb * 128:(qb + 1) * 128].rearrange("(g p) t -> p g t", p=64),
                      reads=[S.t_QT[b]], writes=[t_qT])
                po, tpo = k.ps()
                for i, (kb, m) in enumerate(kbl):
                    ps, tps = k.ps()
                    k.op("pe", lambda e: e.matmul(ps[:, 0:512], kT[:, kb * 128:(kb + 1) * 128], qT[:, :, :], start=True, stop=True),
                         reads=[t_kT, t_qT], writes=[tps])
                    pt, tpt = pts[pi % 3]
                    pi += 1
                    k.op("act", lambda e: e.activation(out=pt[:], in_=ps[:, 0:512], func=AF.Exp, scale=0.125),
                         reads=[tps], writes=[tpt])
                    if m is not None:
                        k.op("pool", lambda e: e.tensor_tensor(
                            out=pt[:].rearrange("p (g q) -> p g q", g=AG), in0=pt[:].rearrange("p (g q) -> p g q", g=AG),
                            in1=mskb[:, m:m + 1, :].to_broadcast([128, AG, 128]), op=ALU.mult),
                            reads=[tpt, t_msk], writes=[tpt])
                    k.op("pe", lambda e: e.matmul(po[0:65, 0:512], va[:, kb, :], pt[:], start=(i == 0),
                                                  stop=(i == len(kbl) - 1)), reads=[t_va, tpt], writes=[tpo])
                den, tden = dens[0]
                rb, trb = rbs[0]
                ob, tob = obs[oi % 2]
                oi += 1
                k.op("dve", lambda e: e.tensor_tensor(
                    out=den[64:65, :].rearrange("p (g q) -> p g q", g=AG), in0=po[64:65, 0:512].rearrange("p (g q) -> p g q", g=AG),
                    in1=esk[64:65, kv * AG:(kv + 1) * AG].unsqueeze(2).to_broadcast([1, AG, 128]), op=ALU.add),
                    reads=[tpo, t_esk], writes=[tden])
                k.op("dve", lambda e: e.reciprocal(out=den[64:65, :], in_=den[64:65, :]), reads=[tden], writes=[tden])
                pb, tpb = k.ps()
                k.op("pe", lambda e: e.matmul(pb[0:64, 0:512], onesr[64:65, 0:64], den[64:65, :], start=True, stop=True),
                     reads=[t_on, tden], writes=[tpb])
                k.op("act", lambda e: e.copy(out=rb[:], in_=pb[0:64, 0:512]), reads=[tpb], writes=[trb])
                k.op("dve", lambda e: e.tensor_tensor(out=ob[:], in0=po[0:64, 0:512], in1=rb[:], op=ALU.mult),
                     reads=[tpo, trb], writes=[tob])
                k.dma(S.YT[b, 512 + kv * 256:512 + (kv + 1) * 256, qb * 128:(qb + 1) * 128].rearrange("(g p) t -> p g t", p=64),
                      ob[:].rearrange("p (g q) -> p g q", g=AG), reads=[tob], writes=[S.t_YTa[b]])
                yield


def ln_block(S, z, tz, lng, lnb, t_ln, sm, tsm):
    k = S.k
    k.op("dve", lambda e: e.bn_stats(out=sm[:, 0:6], in_=z[:, 0:512]), reads=[tz], writes=[tsm])
    k.op("dve", lambda e: e.bn_stats(out=sm[:, 6:12], in_=z[:, 512:1024]), reads=[tz], writes=[tsm])
    k.op("dve", lambda e: e.bn_aggr(out=sm[:, 12:14], in_=sm[:, 0:12].rearrange("p (a b) -> p a b", b=6)),
         reads=[tsm], writes=[tsm])
    rsqrt_eps(k, sm[:, 14:15], sm[:, 13:14], LN_EPS, tsm)
    k.op("dve", lambda e: e.scalar_tensor_tensor(out=sm[:, 15:16], in0=sm[:, 12:13], scalar=-1.0, in1=sm[:, 14:15],
                                                 op0=ALU.mult, op1=ALU.mult), reads=[tsm], writes=[tsm])
    k.op("act", lambda e: e.activation(out=z[:], in_=z[:], func=AF.Identity, scale=sm[:, 14:15], bias=sm[:, 15:16]),
         reads=[tz, tsm], writes=[tz])
    k.op("dve", lambda e: e.tensor_tensor(out=z[:], in0=z[:], in1=lng[:], op=ALU.mult), reads=[tz, t_ln], writes=[tz])
    k.op("pool", lambda e: e.tensor_tensor(out=z[:], in0=z[:], in1=lnb[:], op=ALU.add), reads=[tz, t_ln], writes=[tz])


def rsqrt_eps(k, out, in_, eps, tr):
    k.op("dve", lambda e: e.tensor_scalar(out=out, in0=in_, scalar1=float(eps), scalar2=None, op0=ALU.add), reads=[tr], writes=[tr])
    k.op("act", lambda e: e.activation(out=out, in_=out, func=AF.Sqrt), reads=[tr], writes=[tr])
    k.op("dve", lambda e: e.reciprocal(out=out, in_=out), reads=[tr], writes=[tr])


def load_bcast_row(S, st, name, src_row, t):
    k = S.k
    row = k.sbuf(st, name + "r", [1, D]); trow = Tr()
    k.dma(row[:], src_row, writes=[trow])
    ones = k.sbuf(st, name + "o", [1, 128]); to = Tr()
    k.op("dve", lambda e: e.memset(ones[:], 1.0), writes=[to])
    out = k.sbuf(st, name, [128, D])
    for h in range(2):
        ps, tps = k.ps()
        k.op("pe", lambda e: e.matmul(ps[:, 0:512], ones[0:1, :], row[0:1, h * 512:(h + 1) * 512], start=True, stop=True),
             reads=[trow, to], writes=[tps])
        k.op("act", lambda e: e.copy(out=out[:, h * 512:(h + 1) * 512], in_=ps[:, 0:512]), reads=[tps], writes=[t])
    return out


def stage_outln(S, l, last):
    k, cfg = S.k, S.cfg
    NB, C, L, T = cfg.NB, cfg.C, cfg.L, cfg.T
    st = contextlib.ExitStack()
    wo = k.sbuf(st, "wo", [128, 8, D], BF16); two = Tr()
    for kc in range(8):
        k.dma(wo[:, kc, :], S.inp["w_out"][l, kc * 128:(kc + 1) * 128, :], writes=[two], q="pool")
    t_ln = Tr()
    lng = load_bcast_row(S, st, "lng", S.inp["ln1_g"][l:l + 1, :], t_ln)
    lnb = load_bcast_row(S, st, "lnb", S.inp["ln1_b"][l:l + 1, :], t_ln)
    R_ = NB + 1
    gb0 = k.sbuf(st, "gb0", [128, R_, D]); t_gb0 = Tr()
    k.dma(gb0[:].rearrange("p a b -> p (a b)"), S.GBD[:, 0:R_ * D], reads=[S.t_GBD], writes=[t_gb0])
    yts = [(k.sbuf(st, "yt", [128, 8, 128], BF16), Tr()) for _ in range(4)]
    xrs = [(k.sbuf(st, "xr", [128, D]), Tr()) for _ in range(4)]
    zs = [(k.sbuf(st, "z", [128, D]), Tr()) for _ in range(4)]
    sms = [(k.sbuf(st, "sm", [128, 32]), Tr()) for _ in range(4)]
    it = 0
    for b in range(NB):
        for t0 in range(C if last else 0, T, 128):
            r = NB if t0 < C else b
            yt, tyt = yts[it % 4]; xr, txr = xrs[it % 4]; z, tz = zs[it % 4]; sm, tsm = sms[it % 4]
            it += 1
            k.dma(yt[:], S.YT[b, :, t0:t0 + 128].rearrange("(kc p) t -> p kc t", p=128), reads=[S.t_YT[b]], writes=[tyt])
            k.dma(xr[:], S.XR[b, t0:t0 + 128, :], reads=[S.t_XR[b]], writes=[txr], q="act")
            for h in range(2):
                ps, tps = k.ps()
                for kc in range(8):
                    k.op("pe", lambda e: e.matmul(ps[:, 0:512], yt[:, kc, :], wo[:, kc, h * 512:(h + 1) * 512],
                                                  start=(kc == 0), stop=(kc == 7)), reads=[tyt, two], writes=[tps])
                k.op("dve", lambda e: e.tensor_tensor(out=z[:, h * 512:(h + 1) * 512], in0=ps[:, 0:512],
                                                      in1=gb0[:, r, h * 512:(h + 1) * 512], op=ALU.mult),
                     reads=[tps, t_gb0], writes=[tz])
            k.op("dve", lambda e: e.scalar_tensor_tensor(out=z[:], in0=xr[:], scalar=ALPHA, in1=z[:], op0=ALU.mult,
                                                          op1=ALU.add), reads=[txr, tz], writes=[tz])
            ln_block(S, z, tz, lng, lnb, t_ln, sm, tsm)
            k.dma(S.XR[b, t0:t0 + 128, :], z[:], reads=[tz], writes=[S.t_XR[b]])
    k.barrier()
    st.close()


def stage_s5(S, l, last, side=None):
    k, cfg = S.k, S.cfg
    NB, C, L, T = cfg.NB, cfg.C, cfg.L, cfg.T
    st = contextlib.ExitStack()
    nc = k.nc
    tp = Tr()
    P = lambda name, shape: k.sbuf(st, name, shape)
    halfpi = P("halfpi", [128, 1])
    k.op("dve", lambda e: e.memset(halfpi[:], math.pi / 2), writes=[tp])
    lre = P("lre", [128, 2, 8]); lim = P("lim", [128, 2, 8]); dt = P("dt", [128, 2, 8])
    for d in range(2):
        with nc.allow_non_contiguous_dma(reason="tiny param loads"):
            k.dma(lre[:, d, :], S.inp["s5_lam_re"][l, d].rearrange("(rc g2) p -> g2 p rc", g2=2), writes=[tp])
            k.dma(lim[:, d, :], S.inp["s5_lam_im"][l, d].rearrange("(rc g2) p -> g2 p rc", g2=2), writes=[tp])
            for g2 in range(2):
                k.dma(dt[g2 * 64:(g2 + 1) * 64, d, :],
                      S.inp["s5_log_dt"][l, d:d + 1, :].rearrange("o (rc g2) -> o g2 rc", g2=2)[:, g2, :].to_broadcast([64, 8]),
                      writes=[tp])
    dv = lambda fn: k.op("dve", fn, reads=[tp], writes=[tp])
    ac = lambda fn: k.op("act", fn, reads=[tp], writes=[tp])
    ac(lambda e: e.activation(out=dt[:], in_=dt[:], func=AF.Exp))
    mag = P("mag", [128, 2, 8]); th = P("th", [128, 2, 8]); cs = P("cs", [128, 2, 8]); sn = P("sn", [128, 2, 8])
    t1 = P("t1", [128, 2, 8]); t2 = P("t2", [128, 2, 8])
    dv(lambda e: e.tensor_tensor(out=mag[:], in0=lre[:], in1=dt[:], op=ALU.mult))
    ac(lambda e: e.activation(out=mag[:], in_=mag[:], func=AF.Exp))
    dv(lambda e: e.tensor_tensor(out=th[:], in0=lim[:], in1=dt[:], op=ALU.mult))
    ac(lambda e: e.activation(out=sn[:], in_=th[:], func=AF.Sin, scale=1.0 / 16))
    ac(lambda e: e.activation(out=cs[:], in_=th[:], func=AF.Sin, scale=1.0 / 16, bias=halfpi[:, 0:1]))
    for _ in range(4):
        dv(lambda e: e.tensor_tensor(out=t1[:], in0=cs[:], in1=cs[:], op=ALU.mult))
        dv(lambda e: e.tensor_tensor(out=t2[:], in0=sn[:], in1=sn[:], op=ALU.mult))
        dv(lambda e: e.tensor_tensor(out=sn[:], in0=sn[:], in1=cs[:], op=ALU.mult))
        dv(lambda e: e.tensor_scalar(out=sn[:], in0=sn[:], scalar1=2.0, scalar2=None, op0=ALU.mult))
        dv(lambda e: e.tensor_tensor(out=cs[:], in0=t1[:], in1=t2[:], op=ALU.subtract))
    abr = P("abr", [128, 2, 8]); abi = P("abi", [128, 2, 8]); cre = P("cre", [128, 2, 8]); cim = P("cim", [128, 2, 8])
    den = P("den", [128, 2, 8])
    dv(lambda e: e.tensor_tensor(out=abr[:], in0=mag[:], in1=cs[:], op=ALU.mult))
    dv(lambda e: e.tensor_tensor(out=abi[:], in0=mag[:], in1=sn[:], op=ALU.mult))
    dv(lambda e: e.tensor_scalar(out=t1[:], in0=abr[:], scalar1=-1.0, scalar2=None, op0=ALU.add))
    dv(lambda e: e.tensor_tensor(out=den[:], in0=lre[:], in1=lre[:], op=ALU.mult))
    dv(lambda e: e.tensor_tensor(out=t2[:], in0=lim[:], in1=lim[:], op=ALU.mult))
    dv(lambda e: e.tensor_tensor(out=den[:], in0=den[:], in1=t2[:], op=ALU.add))
    dv(lambda e: e.reciprocal(out=den[:], in_=den[:]))
    dv(lambda e: e.tensor_tensor(out=cre[:], in0=t1[:], in1=lre[:], op=ALU.mult))
    dv(lambda e: e.tensor_tensor(out=t2[:], in0=abi[:], in1=lim[:], op=ALU.mult))
    dv(lambda e: e.tensor_tensor(out=cre[:], in0=cre[:], in1=t2[:], op=ALU.add))
    dv(lambda e: e.tensor_tensor(out=cre[:], in0=cre[:], in1=den[:], op=ALU.mult))
    dv(lambda e: e.tensor_tensor(out=cim[:], in0=abi[:], in1=lre[:], op=ALU.mult))
    dv(lambda e: e.tensor_tensor(out=t2[:], in0=t1[:], in1=lim[:], op=ALU.mult))
    dv(lambda e: e.tensor_tensor(out=cim[:], in0=cim[:], in1=t2[:], op=ALU.subtract))
    dv(lambda e: e.tensor_tensor(out=cim[:], in0=cim[:], in1=den[:], op=ALU.mult))
    bre = P("bre", [128, 8, 16]); bim = P("bim", [128, 8, 16])
    with nc.allow_non_contiguous_dma(reason="param loads"):
        k.dma(bre[:], S.inp["s5_b_re"][l].rearrange("(rc g2) p c -> g2 p rc c", g2=2), writes=[tp])
        k.dma(bim[:], S.inp["s5_b_im"][l].rearrange("(rc g2) p c -> g2 p rc c", g2=2), writes=[tp])
    ctr = P("ctr", [128, 8, 48]); cti = P("cti", [128, 8, 48])
    dv(lambda e: e.memset(ctr[:], 0.0))
    dv(lambda e: e.memset(cti[:], 0.0))
    with nc.allow_non_contiguous_dma(reason="param loads"):
        for g2 in range(2):
            src_r = S.inp["s5_c_re"][l].rearrange("(rc g2) c p -> g2 p rc c", g2=2)[g2]
            src_i = S.inp["s5_c_im"][l].rearrange("(rc g2) c p -> g2 p rc c", g2=2)[g2]
            for rc in range(8):
                k.dma(ctr[g2 * 64:(g2 + 1) * 64, rc, g2 * 32:g2 * 32 + 16], src_r[:, rc, :], writes=[tp])
                k.dma(cti[g2 * 64:(g2 + 1) * 64, rc, g2 * 32:g2 * 32 + 16], src_i[:, rc, :], writes=[tp])
    dv(lambda e: e.tensor_scalar(out=cti[:], in0=cti[:], scalar1=-1.0, scalar2=None, op0=ALU.mult))
    bbp = P("bbp", [128, 8, 48]); tq = P("tq", [128, 8, 16]); tq2 = P("tq2", [128, 8, 16])
    bbT = P("bbT", [48, 2, 2, 8, 128])
    for d in range(2):
        for ri in range(2):
            crb = cre[:, d, :].unsqueeze(2).to_broadcast([128, 8, 16])
            cib = cim[:, d, :].unsqueeze(2).to_broadcast([128, 8, 16])
            if ri == 0:
                dv(lambda e: e.tensor_tensor(out=tq[:], in0=bre[:], in1=crb, op=ALU.mult))
                dv(lambda e: e.tensor_tensor(out=tq2[:], in0=bim[:], in1=cib, op=ALU.mult))
                dv(lambda e: e.tensor_tensor(out=tq[:], in0=tq[:], in1=tq2[:], op=ALU.subtract))
            else:
                dv(lambda e: e.tensor_tensor(out=tq[:], in0=bim[:], in1=crb, op=ALU.mult))
                dv(lambda e: e.tensor_tensor(out=tq2[:], in0=bre[:], in1=cib, op=ALU.mult))
                dv(lambda e: e.tensor_tensor(out=tq[:], in0=tq[:], in1=tq2[:], op=ALU.add))
            dv(lambda e: e.memset(bbp[:], 0.0))
            dv(lambda e: e.tensor_copy(out=bbp[0:64, :, 0:16], in_=tq[0:64, :, :]))
            dv(lambda e: e.tensor_copy(out=bbp[64:128, :, 32:48], in_=tq[64:128, :, :]))
            for rc in range(8):
                ps, tps = k.ps()
                k.op("pe", lambda e: e.transpose(out=ps[0:48, 0:128], in_=bbp[:, rc, :], identity=S.ident[:, :]),
                     reads=[tp, S.t_const], writes=[tps])
                k.op("act", lambda e: e.copy(out=bbT[:, d, ri, rc, :], in_=ps[0:48, 0:128]), reads=[tps], writes=[tp])
    nd = 0
    while (1 << nd) < T:
        nd += 1
    rc_n = P("rcn", [128, 2, 8, nd + 1]); rs_n = P("rsn", [128, 2, 8, nd + 1])
    dv(lambda e: e.tensor_copy(out=rc_n[:, :, :, 0], in_=cs[:]))
    dv(lambda e: e.tensor_copy(out=rs_n[:, :, :, 0], in_=sn[:]))
    for i in range(nd):
        dv(lambda e: e.tensor_tensor(out=t1[:], in0=rc_n[:, :, :, i], in1=rc_n[:, :, :, i], op=ALU.mult))
        dv(lambda e: e.tensor_tensor(out=t2[:], in0=rs_n[:, :, :, i], in1=rs_n[:, :, :, i], op=ALU.mult))
        dv(lambda e: e.tensor_tensor(out=rc_n[:, :, :, i + 1], in0=t1[:], in1=t2[:], op=ALU.subtract))
        dv(lambda e: e.tensor_tensor(out=t1[:], in0=rc_n[:, :, :, i], in1=rs_n[:, :, :, i], op=ALU.mult))
        dv(lambda e: e.tensor_scalar(out=rs_n[:, :, :, i + 1], in0=t1[:], scalar1=2.0, scalar2=None, op0=ALU.mult))
    dskip = P("dskip", [128, 2]); glub = P("glub", [128, 2])
    with nc.allow_non_contiguous_dma(reason="param loads"):
        k.dma(dskip[:], S.inp["s5_d"][l].rearrange("(kc p) -> p kc", p=128), writes=[tp])
        k.dma(glub[:], S.inp["s5_glu_b"][l].rearrange("(kc p) -> p kc", p=128), writes=[tp])
    gluw = k.sbuf(st, "gluw", [128, 2, 256], BF16)
    k.dma(gluw[:], S.inp["s5_glu_w"][l].rearrange("(kc p) n -> p kc n", p=128), writes=[tp], q="pool")

    cosT = P("cosT5", [128, T]); sinT = P("sinT5", [128, T]); t_tab = Tr()
    tabT = P("tabT5", [128, T]); t_tabT = Tr()
    st_main = contextlib.ExitStack()
    PM = lambda name, shape: k.sbuf(st_main, name, shape)
    bufs = []
    for b in range(NB):
        d_ = {}
        for nm in ("zr", "zi", "gr", "gi"):
            d_[nm] = (PM(nm + "5", [128, T]), Tr())
        d_["up"] = (PM("up5", [48, T]), Tr())
        d_["stg"] = [(PM("stg5", [48, 512]), Tr()) for _ in range(1)]
        k.op("dve", lambda e: e.memset(d_["up"][0][:], 0.0), writes=[d_["up"][1]])
        bufs.append(d_)
    ustage = PM("ustage5", [48, T]); t_ust = Tr()
    k.op("dve", lambda e: e.memset(ustage[:], 0.0), writes=[t_ust])

    def body(d, rc, b):
        B_ = bufs[b]
        zr, t_zr = B_["zr"]; zi, t_zi = B_["zi"]; gr, t_gr = B_["gr"]; gi, t_gi = B_["gi"]
        up, t_up = B_["up"]; stg = B_["stg"]
        src = S.PS[b].rearrange("(rc g2 c) t -> rc g2 c t", g2=2, c=16)
        if d == 0:
            k.dma(up[0:16, :], src[rc, 0], reads=[S.t_PS[b]], writes=[t_up])
            k.dma(up[32:48, :], src[rc, 1], reads=[S.t_PS[b]], writes=[t_up])
        else:
            k.dma(ustage[0:16, :], src[rc, 0], reads=[S.t_PS[b]], writes=[t_ust])
            k.dma(ustage[32:48, :], src[rc, 1], reads=[S.t_PS[b]], writes=[t_ust])
            k.op("pool", lambda e: e.tensor_copy(out=up[:, 0:C], in_=ustage[:, 0:C][:, ::-1]), reads=[t_ust], writes=[t_up])
            k.op("pool", lambda e: e.tensor_copy(out=up[:, C:T], in_=ustage[:, C:T][:, ::-1]), reads=[t_ust], writes=[t_up])
        u_, tu_ = up, t_up
        yield
        for (t0, wd) in tiles(0, T, 512):
            pr_, tpr = k.ps()
            pi_, tpi = k.ps()
            k.op("pe", lambda e: e.matmul(pr_[:, 0:wd], bbT[:, d, 0, rc, :], u_[:, t0:t0 + wd], start=True, stop=True),
                 reads=[tp, tu_], writes=[tpr])
            k.op("pe", lambda e: e.matmul(pi_[:, 0:wd], bbT[:, d, 1, rc, :], u_[:, t0:t0 + wd], start=True, stop=True),
                 reads=[tp, tu_], writes=[tpi])
            sl = slice(t0, t0 + wd)
            k.op("dve", lambda e: e.tensor_tensor(out=zr[:, sl], in0=pr_[:, 0:wd], in1=cosT[:, sl], op=ALU.mult),
                 reads=[tpr, t_tab], writes=[t_zr])
            k.op("dve", lambda e: e.tensor_tensor(out=gr[:, sl], in0=pi_[:, 0:wd], in1=sinT[:, sl], op=ALU.mult),
                 reads=[tpi, t_tab], writes=[t_gr])
            k.op("pool", lambda e: e.tensor_tensor(out=zr[:, sl], in0=zr[:, sl], in1=gr[:, sl], op=ALU.add),
                 reads=[t_gr], writes=[t_zr])
            k.op("dve", lambda e: e.tensor_tensor(out=zi[:, sl], in0=pi_[:, 0:wd], in1=cosT[:, sl], op=ALU.mult),
                 reads=[tpi, t_tab], writes=[t_zi])
            k.op("dve", lambda e: e.tensor_tensor(out=gi[:, sl], in0=pr_[:, 0:wd], in1=sinT[:, sl], op=ALU.mult),
                 reads=[tpr, t_tab], writes=[t_gi])
            k.op("pool", lambda e: e.tensor_tensor(out=zi[:, sl], in0=zi[:, sl], in1=gi[:, sl], op=ALU.subtract),
                 reads=[t_gi], writes=[t_zi])
            yield
        mg = mag[:, d, rc:rc + 1].to_broadcast([128, T])
        k.op("dve", lambda e: e.tensor_tensor_scan(out=gr[:], data0=mg, data1=zr[:], initial=0.0, op0=ALU.mult, op1=ALU.add),
             reads=[t_zr, tp], writes=[t_gr])
        yield
        k.op("dve", lambda e: e.tensor_tensor_scan(out=gi[:], data0=mg, data1=zi[:], initial=0.0, op0=ALU.mult, op1=ALU.add),
             reads=[t_zi, tp], writes=[t_gi])
        yield
        k.op("dve", lambda e: e.tensor_tensor(out=zr[:], in0=gr[:], in1=cosT[:], op=ALU.mult), reads=[t_gr, t_tab], writes=[t_zr])
        k.op("pool", lambda e: e.tensor_tensor(out=zi[:], in0=gi[:], in1=sinT[:], op=ALU.mult), reads=[t_gi, t_tab], writes=[t_zi])
        yield
        k.op("dve", lambda e: e.tensor_tensor(out=zr[:], in0=zr[:], in1=zi[:], op=ALU.subtract), reads=[t_zi], writes=[t_zr])
        yield
        k.op("pool", lambda e: e.tensor_tensor(out=zi[:], in0=gi[:], in1=cosT[:], op=ALU.mult), reads=[t_gi, t_tab, t_zr], writes=[t_zi])
        k.op("dve", lambda e: e.tensor_tensor(out=gr[:], in0=gr[:], in1=sinT[:], op=ALU.mult), reads=[t_tab], writes=[t_gr])
        yield
        k.op("pool", lambda e: e.tensor_tensor(out=zi[:], in0=zi[:], in1=gr[:], op=ALU.add), reads=[t_gr], writes=[t_zi])
        yield
        for ti, (t0, wd) in enumerate(tiles(0, T, 512)):
            py, tpy = k.ps()
            k.op("pe", lambda e: e.matmul(py[0:48, 0:wd], ctr[:, rc, :], zr[:, t0:t0 + wd], start=True, stop=False),
                 reads=[tp, t_zr], writes=[tpy])
            k.op("pe", lambda e: e.matmul(py[0:48, 0:wd], cti[:, rc, :], zi[:, t0:t0 + wd], start=False, stop=True),
                 reads=[tp, t_zi], writes=[tpy])
            sg, tsg = stg[0]
            k.op("act", lambda e: e.copy(out=sg[:, 0:wd], in_=py[0:48, 0:wd]), reads=[tpy], writes=[tsg])
            dst = S.YS[b, d].rearrange("(rc g2 c) t -> rc g2 c t", g2=2, c=16)
            k.dma(dst[rc, 0, :, t0:t0 + wd], sg[0:16, 0:wd], reads=[tsg], writes=[S.t_YS[b]])
            k.dma(dst[rc, 1, :, t0:t0 + wd], sg[32:48, 0:wd], reads=[tsg], writes=[S.t_YS[b]])
            yield

    side_alive = [side is not None]
    for d in range(2):
        for rc in range(8):
            k.op("dve", lambda e: e.memset(cosT[:, 0:1], 1.0), reads=[t_tab], writes=[t_tab])
            k.op("dve", lambda e: e.memset(sinT[:, 0:1], 0.0), reads=[t_tab], writes=[t_tab])
            n = 1
            i = 0
            while n < T:
                m = min(n, T - n)
                cn = rc_n[:, d, rc, i:i + 1]; sn_ = rs_n[:, d, rc, i:i + 1]
                k.op("dve", lambda e: e.tensor_scalar(out=tabT[:, 0:m], in0=sinT[:, 0:m], scalar1=sn_, scalar2=None, op0=ALU.mult),
                     reads=[t_tab, tp], writes=[t_tabT])
                k.op("dve", lambda e: e.scalar_tensor_tensor(out=cosT[:, n:n + m], in0=cosT[:, 0:m], scalar=cn, in1=tabT[:, 0:m],
                                                             op0=ALU.mult, op1=ALU.subtract), reads=[t_tab, t_tabT, tp], writes=[t_tab])
                k.op("dve", lambda e: e.tensor_scalar(out=tabT[:, 0:m], in0=cosT[:, 0:m], scalar1=sn_, scalar2=None, op0=ALU.mult),
                     reads=[t_tab, tp], writes=[t_tabT])
                k.op("dve", lambda e: e.scalar_tensor_tensor(out=sinT[:, n:n + m], in0=sinT[:, 0:m], scalar=cn, in1=tabT[:, 0:m],
                                                             op0=ALU.mult, op1=ALU.add), reads=[t_tab, t_tabT, tp], writes=[t_tab])
                n *= 2
                i += 1
            gens = [body(d, rc, b) for b in range(NB)]
            while gens:
                for g_ in list(gens):
                    try:
                        next(g_)
                    except StopIteration:
                        gens.remove(g_)
                if side_alive[0]:
                    try:
                        next(side)
                    except StopIteration:
                        side_alive[0] = False
    k.barrier()
    st_main.close()
    TW = 512
    uu = [(P("uu", [128, 2, TW]), Tr()) for _ in range(2)]
    y0 = [(P("y0", [128, 2, TW]), Tr()) for _ in range(2)]
    y1 = [(P("y1", [128, 2, TW]), Tr()) for _ in range(2)]
    zz = [(P("zz", [128, 2, TW]), Tr()) for _ in range(2)]
    zb = [(k.sbuf(st, "zb", [128, 2, TW], BF16), Tr()) for _ in range(2)]
    ob = [(k.sbuf(st, "ob5", [128, 2, TW], BF16), Tr()) for _ in range(2)]
    it = 0
    GC = 2.0 * math.sqrt(2.0 / math.pi)
    for b in range(NB):
        segs = ([] if last else tiles(0, C, TW)) + tiles(C, T, TW)
        for (t0, wd) in segs:
            seg0, seg1 = (0, C) if t0 < C else (C, T)
            r0 = seg0 + (seg1 - (t0 + wd))
            u_, tu = uu[it % 2]; a0, ta0 = y0[it % 2]; a1, ta1 = y1[it % 2]; z_, tz = zz[it % 2]
            zb_, tzb = zb[it % 2]; ob_, tob = ob[it % 2]
            it += 1
            k.dma(u_[:, :, 0:wd], S.PS[b, :, t0:t0 + wd].rearrange("(kc p) t -> p kc t", p=128), reads=[S.t_PS[b]], writes=[tu])
            k.dma(a0[:, :, 0:wd], S.YS[b, 0, :, t0:t0 + wd].rearrange("(kc p) t -> p kc t", p=128), reads=[S.t_YS[b]], writes=[ta0])
            k.dma(a1[:, :, 0:wd], S.YS[b, 1, :, r0:r0 + wd].rearrange("(kc p) t -> p kc t", p=128), reads=[S.t_YS[b]], writes=[ta1],
                  q="act")
            for kc in range(2):
                k.op("dve", lambda e: e.scalar_tensor_tensor(out=z_[:, kc, 0:wd], in0=u_[:, kc, 0:wd], scalar=dskip[:, kc:kc + 1],
                                                             in1=a0[:, kc, 0:wd], op0=ALU.mult, op1=ALU.add),
                     reads=[tu, ta0, tp], writes=[tz])
            k.op("dve", lambda e: e.tensor_tensor(out=z_[:, :, 0:wd], in0=z_[:, :, 0:wd], in1=a1[:, :, 0:wd][:, :, ::-1], op=ALU.add),
                 reads=[ta1], writes=[tz])
            k.op("pool", lambda e: e.tensor_tensor(out=a0[:, :, 0:wd], in0=z_[:, :, 0:wd], in1=z_[:, :, 0:wd], op=ALU.mult),
                 reads=[tz], writes=[ta0])
            k.op("dve", lambda e: e.tensor_scalar(out=a0[:, :, 0:wd], in0=a0[:, :, 0:wd], scalar1=0.044715, scalar2=1.0,
                                                  op0=ALU.mult, op1=ALU.add), reads=[ta0], writes=[ta0])
            k.op("pool", lambda e: e.tensor_tensor(out=a0[:, :, 0:wd], in0=a0[:, :, 0:wd], in1=z_[:, :, 0:wd], op=ALU.mult),
                 reads=[tz, ta0], writes=[ta0])
            k.op("act", lambda e: e.activation(out=a0[:, :, 0:wd], in_=a0[:, :, 0:wd], func=AF.Sigmoid, scale=GC),
                 reads=[ta0], writes=[ta0])
            k.op("dve", lambda e: e.tensor_tensor(out=z_[:, :, 0:wd], in0=z_[:, :, 0:wd], in1=a0[:, :, 0:wd], op=ALU.mult),
                 reads=[ta0], writes=[tz])
            k.op("pool", lambda e: e.tensor_copy(out=zb_[:, :, 0:wd], in_=z_[:, :, 0:wd]), reads=[tz], writes=[tzb])
            for oc in range(2):
                ps, tps = k.ps()
                for kc in range(2):
                    k.op("pe", lambda e: e.matmul(ps[:, 0:wd], gluw[:, kc, oc * 128:(oc + 1) * 128], zb_[:, kc, 0:wd],
                                                  start=(kc == 0), stop=(kc == 1)), reads=[tp, tzb], writes=[tps])
                k.op("act", lambda e: e.activation(out=a1[:, oc, 0:wd], in_=ps[:, 0:wd], func=AF.Sigmoid, bias=glub[:, oc:oc + 1],
                                                   scale=1.0), reads=[tps, tp], writes=[ta1])
            k.op("dve", lambda e: e.tensor_tensor(out=ob_[:, :, 0:wd], in0=z_[:, :, 0:wd], in1=a1[:, :, 0:wd], op=ALU.mult),
                 reads=[tz, ta1], writes=[tob])
            k.dma(S.YT[b, 256:512, t0:t0 + wd].rearrange("(kc p) t -> p kc t", p=128), ob_[:, :, 0:wd], reads=[tob],
                  writes=[S.t_YT[b]])
    k.barrier()
    st.close()


def stage_rwkv_conv(S, l):
    k, cfg = S.k, S.cfg
    NB, C, L, T = cfg.NB, cfg.C, cfg.L, cfg.T
    st = contextlib.ExitStack()
    nc = k.nc
    cwT = k.sbuf(st, "cwT", [128, 8, 3]); tcw = Tr()
    with nc.allow_non_contiguous_dma(reason="tiny param load"):
        for j in range(3):
            k.dma(cwT[:, :, j], S.inp["rwkv_conv"][l, j].rearrange("(kc p) -> p kc", p=128), writes=[tcw])
    pins = [(k.sbuf(st, "pin", [128, 8, 514]), Tr()) for _ in range(2)]
    pos = [(k.sbuf(st, "po", [128, 8, 512]), Tr()) for _ in range(2)]
    it = 0
    for b in range(NB):
        for (s0, s1) in ((0, C), (C, T)):
            for (t0, wd) in tiles(s0, s1, 512):
                pin, tpin = pins[it % 2]; po, tpo = pos[it % 2]
                it += 1
                lo = max(t0 - 1, s0); hi = min(t0 + wd + 1, s1)
                if t0 == s0:
                    k.op("pool", lambda e: e.memset(pin[:, :, 0:1], 0.0), writes=[tpin])
                if t0 + wd == s1:
                    k.op("pool", lambda e: e.memset(pin[:, :, wd + 1:wd + 2], 0.0), writes=[tpin])
                o0 = lo - (t0 - 1)
                for kc in range(8):
                    k.dma(pin[:, kc, o0:o0 + (hi - lo)], S.PR[b, kc * 128:(kc + 1) * 128, lo:hi], reads=[S.t_PR[b]], writes=[tpin],
                          q=("sp" if kc % 2 == 0 else "act"))
                for kc in range(8):
                    en = "dve"
                    k.op(en, lambda e: e.tensor_scalar(out=po[:, kc, 0:wd], in0=pin[:, kc, 0:wd], scalar1=cwT[:, kc, 0:1], scalar2=None,
                                                       op0=ALU.mult), reads=[tpin, tcw], writes=[tpo])
                    for j in (1, 2):
                        k.op(en, lambda e: e.scalar_tensor_tensor(out=po[:, kc, 0:wd], in0=pin[:, kc, j:j + wd], scalar=cwT[:, kc, j:j + 1],
                                                                  in1=po[:, kc, 0:wd], op0=ALU.mult, op1=ALU.add),
                             reads=[tpin, tcw], writes=[tpo])
                for kc in range(8):
                    k.dma(S.PC[b, kc * 128:(kc + 1) * 128, t0:t0 + wd], po[:, kc, 0:wd], reads=[tpo], writes=[S.t_PC[b]])
    k.barrier()
    st.close()


def stage_rwkv(S, l, last):
    stage_rwkv_conv(S, l)
    k, cfg = S.k, S.cfg
    NB, C, L, T = cfg.NB, cfg.C, cfg.L, cfg.T
    st = contextlib.ExitStack()
    nc = k.nc
    CH = 128
    tp = Tr()
    P = lambda name, shape, dt=F32: k.sbuf(st, name, shape, dt)
    kkp = P("kkp", [64, 4]); kap = P("kap", [64, 4]); omka = P("omka", [64, 4]); rkp = P("rkp", [64, 4])
    a0T = P("a0T", [64, 2, 4])
    with nc.allow_non_contiguous_dma(reason="tiny param loads"):
        k.dma(kkp[:], S.inp["rwkv_k_k"][l].rearrange("(h p) -> p h", p=64), writes=[tp])
        k.dma(kap[:], S.inp["rwkv_k_a"][l].rearrange("(h p) -> p h", p=64), writes=[tp])
        k.dma(rkp[:], S.inp["rwkv_r_k"][l].rearrange("h p -> p h"), writes=[tp])
        for d in range(2):
            k.dma(a0T[:, d, :], S.inp["rwkv_a0"][l, d].rearrange("(h p) -> p h", p=64), writes=[tp])
    k.op("dve", lambda e: e.tensor_scalar(out=omka[:], in0=kap[:], scalar1=-1.0, scalar2=1.0, op0=ALU.mult, op1=ALU.add),
         reads=[tp], writes=[tp])
    w2a = P("w2a", [65, 2, 256]); a2 = P("a2", [64, 2, 256]); g2 = P("g2", [128, 256])
    for d in range(2):
        k.dma(w2a[0:64, d, :], S.inp["rwkv_w2"][l, d], writes=[tp])
        k.dma(w2a[64:65, d, :], S.inp["rwkv_w0"][l, d:d + 1, :], writes=[tp])
        k.dma(a2[:, d, :], S.inp["rwkv_a2"][l, d], writes=[tp])
    k.dma(g2[:], S.inp["rwkv_g2"][l], writes=[tp])
    tri = P("tri", [128, 4, 128]); mskb = P("mskb", [128, 4, 128], BF16)
    k.dma(tri[:].rearrange("p a b -> p (a b)"), S.inp["tri"][:, :], writes=[tp])
    k.dma(mskb[:].rearrange("p a b -> p (a b)"), S.inp["msk"][:, :], writes=[tp], q="pool")
    ones64 = P("ones64", [64, 64])
    k.op("dve", lambda e: e.memset(ones64[:], 1.0), reads=[tp], writes=[tp])
    t_gn = Tr()
    gnw = P("gnw", [128, 256]); gnb = P("gnb", [128, 256])
    for (dst, nm) in ((gnw, "rwkv_gn_w"), (gnb, "rwkv_gn_b")):
        row = P("gnrow", [1, 256]); trow = Tr()
        k.dma(row[:], S.inp[nm][l:l + 1, :], writes=[trow])
        onesr = P("gnones", [1, 128])
        k.op("dve", lambda e: e.memset(onesr[:], 1.0), writes=[trow])
        ps, tps = k.ps()
        k.op("pe", lambda e: e.matmul(ps[:, 0:256], onesr[0:1, :], row[0:1, :], start=True, stop=True), reads=[trow], writes=[tps])
        k.op("act", lambda e: e.copy(out=dst[:], in_=ps[:, 0:256]), reads=[tps], writes=[t_gn])

    NS = max(NB, 1)
    def mk(name, shape, dt=F32):
        return [(k.sbuf(st, name, shape, dt), Tr()) for _ in range(NS)]
    rkv_s = mk("rkv", [64, 3, 4, CH]); wl_s = mk("wl", [65, CH]); al_s = mk("al", [64, CH]); gl_s = mk("gl", [128, CH])
    kq_s = mk("kq", [64, 4, CH]); tA_s = mk("tA", [64, 4, CH]); kk_s = mk("kk", [64, 4, CH])
    lw_s = mk("lw", [128, 256]); Ep_s = mk("Ep", [64, 4, CH]); Em_s = mk("Em", [64, 4, CH]); Ex_s = mk("Ex", [64, 4, CH])
    a_s = mk("a", [64, 4, CH]); kdir_s = mk("kdir", [64, 4, CH]); bv_s = mk("bv", [64, 4, CH])
    kkq_s = mk("kkq", [64, 4, CH]); rq_s = mk("rq", [64, 4, CH]); kd_s = mk("kd", [64, 4, CH]); bd_s = mk("bd", [64, 4, CH])
    kE_s = mk("kE", [64, 4, CH]); bE_s = mk("bE", [64, 4, CH])
    fb_s = mk("fb", [64, 4, 4, CH], BF16)
    tm_s = mk("tm", [128, 3, 4, 64], BF16)
    vtm_s = mk("vtm", [128, 4, 64]); vtb_s = mk("vtb", [128, 4, 64], BF16)
    A_s = mk("A", [128, 5, 4, CH], BF16)
    ivs = [{nm: (k.sbuf(st, "iv" + nm, [128, 4, CH], BF16), Tr()) for nm in
            ("N0", "N0T", "Xa", "Xb", "XTa", "XTb", "P0", "P1", "PT0", "PT1", "W", "W2")} for _ in range(NS)]
    bmskb = P("bmskb", [128, 4, 128], BF16)
    k.dma(bmskb[:].rearrange("p a b -> p (a b)"), S.inp["bmsk"][:, :], writes=[tp], q="pool")
    rhs2_s = mk("rhs2", [128, 4, 128], BF16); mu_s = mk("mu", [128, 4, 128], BF16)
    GT_s = mk("GT", [64, 4, 64]); J_s = mk("J", [64, 4, 64]); RT_s = mk("RT", [64, 4, CH])
    ys_s = mk("ysb", [128, 260]); yd0_s = mk("yd0", [128, 260])
    rk_s = mk("rk", [64, 4, CH])
    yc_s = mk("yc", [128, 4, 64]); y2_s = mk("y2", [128, 4, 64]); st_s = mk("stt", [128, 16]); gt_s = mk("gts", [128, 256])
    ot_s = mk("ot", [128, 2, CH], BF16)
    Hb = [[(P("H", [64, 4, 64]), Tr()) for _ in range(2)] for _ in range(NB * 2)]
    def chain(b, d):
        if True:
            Hs = Hb[b * 2 + d]
            hcur = 0
            k.op("dve", lambda e: e.memset(Hs[0][0][:], 0.0), writes=[Hs[0][1]])
            ctxc = list(range(0, C, CH)); latc = list(range(C, T, CH))
            order = (ctxc + latc) if d == 0 else (ctxc[::-1] + latc[::-1])
            iend = CH - 1 if d == 0 else 0
            m_strict, m_incl, m_nt = (0, 1, 2) if d == 0 else (2, 3, 0)
            tri_i, tri_x = (0, 1) if d == 0 else (2, 3)
            for t0 in order:
                emit = not (last and t0 < C)
                s = b % NS
                yield
                g = lambda lst: lst[s]
                rkv, trkv = g(rkv_s); wl, twl = g(wl_s); al, tal = g(al_s); gl, tgl = g(gl_s)
                for f in range(3):
                    k.dma(rkv[:, f], S.PC[b, f * 256:(f + 1) * 256, t0:t0 + CH].rearrange("(h p) t -> p h t", p=64),
                          reads=[S.t_PC[b]], writes=[trkv], q=("sp" if f != 1 else "act"))
                k.dma(wl[0:64, :], S.PC[b, 768:832, t0:t0 + CH], reads=[S.t_PC[b]], writes=[twl])
                k.dma(al[:], S.PC[b, 832:896, t0:t0 + CH], reads=[S.t_PC[b]], writes=[tal], q="act")
                if emit and d == 1:
                    k.dma(gl[:], S.PC[b, 896:1024, t0:t0 + CH], reads=[S.t_PC[b]], writes=[tgl])
                r_, k_, v_ = rkv[:, 0], rkv[:, 1], rkv[:, 2]
                B3 = lambda ap2: ap2.unsqueeze(2).to_broadcast([64, 4, CH])
                def tt(en, out, in0, in1, op, rd, wr):
                    k.op(en, lambda e: e.tensor_tensor(out=out, in0=in0, in1=in1, op=op), reads=rd, writes=wr)
                kq, tkq = g(kq_s); tA, ttA = g(tA_s); kk, tkk = g(kk_s)
                tt("dve", kq[:], k_, B3(kkp[:, :]), ALU.mult, [trkv, tp], [tkq])
                tt("pool", tA[:], kq[:], kq[:], ALU.mult, [tkq], [ttA])
                ps, tps = k.ps()
                k.op("pe", lambda e: e.matmul(ps[0:64, 0:512], ones64[:, :], tA[:].rearrange("p h t -> p (h t)"), start=True, stop=True),
                     reads=[ttA, tp], writes=[tps])
                k.op("dve", lambda e: e.tensor_scalar(out=tA[:].rearrange("p h t -> p (h t)"), in0=ps[0:64, 0:512], scalar1=1e-12,
                                                      scalar2=None, op0=ALU.add), reads=[tps], writes=[ttA])
                k.op("act", lambda e: e.activation(out=tA[:], in_=tA[:], func=AF.Sqrt), reads=[ttA], writes=[ttA])
                k.op("dve", lambda e: e.reciprocal(out=tA[:], in_=tA[:]), reads=[ttA], writes=[ttA])
                tt("dve", kk[:], kq[:], tA[:], ALU.mult, [tkq, ttA], [tkk])
                k.op("act", lambda e: e.activation(out=wl[0:64, :], in_=wl[0:64, :], func=AF.Tanh), reads=[twl], writes=[twl])
                k.op("dve", lambda e: e.memset(wl[64:65, :], 1.0), reads=[twl], writes=[twl])
                yield
                lw, tlw = g(lw_s)
                ps, tps = k.ps()
                k.op("pe", lambda e: e.matmul(ps[:, 0:256], wl[:, :], w2a[:, d, :], start=True, stop=True), reads=[twl, tp], writes=[tps])
                k.op("act", lambda e: e.activation(out=lw[:], in_=ps[:, 0:256], func=AF.Sigmoid), reads=[tps], writes=[tlw])
                pL, tpL = k.ps()
                pX, tpX = k.ps()
                for h in range(4):
                    k.op("pe", lambda e: e.matmul(pL[0:64, h * CH:(h + 1) * CH], lw[:, h * 64:(h + 1) * 64], tri[:, tri_i, :],
                                                  start=True, stop=True), reads=[tlw, tp], writes=[tpL])
                    k.op("pe", lambda e: e.matmul(pX[0:64, h * CH:(h + 1) * CH], lw[:, h * 64:(h + 1) * 64], tri[:, tri_x, :],
                                                  start=True, stop=True), reads=[tlw, tp], writes=[tpX])
                Ep, tEp = g(Ep_s); Em, tEm = g(Em_s); Ex, tEx = g(Ex_s)
                F2 = lambda t_: t_[:].rearrange("p h t -> p (h t)")
                k.op("act", lambda e: e.activation(out=F2(Ep), in_=pL[0:64, 0:512], func=AF.Exp), reads=[tpL], writes=[tEp])
                k.op("act", lambda e: e.activation(out=F2(Em), in_=pL[0:64, 0:512], func=AF.Exp, scale=-1.0), reads=[tpL], writes=[tEm])
                k.op("act", lambda e: e.activation(out=F2(Ex), in_=pX[0:64, 0:512], func=AF.Exp), reads=[tpX], writes=[tEx])
                a_, ta_ = g(a_s)
                pa, tpa = k.ps()
                for h in range(4):
                    k.op("pe", lambda e: e.matmul(pa[0:64, h * CH:(h + 1) * CH], a2[:, d, h * 64:(h + 1) * 64], al[:, :], start=True, stop=True),
                         reads=[tal, tp], writes=[tpa])
                for h in range(4):
                    k.op("act", lambda e: e.activation(out=a_[:, h, :], in_=pa[0:64, h * CH:(h + 1) * CH], func=AF.Sigmoid,
                                                       bias=a0T[:, d, h:h + 1], scale=1.0), reads=[tpa, tp], writes=[ta_])
                yield
                kdir, tkd_ = g(kdir_s); bv, tbv = g(bv_s)
                tt("dve", kdir[:], a_[:], B3(kap[:, :]), ALU.mult, [ta_, tp], [tkd_])
                tt("pool", kdir[:], kdir[:], B3(omka[:, :]), ALU.add, [tp], [tkd_])
                tt("dve", kdir[:], kdir[:], k_, ALU.mult, [trkv], [tkd_])
                tt("pool", bv[:], kk[:], a_[:], ALU.mult, [tkk, ta_], [tbv])
                kkq, tkkq = g(kkq_s); rq, trq = g(rq_s); kd, tkd = g(kd_s); bd, tbd = g(bd_s); kE, tkE = g(kE_s); bE, tbE = g(bE_s)
                tt("dve", kkq[:], kk[:], Ex[:], ALU.mult, [tkk, tEx], [tkkq])
                tt("pool", rq[:], r_, Ep[:], ALU.mult, [trkv, tEp], [trq])
                tt("dve", kd[:], kdir[:], Em[:], ALU.mult, [tkd_, tEm], [tkd])
                tt("pool", bd[:], bv[:], Em[:], ALU.mult, [tbv, tEm], [tbd])
                wCb = Ep[:, :, iend:iend + 1].to_broadcast([64, 4, CH])
                tt("dve", kE[:], kd[:], wCb, ALU.mult, [tkd, tEp], [tkE])
                tt("pool", bE[:], bd[:], wCb, ALU.mult, [tbd, tEp], [tbE])
                fb, tfb = g(fb_s)
                for i_, (src, tsrc) in enumerate(((kkq, tkkq), (rq, trq), (kd, tkd), (bd, tbd))):
                    k.op("pool" if i_ % 2 else "act", (lambda e: e.tensor_copy(out=fb[:, i_], in_=src[:])) if i_ % 2 else
                         (lambda e: e.copy(out=fb[:, i_], in_=src[:])), reads=[tsrc], writes=[tfb])
                yield
                tm, ttm = g(tm_s); vtm, tvtm = g(vtm_s); vtb, tvtb = g(vtb_s)
                for i_, (src, tsrc) in enumerate(((kkq, tkkq), (kE, tkE), (bE, tbE))):
                    ps, tps = k.ps()
                    for h in range(4):
                        k.op("pe", lambda e: e.transpose(out=ps[:, h * 64:(h + 1) * 64], in_=src[:, h, :], identity=S.ident[0:64, 0:64]),
                             reads=[tsrc, S.t_const], writes=[tps])
                    k.op("act" if i_ % 2 else "dve", (lambda e: e.copy(out=tm[:, i_].rearrange("p h v -> p (h v)"), in_=ps[:, 0:256])) if i_ % 2
                         else (lambda e: e.tensor_copy(out=tm[:, i_].rearrange("p h v -> p (h v)"), in_=ps[:, 0:256])), reads=[tps], writes=[ttm])
                ps, tps = k.ps()
                for h in range(4):
                    k.op("pe", lambda e: e.transpose(out=ps[:, h * 64:(h + 1) * 64], in_=v_[:, h, :], identity=S.ident[0:64, 0:64]),
                         reads=[trkv, S.t_const], writes=[tps])
                k.op("act", lambda e: e.copy(out=vtm[:].rearrange("p h v -> p (h v)"), in_=ps[:, 0:256]), reads=[tps], writes=[tvtm])
                k.op("dve", lambda e: e.tensor_copy(out=vtb[:].rearrange("p h v -> p (h v)"), in_=ps[:, 0:256]), reads=[tps], writes=[tvtb])
                yield
                A, tA5 = g(A_s)
                specs = ((3, 0, m_strict), (0, 3, m_nt), (2, 0, m_strict), (2, 1, m_incl), (3, 1, m_incl))
                for ai, (li, ri, mi) in enumerate(specs):
                    ps, tps = k.ps()
                    for h in range(4):
                        k.op("pe", lambda e: e.matmul(ps[:, h * CH:(h + 1) * CH], fb[:, li, h, :], fb[:, ri, h, :], start=True, stop=True),
                             reads=[tfb], writes=[tps])
                    k.op("dve", lambda e: e.tensor_tensor(out=A[:, ai], in0=ps[:, 0:512].rearrange("p (h t) -> p h t", h=4),
                                                          in1=mskb[:, mi:mi + 1, :].to_broadcast([128, 4, CH]), op=ALU.mult),
                         reads=[tps, tp], writes=[tA5])
                yield
                F3 = lambda t_: t_[:].rearrange("p h t -> p (h t)")
                def mmg(lhs, tl, rhs, tr_):
                    ps_, tps_ = k.ps()
                    for h in range(4):
                        k.op("pe", lambda e: e.matmul(ps_[:, h * CH:(h + 1) * CH], lhs[:, h, :], rhs[:, h, :], start=True, stop=True),
                             reads=[tl, tr_], writes=[tps_])
                    return ps_, tps_
                def bm(i_):
                    return bmskb[:, i_:i_ + 1, :].to_broadcast([128, 4, CH])
                iv = ivs[s]
                N0, tN0 = iv["N0"]; N0T, tN0T = iv["N0T"]
                Xc, tXc = iv["Xa"]; XTc, tXTc = iv["XTa"]
                Xo, tXo = iv["Xb"]; XTo, tXTo = iv["XTb"]
                tt("pool", N0[:], A[:, 0], bm(0), ALU.mult, [tA5, tp], [tN0])
                tt("pool", N0T[:], A[:, 1], bm(0), ALU.mult, [tA5, tp], [tN0T])
                idb = S.identb[:, :].unsqueeze(1).to_broadcast([128, 4, CH])
                tt("dve", Xc[:], idb, N0[:], ALU.subtract, [tN0, S.t_const], [tXc])
                tt("dve", XTc[:], idb, N0T[:], ALU.subtract, [tN0T, S.t_const], [tXTc])
                Pc, tPc, PTc, tPTc = N0, tN0, N0T, tN0T
                for j in range(3):
                    Pn, tPn = iv["P%d" % (j % 2)]; PTn, tPTn = iv["PT%d" % (j % 2)]
                    ps_, tps_ = mmg(PTc, tPTc, Pc, tPc)
                    k.op("act", lambda e: e.copy(out=F3(Pn), in_=ps_[:, 0:512]), reads=[tps_], writes=[tPn])
                    ps_, tps_ = mmg(Pc, tPc, PTc, tPTc)
                    k.op("dve", lambda e: e.tensor_copy(out=F3(PTn), in_=ps_[:, 0:512]), reads=[tps_], writes=[tPTn])
                    ps_, tps_ = mmg(PTn, tPTn, Xc, tXc)
                    k.op("dve", lambda e: e.tensor_tensor(out=F3(Xo), in0=ps_[:, 0:512], in1=F3(Xc), op=ALU.add), reads=[tps_, tXc], writes=[tXo])
                    ps_, tps_ = mmg(Pn, tPn, XTc, tXTc)
                    k.op("dve", lambda e: e.tensor_tensor(out=F3(XTo), in0=ps_[:, 0:512], in1=F3(XTc), op=ALU.add), reads=[tps_, tXTc], writes=[tXTo])
                    Xc, tXc, Xo, tXo = Xo, tXo, Xc, tXc
                    XTc, tXTc, XTo, tXTo = XTo, tXTo, XTc, tXTc
                    Pc, tPc, PTc, tPTc = Pn, tPn, PTn, tPTn
                yield
                for lv in range(3):
                    Np, tNp = iv["P0"]; NpT, tNpT = iv["PT0"]
                    Wt, tWt = iv["W"]; Wt2, tWt2 = iv["W2"]
                    tt("pool", Np[:], A[:, 0], bm(1 + lv), ALU.mult, [tA5, tp], [tNp])
                    tt("pool", NpT[:], A[:, 1], bm(1 + lv), ALU.mult, [tA5, tp], [tNpT])
                    ps_, tps_ = mmg(NpT, tNpT, Xc, tXc)
                    k.op("act", lambda e: e.copy(out=F3(Wt), in_=ps_[:, 0:512]), reads=[tps_], writes=[tWt])
                    if lv < 2:
                        ps_, tps_ = mmg(Np, tNp, XTc, tXTc)
                        k.op("dve", lambda e: e.tensor_copy(out=F3(Wt2), in_=ps_[:, 0:512]), reads=[tps_], writes=[tWt2])
                    ps_, tps_ = mmg(XTc, tXTc, Wt, tWt)
                    k.op("dve", lambda e: e.tensor_tensor(out=F3(Xo), in0=F3(Xc), in1=ps_[:, 0:512], op=ALU.subtract), reads=[tps_, tXc], writes=[tXo])
                    if lv < 2:
                        ps_, tps_ = mmg(Xc, tXc, Wt2, tWt2)
                        k.op("dve", lambda e: e.tensor_tensor(out=F3(XTo), in0=F3(XTc), in1=ps_[:, 0:512], op=ALU.subtract),
                             reads=[tps_, tXTc], writes=[tXTo])
                        XTc, tXTc, XTo, tXTo = XTo, tXTo, XTc, tXTc
                    Xc, tXc, Xo, tXo = Xo, tXo, Xc, tXc
                    yield
                rhs2, trh = g(rhs2_s); mu, tmu = g(mu_s)
                ps, tps = k.ps()
                for h in range(4):
                    k.op("pe", lambda e: e.matmul(ps[:, h * 64:(h + 1) * 64], A[:, 2, h, :], vtb[:, h, :], start=True, stop=True),
                         reads=[tA5, tvtb], writes=[tps])
                k.op("dve", lambda e: e.tensor_scalar(out=rhs2[:, :, 64:128], in0=ps[:, 0:256].rearrange("p (h v) -> p h v", h=4), scalar1=-1.0,
                                                      scalar2=None, op0=ALU.mult), reads=[tps], writes=[trh])
                k.op("pool", lambda e: e.tensor_copy(out=rhs2[:, :, 0:64], in_=tm[:, 0]), reads=[ttm], writes=[trh])
                ps, tps = k.ps()
                for h in range(4):
                    k.op("pe", lambda e: e.matmul(ps[:, h * 128:(h + 1) * 128], Xc[:, h, :], rhs2[:, h, :], start=True, stop=True),
                         reads=[tXc, trh], writes=[tps])
                k.op("act", lambda e: e.copy(out=mu[:].rearrange("p h t -> p (h t)"), in_=ps[:, 0:512]), reads=[tps], writes=[tmu])
                yield
                GT, tGT = g(GT_s); J, tJ = g(J_s); RT, tRT = g(RT_s)
                ps, tps = k.ps()
                for h in range(4):
                    k.op("pe", lambda e: e.matmul(ps[0:64, h * 64:(h + 1) * 64], mu[:, h, 0:64], tm[:, 2, h, :], start=True, stop=True),
                         reads=[tmu, ttm], writes=[tps])
                for h in range(4):
                    k.op("dve", lambda e: e.scalar_tensor_tensor(out=GT[:, h, :], in0=S.ident[0:64, 0:64], scalar=Ep[:, h, iend:iend + 1],
                                                                 in1=ps[0:64, h * 64:(h + 1) * 64], op0=ALU.mult, op1=ALU.subtract),
                         reads=[tps, tEp, S.t_const], writes=[tGT])
                ps, tps = k.ps()
                for h in range(4):
                    k.op("pe", lambda e: e.matmul(ps[0:64, h * 64:(h + 1) * 64], tm[:, 1, h, :], vtb[:, h, :], start=True, stop=False),
                         reads=[ttm, tvtb], writes=[tps])
                    k.op("pe", lambda e: e.matmul(ps[0:64, h * 64:(h + 1) * 64], tm[:, 2, h, :], mu[:, h, 64:128], start=False, stop=True),
                         reads=[ttm, tmu], writes=[tps])
                k.op("act", lambda e: e.copy(out=J[:].rearrange("p h v -> p (h v)"), in_=ps[0:64, 0:256]), reads=[tps], writes=[tJ])
                Hc, tHc = Hs[hcur]
                Hn, tHn = Hs[1 - hcur]
                if emit:
                    ps, tps = k.ps()
                    for h in range(4):
                        k.op("pe", lambda e: e.matmul(ps[0:64, h * CH:(h + 1) * CH], mu[:, h, 0:64], A[:, 4, h, :], start=True, stop=True),
                             reads=[tmu, tA5], writes=[tps])
                    k.op("dve", lambda e: e.tensor_tensor(out=RT[:].rearrange("p h t -> p (h t)"), in0=rq[:].rearrange("p h t -> p (h t)"),
                                                          in1=ps[0:64, 0:512], op=ALU.subtract), reads=[tps, trq], writes=[tRT])
                    py, tpy = k.ps()
                    for h in range(4):
                        k.op("pe", lambda e: e.matmul(py[:, h * 64:(h + 1) * 64], A[:, 3, h, :], vtb[:, h, :], start=True, stop=False),
                             reads=[tA5, tvtb], writes=[tpy])
                        k.op("pe", lambda e: e.matmul(py[:, h * 64:(h + 1) * 64], A[:, 4, h, :], mu[:, h, 64:128], start=False, stop=False),
                             reads=[tA5, tmu], writes=[tpy])
                        k.op("pe", lambda e: e.matmul(py[:, h * 64:(h + 1) * 64], RT[:, h, :], Hc[:, h, :], start=False, stop=True),
                             reads=[tRT, tHc], writes=[tpy])
                    rk, trk = g(rk_s)
                    tt("pool", rk[:], r_, kdir[:], ALU.mult, [trkv, tkd_], [trk])
                    tt("pool", rk[:], rk[:], B3(rkp[:, :]), ALU.mult, [tp], [trk])
                    for h in range(4):
                        k.op("pe", lambda e: e.matmul(py[:, 256 + h:257 + h], rk[:, h, :], ones64[:, 0:1], start=True, stop=True),
                             reads=[trk, tp], writes=[tpy])
                    ysb, tys = g(ys_s)
                    if d == 0:
                        k.op("act", lambda e: e.copy(out=ysb[:, 0:260], in_=py[:, 0:260]), reads=[tpy], writes=[tys])
                        k.dma(S.YD[b, t0:t0 + CH, :], ysb[:, :], reads=[tys], writes=[S.t_YD[b]])
                    else:
                        yd0, tyd0 = g(yd0_s)
                        k.dma(yd0[:], S.YD[b, t0:t0 + CH, :], reads=[S.t_YD[b]], writes=[tyd0])
                        k.op("dve", lambda e: e.tensor_tensor(out=ysb[:, 0:260], in0=py[:, 0:260], in1=yd0[:, 0:260], op=ALU.add),
                             reads=[tpy, tyd0], writes=[tys])
                yield
                ph, tph = k.ps()
                for h in range(4):
                    k.op("pe", lambda e: e.matmul(ph[0:64, h * 64:(h + 1) * 64], GT[:, h, :], Hc[:, h, :], start=True, stop=True),
                         reads=[tGT, tHc], writes=[tph])
                k.op("dve", lambda e: e.tensor_tensor(out=Hn[:].rearrange("p h v -> p (h v)"), in0=ph[0:64, 0:256],
                                                      in1=J[:].rearrange("p h v -> p (h v)"), op=ALU.add), reads=[tph, tJ], writes=[tHn])
                hcur = 1 - hcur
                if not emit:
                    continue
                yield
                ysb, tys = g(ys_s)
                if d == 0:
                    continue
                yc, tyc = g(yc_s); y2, ty2 = g(y2_s); stt, tst = g(st_s); gts, tgts = g(gt_s)
                y3 = ysb[:, 0:256].rearrange("p (h v) -> p h v", h=4)
                k.op("dve", lambda e: e.tensor_reduce(out=stt[:, 0:4], in_=y3, axis=AX.X, op=ALU.add), reads=[tys], writes=[tst])
                k.op("dve", lambda e: e.tensor_scalar(out=stt[:, 0:4], in0=stt[:, 0:4], scalar1=1.0 / 64, scalar2=None, op0=ALU.mult),
                     reads=[tst], writes=[tst])
                k.op("dve", lambda e: e.tensor_tensor(out=yc[:], in0=y3, in1=stt[:, 0:4].unsqueeze(2).to_broadcast([128, 4, 64]),
                                                      op=ALU.subtract), reads=[tys, tst], writes=[tyc])
                k.op("pool", lambda e: e.tensor_tensor(out=y2[:], in0=yc[:], in1=yc[:], op=ALU.mult), reads=[tyc], writes=[ty2])
                k.op("dve", lambda e: e.tensor_reduce(out=stt[:, 4:8], in_=y2[:], axis=AX.X, op=ALU.add), reads=[ty2], writes=[tst])
                k.op("dve", lambda e: e.tensor_scalar(out=stt[:, 4:8], in0=stt[:, 4:8], scalar1=1.0 / 64, scalar2=GN_EPS, op0=ALU.mult,
                                                      op1=ALU.add), reads=[tst], writes=[tst])
                k.op("act", lambda e: e.activation(out=stt[:, 4:8], in_=stt[:, 4:8], func=AF.Sqrt), reads=[tst], writes=[tst])
                k.op("dve", lambda e: e.reciprocal(out=stt[:, 4:8], in_=stt[:, 4:8]), reads=[tst], writes=[tst])
                k.op("dve", lambda e: e.tensor_tensor(out=yc[:], in0=yc[:], in1=stt[:, 4:8].unsqueeze(2).to_broadcast([128, 4, 64]),
                                                      op=ALU.mult), reads=[tst], writes=[tyc])
                ycf = yc[:].rearrange("p h v -> p (h v)")
                k.op("pool", lambda e: e.tensor_tensor(out=ycf, in0=ycf, in1=gnw[:], op=ALU.mult), reads=[t_gn], writes=[tyc])
                k.op("pool", lambda e: e.tensor_tensor(out=ycf, in0=ycf, in1=gnb[:], op=ALU.add), reads=[t_gn], writes=[tyc])
                yield
                k.op("dve", lambda e: e.tensor_tensor(out=y2[:], in0=vtm[:], in1=ysb[:, 256:260].unsqueeze(2).to_broadcast([128, 4, 64]),
                                                      op=ALU.mult), reads=[tvtm, tys], writes=[ty2])
                k.op("dve", lambda e: e.tensor_tensor(out=yc[:], in0=yc[:], in1=y2[:], op=ALU.add), reads=[ty2], writes=[tyc])
                k.op("act", lambda e: e.activation(out=gl[:], in_=gl[:], func=AF.Sigmoid), reads=[tgl], writes=[tgl])
                pg, tpg = k.ps()
                k.op("pe", lambda e: e.matmul(pg[:, 0:256], gl[:, :], g2[:, :], start=True, stop=True), reads=[tgl, tp], writes=[tpg])
                k.op("dve", lambda e: e.tensor_tensor(out=gts[:], in0=pg[:, 0:256], in1=ycf, op=ALU.mult), reads=[tpg, tyc], writes=[tgts])
                ot, tot = g(ot_s)
                ps, tps = k.ps()
                for c2 in range(2):
                    k.op("pe", lambda e: e.transpose(out=ps[:, c2 * CH:(c2 + 1) * CH], in_=gts[:, c2 * 128:(c2 + 1) * 128], identity=S.ident[:, :]),
                         reads=[tgts, S.t_const], writes=[tps])
                k.op("act", lambda e: e.copy(out=ot[:].rearrange("p c t -> p (c t)"), in_=ps[:, 0:256]), reads=[tps], writes=[tot])
                k.dma(S.YT[b, 0:256, t0:t0 + CH].rearrange("(c p) t -> p c t", p=128), ot[:], reads=[tot], writes=[S.t_YT[b]])
    for d in range(2):
        gens = [chain(b, d) for b in range(NB)]
        for gi_, g_ in enumerate(gens[:-1]):
            for _ in range(RWKV_STAGGER * (len(gens) - 1 - gi_)):
                next(g_)
        while gens:
            for g_ in list(gens):
                try:
                    next(g_)
                except StopIteration:
                    gens.remove(g_)
    k.barrier()
    st.close()


def stage_moe2(S, l, last):
    k, cfg = S.k, S.cfg
    NB, C, L, T = cfg.NB, cfg.C, cfg.L, cfg.T
    nc = k.nc
    st = contextlib.ExitStack()
    blocks = [(b, t0) for b in range(NB) for t0 in range(C if last else 0, T, 128)]
    NBK = len(blocks)
    Tn = NBK * 128
    NBLK = (2 * Tn + EB - 1) // EB + NEXP
    PMAX = NBLK * EB
    MAXB = (Tn + EB - 1) // EB
    R = NB + 1
    tp = Tr()
    P = lambda name, shape, dt=F32: k.sbuf(st, name, shape, dt)
    t_ln = Tr()
    lng = load_bcast_row(S, st, "lng2", S.inp["ln2_g"][l:l + 1, :], t_ln)
    lnb = load_bcast_row(S, st, "lnb2", S.inp["ln2_b"][l:l + 1, :], t_ln)
    wr = P("wr", [128, 8, 36])
    k.dma(wr[:, :, 0:4], S.inp["router_group_w"][l].rearrange("(kc p) n -> p kc n", p=128), writes=[tp])
    k.dma(wr[:, :, 4:36], S.inp["router_expert_w"][l].rearrange("(kc p) n -> p kc n", p=128), writes=[tp])
    rbias = P("rbias", [1, 36])
    k.dma(rbias[0:1, 0:4], S.inp["router_group_b"][l:l + 1, :], writes=[tp])
    k.dma(rbias[0:1, 4:36], S.inp["router_expert_b"][l:l + 1, :], writes=[tp])
    ones = P("onesm", [128, 128])
    k.op("dve", lambda e: e.memset(ones[:], 1.0), reads=[tp], writes=[tp])
    tris = P("tris", [128, 128])
    k.dma(tris[:], S.inp["msk"][:, 0:128], writes=[tp])
    iop = P("iop", [128, 1])
    k.dma(iop[:], S.inp["iotap"][:, :], writes=[tp])
    w12 = P("w12", [128, NBK, 2]); t_w12 = Tr()
    dsti = P("dsti", [128, NBK, 2], I32)
    widi = P("widi", [128, NBLK], I32)
    st1 = contextlib.ExitStack()
    P1 = lambda name, shape, dt=F32: k.sbuf(st1, name, shape, dt)
    scb = P1("scb", [128, R, D]); shb = P1("shb", [128, R, D]); dm = [(P1("dm", [128, 128]), Tr()) for _ in range(2)]
    di = 0
    for (dst, base) in ((scb, 32), (shb, 24)):
        for r in range(R):
            for half in range(2):
                ps, tps = k.ps()
                for f4 in range(4):
                    fc = half * 4 + f4
                    dmt, tdm = dm[di % 2]
                    di += 1
                    k.op("dve", lambda e: e.tensor_scalar(out=dmt[:], in0=S.ident[:, :], scalar1=S.mod[:, base + fc, r:r + 1],
                                                          scalar2=None, op0=ALU.mult), reads=[S.t_mod, S.t_const], writes=[tdm])
                    k.op("pe", lambda e: e.matmul(ps[:, f4 * 128:(f4 + 1) * 128], ones[:, :], dmt[:, :], start=True, stop=True),
                         reads=[tdm, tp], writes=[tps])
                k.op("act", lambda e: e.copy(out=dst[:, r, half * 512:(half + 1) * 512], in_=ps[:, 0:512]), reads=[tps], writes=[tp])
    OH = P1("OH", [128, NBK, 2, 32]); t_OH = Tr()
    x1s = [(P1("x1", [128, D]), Tr()) for _ in range(2)]
    hfs = [(P1("hf", [128, 8, 128]), Tr()) for _ in range(2)]
    hts = [(P1("ht", [128, D]), Tr()) for _ in range(2)]
    htb = [(k.sbuf(st1, "htb", [128, D], BF16), Tr()) for _ in range(2)]
    lga = P1("lga", [128, NBK, 36]); t_lga = Tr()
    it = 0
    for j, (b, t0) in enumerate(blocks):
        r = NB if t0 < C else b
        x1, tx1 = x1s[it % 2]; hf, thf = hfs[it % 2]; ht, tht = hts[it % 2]; hb_, thb = htb[it % 2]
        it += 1
        k.dma(x1[:], S.XR[b, t0:t0 + 128, :], reads=[S.t_XR[b]], writes=[tx1])
        k.op("pool", lambda e: e.tensor_tensor(out=ht[:], in0=x1[:], in1=scb[:, r, :], op=ALU.mult), reads=[tx1, tp], writes=[tht])
        k.op("pool", lambda e: e.tensor_tensor(out=hb_[:], in0=ht[:], in1=shb[:, r, :], op=ALU.add), reads=[tht, tp], writes=[thb])
        k.dma(S.HS[j * 128:(j + 1) * 128, :], hb_[:], reads=[thb], writes=[S.t_HS])
        for hh in range(2):
            ps, tps = k.ps()
            for f4 in range(4):
                fc = hh * 4 + f4
                k.op("pe", lambda e: e.transpose(out=ps[:, f4 * 128:(f4 + 1) * 128], in_=x1[:, fc * 128:(fc + 1) * 128],
                                                 identity=S.ident[:, :]), reads=[tx1, S.t_const], writes=[tps])
            for f4 in range(4):
                fc = hh * 4 + f4
                k.op("act", lambda e: e.activation(out=hf[:, fc, :], in_=ps[:, f4 * 128:(f4 + 1) * 128], func=AF.Identity,
                                                   scale=S.mod[:, 32 + fc, r:r + 1], bias=S.mod[:, 24 + fc, r:r + 1]),
                     reads=[tps, S.t_mod], writes=[thf])
        ps, tps = k.ps()
        for fc in range(8):
            k.op("pe", lambda e: e.matmul(ps[:, 0:36], hf[:, fc, :], wr[:, fc, :], start=(fc == 0), stop=False),
                 reads=[thf, tp], writes=[tps])
        k.op("pe", lambda e: e.matmul(ps[:, 0:36], ones[0:1, :], rbias[0:1, :], start=False, stop=True), reads=[tp], writes=[tps])
        k.op("act", lambda e: e.copy(out=lga[:, j, :], in_=ps[:, 0:36]), reads=[tps], writes=[t_lga])
    RB = lambda name, n: P1(name, [128, NBK, n])
    gmx = RB("gmx", 1); gm = RB("gm", 4); ge = RB("ge", 4); gpr = RB("gpr", 1); elm = RB("elm", 32); els = RB("els", 8)
    m1 = RB("m1", 1); mk1 = RB("mk1", 8); el2 = RB("el2", 8); m2 = RB("m2", 1); mk2 = RB("mk2", 8); dd = RB("dd", 1); w1 = RB("w1", 1)
    t_r = Tr()
    rv = lambda fn, rd=(), wrs=(), en="dve": k.op(en, fn, reads=[t_r, t_lga] + list(rd), writes=[t_r] + list(wrs))
    BC = lambda ap, n: ap.to_broadcast([128, NBK, n])
    rv(lambda e: e.tensor_reduce(out=gmx[:, :, 0], in_=lga[:, :, 0:4], axis=AX.X, op=ALU.max))
    rv(lambda e: e.tensor_tensor(out=gm[:], in0=lga[:, :, 0:4], in1=BC(gmx[:, :, 0:1], 4), op=ALU.is_equal))
    rv(lambda e: e.tensor_tensor(out=ge[:], in0=lga[:, :, 0:4], in1=BC(gmx[:, :, 0:1], 4), op=ALU.subtract))
    rv(lambda e: e.activation(out=ge[:], in_=ge[:], func=AF.Exp), en="act")
    rv(lambda e: e.tensor_reduce(out=gpr[:, :, 0], in_=ge[:], axis=AX.X, op=ALU.add))
    rv(lambda e: e.reciprocal(out=gpr[:], in_=gpr[:]))
    for g_ in range(4):
        rv(lambda e: e.tensor_tensor(out=elm[:, :, g_ * 8:(g_ + 1) * 8], in0=lga[:, :, 4 + g_ * 8:12 + g_ * 8], in1=BC(gm[:, :, g_:g_ + 1], 8),
                                     op=ALU.mult))
    rv(lambda e: e.tensor_tensor(out=els[:], in0=elm[:, :, 0:8], in1=elm[:, :, 8:16], op=ALU.add))
    rv(lambda e: e.tensor_tensor(out=els[:], in0=els[:], in1=elm[:, :, 16:24], op=ALU.add))
    rv(lambda e: e.tensor_tensor(out=els[:], in0=els[:], in1=elm[:, :, 24:32], op=ALU.add))
    rv(lambda e: e.tensor_reduce(out=m1[:, :, 0], in_=els[:], axis=AX.X, op=ALU.max))
    rv(lambda e: e.tensor_tensor(out=mk1[:], in0=els[:], in1=BC(m1[:, :, 0:1], 8), op=ALU.is_equal))
    rv(lambda e: e.scalar_tensor_tensor(out=el2[:], in0=mk1[:], scalar=-1e30, in1=els[:], op0=ALU.mult, op1=ALU.add))
    rv(lambda e: e.tensor_reduce(out=m2[:, :, 0], in_=el2[:], axis=AX.X, op=ALU.max))
    rv(lambda e: e.tensor_tensor(out=mk2[:], in0=el2[:], in1=BC(m2[:, :, 0:1], 8), op=ALU.is_equal))
    rv(lambda e: e.tensor_tensor(out=dd[:], in0=m2[:], in1=m1[:], op=ALU.subtract))
    rv(lambda e: e.activation(out=dd[:], in_=dd[:], func=AF.Exp), en="act")
    rv(lambda e: e.tensor_scalar(out=w1[:], in0=dd[:], scalar1=1.0, scalar2=None, op0=ALU.add))
    rv(lambda e: e.reciprocal(out=w1[:], in_=w1[:]))
    rv(lambda e: e.tensor_tensor(out=dd[:], in0=dd[:], in1=w1[:], op=ALU.mult))
    rv(lambda e: e.tensor_tensor(out=w12[:, :, 0:1], in0=w1[:], in1=gpr[:], op=ALU.mult), wrs=[t_w12])
    rv(lambda e: e.tensor_tensor(out=w12[:, :, 1:2], in0=dd[:], in1=gpr[:], op=ALU.mult), wrs=[t_w12])
    for kk_, mk in ((0, mk1), (1, mk2)):
        for g_ in range(4):
            rv(lambda e: e.tensor_tensor(out=OH[:, :, kk_, g_ * 8:(g_ + 1) * 8], in0=mk[:], in1=BC(gm[:, :, g_:g_ + 1], 8), op=ALU.mult),
               wrs=[t_OH])
    OHs = P1("OHs", [128, NBK, 32]); t_OHs = Tr()
    k.op("dve", lambda e: e.tensor_tensor(out=OHs[:], in0=OH[:, :, 0, :], in1=OH[:, :, 1, :], op=ALU.add), reads=[t_OH], writes=[t_OHs])
    pref = P1("pref", [128, NBK, 32]); t_pref = Tr()
    for j in range(NBK):
        ps, tps = k.ps()
        k.op("pe", lambda e: e.matmul(ps[:, 0:32], tris[:, :], OHs[:, j, :], start=True, stop=(j == 0)), reads=[t_OHs, tp], writes=[tps])
        for j2 in range(j):
            k.op("pe", lambda e: e.matmul(ps[:, 0:32], ones[:, :], OHs[:, j2, :], start=False, stop=(j2 == j - 1)),
                 reads=[t_OHs, tp], writes=[tps])
        k.op("act", lambda e: e.copy(out=pref[:, j, :], in_=ps[:, 0:32]), reads=[tps], writes=[t_pref])
    cst = P1("cst", [128, 8, 32]); t_c = Tr()
    ps, tps = k.ps()
    for j in range(NBK):
        k.op("pe", lambda e: e.matmul(ps[:, 0:32], ones[:, :], OHs[:, j, :], start=(j == 0), stop=(j == NBK - 1)),
             reads=[t_OHs, tp], writes=[tps])
    cd = lambda fn, rd=(): k.op("dve", fn, reads=[t_c] + list(rd), writes=[t_c])
    cd(lambda e: e.tensor_copy(out=cst[:, 0, :], in_=ps[:, 0:32]), rd=[tps])
    cd(lambda e: e.memset(cst[:, 1, :], 0.0))
    cd(lambda e: e.memset(cst[:, 6, :], 1.0))
    for m in range(MAXB):
        cd(lambda e: e.tensor_scalar(out=cst[:, 5, :], in0=cst[:, 0, :], scalar1=float(m * EB), scalar2=None, op0=ALU.is_gt))
        cd(lambda e: e.tensor_tensor(out=cst[:, 1, :], in0=cst[:, 1, :], in1=cst[:, 5, :], op=ALU.add))
    cd(lambda e: e.tensor_scalar(out=cst[:, 2, :], in0=cst[:, 1, :], scalar1=float(EB), scalar2=None, op0=ALU.mult))
    cd(lambda e: e.tensor_tensor_scan(out=cst[:, 3, :], data0=cst[:, 6, :], data1=cst[:, 2, :], initial=0.0, op0=ALU.mult, op1=ALU.add))
    cd(lambda e: e.tensor_tensor(out=cst[:, 4, :], in0=cst[:, 3, :], in1=cst[:, 2, :], op=ALU.subtract))
    k.op("dve", lambda e: e.tensor_tensor(out=pref[:], in0=pref[:], in1=cst[:, 4:5, :].to_broadcast([128, NBK, 32]), op=ALU.add),
         reads=[t_c], writes=[t_pref])
    dstf = P1("dstf", [128, NBK, 2]); t_d = Tr()
    tmpo = P1("tmpo", [128, NBK, 32]); t_to = Tr()
    for kk_ in range(2):
        k.op("dve", lambda e: e.tensor_tensor(out=tmpo[:], in0=OH[:, :, kk_, :], in1=pref[:], op=ALU.mult), reads=[t_OH, t_pref], writes=[t_to])
        k.op("dve", lambda e: e.tensor_reduce(out=dstf[:, :, kk_], in_=tmpo[:], axis=AX.X, op=ALU.add), reads=[t_to], writes=[t_d])
    k.op("dve", lambda e: e.tensor_copy(out=dsti[:], in_=dstf[:]), reads=[t_d], writes=[t_d])
    bexp = P1("bexp", [128, NBLK]); t_be = Tr()
    for bk in range(NBLK):
        cd(lambda e: e.tensor_scalar(out=cst[:, 5, :], in0=cst[:, 3, :], scalar1=float(bk * EB), scalar2=None, op0=ALU.is_le))
        k.op("dve", lambda e: e.reduce_sum(out=bexp[:, bk:bk + 1], in_=cst[:, 5, :], axis=AX.X), reads=[t_c], writes=[t_be])
    k.op("dve", lambda e: e.tensor_scalar(out=bexp[:], in0=bexp[:], scalar1=float(NEXP - 1), scalar2=None, op0=ALU.min), reads=[t_be], writes=[t_be])
    widf = P1("widf", [128, NBLK]); t_wi = Tr()
    k.op("dve", lambda e: e.tensor_scalar(out=widf[:], in0=bexp[:], scalar1=128.0, scalar2=float(l * NEXP * 128), op0=ALU.mult, op1=ALU.add),
         reads=[t_be], writes=[t_wi])
    k.op("dve", lambda e: e.tensor_tensor(out=widf[:], in0=widf[:], in1=iop[:, 0:1].to_broadcast([128, NBLK]), op=ALU.add),
         reads=[tp], writes=[t_wi])
    k.op("dve", lambda e: e.tensor_copy(out=widi[:], in_=widf[:]), reads=[t_wi], writes=[t_wi])
    hbs = [(k.sbuf(st1, "hb2", [128, D], BF16), Tr()) for _ in range(2)]
    for j in range(NBK):
        hb_, thb = hbs[j % 2]
        k.dma(hb_[:], S.HS[j * 128:(j + 1) * 128, :], reads=[S.t_HS], writes=[thb])
        for kk_ in range(2):
            k.idma(S.HSORT[:, :], hb_[:], out_off=dsti[:, j, kk_:kk_ + 1], bound=PMAX - 1, reads=[thb, t_d], writes=[S.t_HSORT])
    k.barrier()
    st1.close()
    if getattr(cfg, "moe_stop", 9) < 2:
        st.close()
        return
    st2 = contextlib.ExitStack()
    P2 = lambda name, shape, dt=F32: k.sbuf(st2, name, shape, dt)
    wgs = [(P2("wg", [128, 8, DE], BF16), P2("wu", [128, 8, DE], BF16), P2("wd", [128, 4, D], BF16), Tr()) for _ in range(2)]
    htk = [(P2("htk", [128, 4, D], BF16), Tr()) for _ in range(2)]
    hTs = [(P2("hT", [128, 8, EB], BF16), Tr()) for _ in range(2)]
    sgs = [(P2("sg", [128, 512]), Tr()) for _ in range(2)]
    aTs = [(P2("aT", [128, 4, 512], BF16), Tr()) for _ in range(2)]
    ybs = [(P2("yb", [128, 4, D], BF16), Tr()) for _ in range(2)]
    for bk in range(NBLK):
        wg, wu, wd, twe = wgs[bk % 2]
        bnd = DEPTH * NEXP * 128 - 1
        for (dst, nm, hc) in ((wg, "ewg", 4), (wu, "ewu", 4), (wd, "ewd", 2)):
            k.idma(dst[:, 0:hc, :].rearrange("p a b -> p (a b)"), S.inp[nm + "_a"][:, :], in_off=widi[:, bk:bk + 1], bound=bnd,
                   reads=[t_wi], writes=[twe])
            k.idma(dst[:, hc:2 * hc, :].rearrange("p a b -> p (a b)"), S.inp[nm + "_b"][:, :], in_off=widi[:, bk:bk + 1], bound=bnd,
                   reads=[t_wi], writes=[twe])
        hk, thk = htk[bk % 2]
        hT, thT = hTs[bk % 2]
        k.dma(hk[:], S.HSORT[bk * EB:(bk + 1) * EB, :].rearrange("(n p) d -> p n d", p=128), reads=[S.t_HSORT], writes=[thk])
        for fc in range(8):
            ps, tps = k.ps()
            psb = ps[:].bitcast(BF16)
            for n in range(4):
                k.op("pe", lambda e: e.transpose(out=psb[:, n * 128:(n + 1) * 128], in_=hk[:, n, fc * 128:(fc + 1) * 128],
                                                 identity=S.identb[:, :]), reads=[thk, S.t_const], writes=[tps])
            k.op("act" if fc % 2 else "dve", (lambda e: e.copy(out=hT[:, fc, :], in_=psb[:, 0:512])) if fc % 2 else
                 (lambda e: e.tensor_copy(out=hT[:, fc, :], in_=psb[:, 0:512])), reads=[tps], writes=[thT])
        aT, taT = aTs[bk % 2]
        for f in range(4):
            pg, tpg = k.ps()
            pu, tpu = k.ps()
            for kc in range(8):
                k.op("pe", lambda e: e.matmul(pg[:, 0:EB], wg[:, kc, f * 128:(f + 1) * 128], hT[:, kc, :], start=(kc == 0), stop=(kc == 7)),
                     reads=[twe, thT], writes=[tpg])
            for kc in range(8):
                k.op("pe", lambda e: e.matmul(pu[:, 0:EB], wu[:, kc, f * 128:(f + 1) * 128], hT[:, kc, :], start=(kc == 0), stop=(kc == 7)),
                     reads=[twe, thT], writes=[tpu])
            sg, tsg = sgs[f % 2]
            k.op("act", lambda e: e.activation(out=sg[:], in_=pg[:, 0:EB], func=AF.Silu), reads=[tpg], writes=[tsg])
            k.op("dve", lambda e: e.tensor_tensor(out=aT[:, f, :], in0=pu[:, 0:EB], in1=sg[:], op=ALU.mult), reads=[tpu, tsg], writes=[taT])
        yb, tyb = ybs[bk % 2]
        for jb in range(4):
            for h in range(2):
                py, tpy = k.ps()
                for f in range(4):
                    k.op("pe", lambda e: e.matmul(py[:, 0:512], aT[:, f, jb * 128:(jb + 1) * 128], wd[:, f, h * 512:(h + 1) * 512],
                                                  start=(f == 0), stop=(f == 3)), reads=[taT, twe], writes=[tpy])
                k.op("act" if h else "dve", (lambda e: e.copy(out=yb[:, jb, h * 512:(h + 1) * 512], in_=py[:, 0:512])) if h else
                     (lambda e: e.tensor_copy(out=yb[:, jb, h * 512:(h + 1) * 512], in_=py[:, 0:512])), reads=[tpy], writes=[tyb])
        k.dma(S.YB[bk * EB:(bk + 1) * EB, :].rearrange("(n p) d -> p n d", p=128), yb[:], reads=[tyb], writes=[S.t_YB])
    k.barrier()
    st2.close()
    if getattr(cfg, "moe_stop", 9) < 3:
        st.close()
        return
    NB3 = 4
    gb1 = P("gb1", [128, R, D]); t_gb1 = Tr()
    k.dma(gb1[:].rearrange("p a b -> p (a b)"), S.GBD[:, R * D:2 * R * D], reads=[S.t_GBD], writes=[t_gb1])
    x1s = [(P("x1c", [128, D]), Tr()) for _ in range(NB3)]
    g1s = [(k.sbuf(st, "g1", [128, D], BF16), k.sbuf(st, "g2_", [128, D], BF16), Tr()) for _ in range(NB3)]
    zs = [(P("z2", [128, D]), Tr()) for _ in range(NB3)]
    sms = [(P("sm2", [128, 32]), Tr()) for _ in range(NB3)]
    for j, (b, t0) in enumerate(blocks):
        r = NB if t0 < C else b
        x1, tx1 = x1s[j % NB3]; ga, gb, tg = g1s[j % NB3]; z, tz = zs[j % NB3]; sm, tsm = sms[j % NB3]
        k.dma(x1[:], S.XR[b, t0:t0 + 128, :], reads=[S.t_XR[b]], writes=[tx1])
        k.idma(ga[:], S.YB[:, :], in_off=dsti[:, j, 0:1], bound=PMAX - 1, reads=[S.t_YB, t_d], writes=[tg])
        k.idma(gb[:], S.YB[:, :], in_off=dsti[:, j, 1:2], bound=PMAX - 1, reads=[S.t_YB, t_d], writes=[tg])
        k.op("dve", lambda e: e.tensor_scalar(out=z[:], in0=ga[:], scalar1=w12[:, j, 0:1], scalar2=None, op0=ALU.mult),
             reads=[tg, t_w12], writes=[tz])
        k.op("dve", lambda e: e.scalar_tensor_tensor(out=z[:], in0=gb[:], scalar=w12[:, j, 1:2], in1=z[:], op0=ALU.mult, op1=ALU.add),
             reads=[tg, t_w12], writes=[tz])
        k.op("pool", lambda e: e.tensor_tensor(out=z[:], in0=z[:], in1=gb1[:, r, :], op=ALU.mult), reads=[t_gb1], writes=[tz])
        k.op("dve", lambda e: e.scalar_tensor_tensor(out=z[:], in0=x1[:], scalar=ALPHA, in1=z[:], op0=ALU.mult, op1=ALU.add),
             reads=[tx1], writes=[tz])
        ln_block(S, z, tz, lng, lnb, t_ln, sm, tsm)
        if last:
            k.dma(S.out[b, t0 - C:t0 - C + 128, :], z[:], reads=[tz], writes=[S.t_out])
        else:
            k.dma(S.XR[b, t0:t0 + 128, :], z[:], reads=[tz], writes=[S.t_XR[b]])
    k.barrier()
    st.close()
```

```python
import contextlib
import math
import numpy as np
import concourse.bass as bass
import concourse.mybir as mybir
from concourse.bass_utils import run_bass_kernel_spmd

F32 = mybir.dt.float32
BF16 = mybir.dt.bfloat16
AF = mybir.ActivationFunctionType
ALU = mybir.AluOpType
AX = mybir.AxisListType

D = 1024
DEPTH = 2
HD = 64
RW = 256
NH = 4
S5W = 256
S5G = 16
S5P = 64
S5C = 16
AW = 512
AH = 8
AKV = 2
AG = 4
DIN = 2048
NEXP = 32
DE = 512
ALPHA = (2.0 * DEPTH) ** 0.25
LN_EPS = 1e-5
GN_EPS = 64e-5
GRID_W = 64
SEM_LIMIT = 20000
RWKV_STAGGER = 0
I32 = mybir.dt.int32
EB = 512


class Tr:
    __slots__ = ("w", "r")

    def __init__(self):
        self.w = None
        self.r = {}


class KB:
    def __init__(self, nc):
        self.nc = nc
        self.es = contextlib.ExitStack()
        self.eng = {"pe": nc.tensor, "dve": nc.vector, "act": nc.scalar, "pool": nc.gpsimd, "sp": nc.sync}
        self.sems = {}
        self.cnt = {}
        self.phase = {k: 0 for k in self.eng}
        self.waited = {k: {} for k in self.eng}
        self.ndq = 8
        self.dq_next = {}
        self.n_inst = 0
        self._uid = 0
        self.psring = []
        self.nw = {}
        self.bregs = {}
        self.ps_i = 0

    def sbuf(self, st, name, shape, dt=F32):
        self._uid += 1
        return st.enter_context(self.nc.sbuf_tensor("%s_%d" % (name, self._uid), list(shape), dt))

    def dram(self, name, shape, dt=F32, kind="Internal"):
        return self.nc.dram_tensor(name, list(shape), dt, kind=kind).ap()

    def init_psum(self):
        for i in range(8):
            t = self.es.enter_context(self.nc.psum_tensor("psr%d" % i, [128, 512], F32))
            self.psring.append((t, Tr()))

    def ps(self):
        t = self.psring[self.ps_i]
        self.ps_i = (self.ps_i + 1) % 8
        return t

    def _sem(self, key):
        if key not in self.sems:
            self._uid += 1
            self.sems[key] = self.es.enter_context(self.nc.semaphore("s%d" % self._uid))
            self.cnt[key] = 0
        return self.sems[key]

    def _cur_key(self, e):
        key = (e, self.phase[e])
        self._sem(key)
        if self.cnt[key] >= SEM_LIMIT:
            self.phase[e] += 1
            key = (e, self.phase[e])
            self._sem(key)
        return key

    def _wait(self, e, deps):
        best = {}
        for d in deps:
            if d is None:
                continue
            key, n = d
            if best.get(key, 0) < n:
                best[key] = n
        for key, n in best.items():
            if key[0] == e and e == "pe":
                continue
            if self.waited[e].get(key, 0) >= n:
                continue
            self.eng[e].wait_ge(self._sem(key), n)
            self.waited[e][key] = n

    @staticmethod
    def _deps(reads, writes):
        deps = []
        for t in reads:
            deps.append(t.w)
        for t in writes:
            deps.append(t.w)
            for kk, n in t.r.items():
                deps.append((kk, n))
        return deps

    @staticmethod
    def _mark(reads, writes, me):
        key, n = me
        for t in reads:
            t.r[key] = n
        for t in writes:
            t.w = me
            t.r = {}

    def op(self, e, fn, reads=(), writes=()):
        self._wait(e, self._deps(reads, writes))
        key = self._cur_key(e)
        ins = fn(self.eng[e])
        self.nw[e] = 0
        ins.then_inc(self.sems[key], 1)
        self.cnt[key] += 1
        me = (key, self.cnt[key])
        self._mark(reads, writes, me)
        self.n_inst += 1
        return me

    def dma(self, out, in_, reads=(), writes=(), q="sp", **kw):
        i = self.dq_next.get(q, 0)
        self.dq_next[q] = (i + 1) % self.ndq
        key = ("dq" + q, i)
        self._sem(key)
        deps = self._deps(reads, writes)
        if self.cnt[key] > 0:
            deps.append((key, self.cnt[key]))
        self._wait(q, deps)
        ins = self.eng[q].dma_start(out=out, in_=in_, **kw)
        self.nw[q] = 0
        ins.then_inc(self.sems[key], 16)
        self.cnt[key] += 16
        me = (key, self.cnt[key])
        self._mark(reads, writes, me)
        self.n_inst += 1
        return me

    def idma(self, out, in_, in_off=None, out_off=None, bound=0, reads=(), writes=()):
        q = "pool"
        i = self.dq_next.get("ind", 0)
        self.dq_next["ind"] = (i + 1) % self.ndq
        key = ("dqind", i)
        self._sem(key)
        deps = self._deps(reads, writes)
        if self.cnt[key] > 0:
            deps.append((key, self.cnt[key]))
        self._wait(q, deps)
        if bound not in self.bregs:
            rg = self.nc.gpsimd.alloc_register("bnd%d" % len(self.bregs))
            self.nc.gpsimd.reg_mov(rg, int(bound))
            self.bregs[bound] = rg
        ins = self.nc.gpsimd.indirect_dma_start(
            out=out, out_offset=(bass.IndirectOffsetOnAxis(ap=out_off, axis=0) if out_off is not None else None),
            in_=in_, in_offset=(bass.IndirectOffsetOnAxis(ap=in_off, axis=0) if in_off is not None else None),
            bounds_check=self.bregs[bound], oob_is_err=False)
        self.nw[q] = 0
        ins.then_inc(self.sems[key], 16)
        self.cnt[key] += 16
        me = (key, self.cnt[key])
        self._mark(reads, writes, me)
        self.n_inst += 1
        return me

    def barrier(self):
        allk = [(key, c) for key, c in self.cnt.items() if c > 0]
        for e in self.eng:
            self._wait(e, allk)

    def close(self):
        self.es.close()


class Cfg:
    def __init__(self, NB=2, C=256, L=2048, layers=(0, 1), dbg=False, stages=None):
        self.NB, self.C, self.L = NB, C, L
        self.T = C + L
        self.layers = layers
        self.dbg = dbg
        self.stages = stages


def tiles(t0, t1, w):
    out = []
    t = t0
    while t < t1:
        ww = min(w, t1 - t)
        out.append((t, ww))
        t += ww
    return out


def host_consts(cfg):
    L = cfg.L
    c = {}
    c["ident"] = np.eye(128, dtype=np.float32)
    rows = L // GRID_W
    rid, cid = np.meshgrid(np.arange(rows, dtype=np.float32), np.arange(GRID_W, dtype=np.float32), indexing="ij")
    nf = HD // 4
    inv = (10000.0 ** (-np.arange(nf, dtype=np.float32) / nf)).astype(np.float32)
    ang = np.concatenate([rid.reshape(-1, 1) * inv, cid.reshape(-1, 1) * inv], axis=-1).astype(np.float32)
    cos = np.cos(ang).astype(np.float32).T
    sin = np.sin(ang).astype(np.float32).T
    c["rope_cos"] = np.ascontiguousarray(np.concatenate([cos, cos, cos, cos], axis=0))
    c["rope_sin"] = np.ascontiguousarray(np.concatenate([-sin, sin, -sin, sin], axis=0))
    sel = np.zeros((3, 3, 128), np.float32)
    for r in range(3):
        sel[r, r, :] = 1.0
    c["sel3"] = sel.reshape(3, 3 * 128)
    s = np.arange(128)[:, None]
    t = np.arange(128)[None, :]
    cw = -math.exp(-0.5)
    c["tri"] = np.stack([
        np.where(s <= t, cw, 0.0), np.where(s < t, cw, 0.0),
        np.where(s >= t, cw, 0.0), np.where(s > t, cw, 0.0),
    ]).astype(np.float32).transpose(1, 0, 2).reshape(128, 4 * 128)
    c["msk"] = np.stack([s < t, s <= t, s > t, s >= t]).astype(np.float32).transpose(1, 0, 2).reshape(128, 4 * 128)
    c["iotap"] = np.arange(128, dtype=np.float32).reshape(128, 1)
    c["blkoff"] = np.ascontiguousarray(np.broadcast_to((np.arange(128, dtype=np.float32) * EB)[None, :], (128, 128)))
    bi = np.arange(128)
    bmk = lambda B: (bi[:, None] // B == bi[None, :] // B).astype(np.float32)
    c["bmsk"] = np.stack([bmk(16), bmk(32) - bmk(16), bmk(64) - bmk(32), bmk(128) - bmk(64)]).transpose(1, 0, 2).reshape(128, 512)
    return c


CONST_SHAPES = lambda cfg: {
    "ident": [128, 128], "rope_cos": [128, cfg.L], "rope_sin": [128, cfg.L], "sel3": [3, 384],
    "tri": [128, 512], "msk": [128, 512], "bmsk": [128, 512], "iotap": [128, 1], "blkoff": [128, 128],
}

PARAM_SHAPES = {
    "w_mod": [DEPTH, D, 6 * D], "b_mod": [DEPTH, 6 * D], "w_in": [DEPTH, D, DIN], "rwkv_conv": [DEPTH, 3, 1024],
    "rwkv_w0": [DEPTH, 2, RW], "rwkv_w2": [DEPTH, 2, 64, RW], "rwkv_a0": [DEPTH, 2, RW], "rwkv_a2": [DEPTH, 2, 64, RW],
    "rwkv_g2": [DEPTH, 128, RW], "rwkv_k_k": [DEPTH, RW], "rwkv_k_a": [DEPTH, RW], "rwkv_r_k": [DEPTH, NH, HD],
    "rwkv_gn_w": [DEPTH, RW], "rwkv_gn_b": [DEPTH, RW],
    "s5_lam_re": [DEPTH, 2, S5G, S5P], "s5_lam_im": [DEPTH, 2, S5G, S5P], "s5_log_dt": [DEPTH, 2, S5G],
    "s5_b_re": [DEPTH, S5G, S5P, S5C], "s5_b_im": [DEPTH, S5G, S5P, S5C], "s5_c_re": [DEPTH, S5G, S5C, S5P],
    "s5_c_im": [DEPTH, S5G, S5C, S5P], "s5_d": [DEPTH, S5W], "s5_glu_w": [DEPTH, S5W, S5W], "s5_glu_b": [DEPTH, S5W],
    "attn_sink": [DEPTH, AH], "w_out": [DEPTH, D, D], "ln1_g": [DEPTH, D], "ln1_b": [DEPTH, D], "ln2_g": [DEPTH, D],
    "ln2_b": [DEPTH, D], "router_group_w": [DEPTH, D, 4], "router_group_b": [DEPTH, 4],
    "router_expert_w": [DEPTH, D, NEXP], "router_expert_b": [DEPTH, NEXP],
    "ewg_a": [DEPTH * NEXP * 128, 2048], "ewg_b": [DEPTH * NEXP * 128, 2048], "ewu_a": [DEPTH * NEXP * 128, 2048],
    "ewu_b": [DEPTH * NEXP * 128, 2048], "ewd_a": [DEPTH * NEXP * 128, 2048], "ewd_b": [DEPTH * NEXP * 128, 2048],
}


class State:
    pass


def R3(ap, pat, **kw):
    return ap.rearrange(pat, **kw)


def stage_mod(S, l):
    k, cfg = S.k, S.cfg
    R = cfg.NB + 1
    st = contextlib.ExitStack()
    cc = k.sbuf(st, "cc", [R, D]); t_cc = Tr()
    k.dma(cc[:], S.inp["cc"][:, :], writes=[t_cc])
    k.op("act", lambda e: e.activation(out=cc[:], in_=cc[:], func=AF.Silu), reads=[t_cc], writes=[t_cc])
    siluT = k.sbuf(st, "siluT", [128, 8, R]); t_sT = Tr()
    ps, tps = k.ps()
    for kc in range(8):
        k.op("pe", lambda e: e.transpose(out=ps[:, kc * R:(kc + 1) * R], in_=cc[:, kc * 128:(kc + 1) * 128],
                                         identity=S.ident[0:R, 0:R]), reads=[t_cc, S.t_const], writes=[tps])
    k.op("dve", lambda e: e.tensor_copy(out=siluT[:].rearrange("p a b -> p (a b)"), in_=ps[:, 0:8 * R]),
         reads=[tps], writes=[t_sT])
    bmod = k.sbuf(st, "bmod", [1, 6 * D]); t_bm = Tr()
    k.dma(bmod[:], S.inp["b_mod"][l:l + 1, :], writes=[t_bm])
    ones = k.sbuf(st, "ones", [1, 128]); t_on = Tr()
    k.op("dve", lambda e: e.memset(ones[:], 1.0), writes=[t_on])
    gaterow = k.sbuf(st, "gaterow", [R, 2, D]); t_gr = Tr()
    wms = [(k.sbuf(st, "wm", [128, 8, 1024]), Tr()) for _ in range(2)]
    psm, tpsm = k.ps()
    wsrc = S.inp["w_mod"]
    for g in range(6):
        wm, twm = wms[g % 2]
        for kc in range(8):
            k.dma(wm[:, kc, :], wsrc[l, kc * 128:(kc + 1) * 128, g * 1024:(g + 1) * 1024], writes=[twm],
                  q=("sp" if kc % 2 == 0 else "act"))
        for oc in range(8):
            col = (g * 8 + oc) * R
            for kc in range(8):
                k.op("pe", lambda e: e.matmul(psm[:, col:col + R], wm[:, kc, oc * 128:(oc + 1) * 128], siluT[:, kc, :],
                                              start=(kc == 0), stop=False), reads=[twm, t_sT], writes=[tpsm])
            k.op("pe", lambda e: e.matmul(psm[:, col:col + R], bmod[0:1, (g * 8 + oc) * 128:(g * 8 + oc + 1) * 128],
                                          ones[0:1, 0:R], start=False, stop=True), reads=[t_bm, t_on], writes=[tpsm])
        if g in (2, 5):
            gi = 0 if g == 2 else 1
            for half in range(2):
                pg, tpg = k.ps()
                for kc in range(8):
                    k.op("pe", lambda e: e.matmul(pg[0:R, 0:512], siluT[:, kc, :], wm[:, kc, half * 512:(half + 1) * 512],
                                                  start=(kc == 0), stop=False), reads=[twm, t_sT], writes=[tpg])
                k.op("pe", lambda e: e.matmul(pg[0:R, 0:512], ones[0:1, 0:R],
                                              bmod[0:1, g * 1024 + half * 512:g * 1024 + (half + 1) * 512],
                                              start=False, stop=True), reads=[t_bm, t_on], writes=[tpg])
                k.op("act", lambda e: e.copy(out=gaterow[:, gi, half * 512:(half + 1) * 512], in_=pg[0:R, 0:512]),
                     reads=[tpg], writes=[t_gr])
    k.op("dve", lambda e: e.tensor_copy(out=S.mod[:].rearrange("p a b -> p (a b)"), in_=psm[:, 0:48 * R]),
         reads=[tpsm], writes=[S.t_mod])
    for base in (8, 32):
        k.op("dve", lambda e: e.tensor_scalar_add(out=S.mod[:, base:base + 8, :], in0=S.mod[:, base:base + 8, :],
                                                  scalar1=1.0), reads=[S.t_mod], writes=[S.t_mod])
    for gi in range(2):
        for r in range(R):
            for half in range(2):
                pg, tpg = k.ps()
                k.op("pe", lambda e: e.matmul(pg[:, 0:512], S.sel3[0:R, r * 128:(r + 1) * 128],
                                              gaterow[0:R, gi, half * 512:(half + 1) * 512], start=True, stop=True),
                     reads=[t_gr, S.t_const], writes=[tpg])
                k.op("act", lambda e: e.copy(out=S.gateb[:, gi, r, half * 512:(half + 1) * 512], in_=pg[:, 0:512]),
                     reads=[tpg], writes=[S.t_gateb])
    k.barrier()
    st.close()


def stage_inproj(S, l):
    k, cfg = S.k, S.cfg
    NB, C, L, T = cfg.NB, cfg.C, cfg.L, cfg.T
    st = contextlib.ExitStack()
    w = k.sbuf(st, "win", [128, 8, DIN], BF16); tw = Tr()
    ws = k.sbuf(st, "wsw", [128, 8, 640], BF16); tws = Tr()
    win = S.inp["w_in"]
    for kc in range(8):
        k.dma(w[:, kc, :], win[l, kc * 128:(kc + 1) * 128, :], writes=[tw], q="pool")
        src = win[l, kc * 128:(kc + 1) * 128, 1280:1920].rearrange("p (h two j) -> p h two j", two=2, j=32)
        dst = ws[:, kc, :].rearrange("p (h two j) -> p h two j", two=2, j=32)
        for half in range(2):
            k.dma(dst[:, :, 1 - half, :], src[:, :, half, :], writes=[tws], q="pool")
    cosT = k.sbuf(st, "cosT", [128, L]); sinT = k.sbuf(st, "sinT", [128, L]); t_rope = Tr()
    k.dma(cosT[:], S.inp["rope_cos"][:, :], writes=[t_rope])
    k.dma(sinT[:], S.inp["rope_sin"][:, :], writes=[t_rope])
    xins = [[(k.sbuf(st, "xin", [128, D]), Tr()) for _ in range(4)] for _ in range(2)]
    xms = [(k.sbuf(st, "xm", [128, 8, 512], BF16), Tr()) for _ in range(2)]
    stg = [(k.sbuf(st, "stg", [128, 512]), Tr()) for _ in range(4)]
    stgb = [(k.sbuf(st, "stgb", [128, 512], BF16), Tr()) for _ in range(4)]
    tmpa = [(k.sbuf(st, "tmpa", [128, 512]), Tr()) for _ in range(2)]
    tmpb = [(k.sbuf(st, "tmpb", [128, 512]), Tr()) for _ in range(2)]
    it = 0
    si = 0
    for b in range(NB):
        for (t0, wd) in tiles(0, C, 512) + tiles(C, T, 512):
            is_ctx = t0 < C
            r = NB if is_ctx else b
            tl = t0 - C
            nb = wd // 128
            xin = xins[it % 2]
            xm, txm = xms[it % 2]
            it += 1
            for tb in range(nb):
                k.dma(xin[tb][0][:], S.XR[b, t0 + tb * 128:t0 + (tb + 1) * 128, :], reads=[S.t_XR[b]], writes=[xin[tb][1]],
                      q=("sp" if tb % 2 == 0 else "act"))
            for fc in range(8):
                ps, tps = k.ps()
                for tb in range(nb):
                    k.op("pe", lambda e: e.transpose(out=ps[:, tb * 128:(tb + 1) * 128],
                                                     in_=xin[tb][0][:, fc * 128:(fc + 1) * 128], identity=S.ident[:, :]),
                         reads=[xin[tb][1], S.t_const], writes=[tps])
                k.op("act", lambda e: e.activation(out=xm[:, fc, 0:wd], in_=ps[:, 0:wd], func=AF.Identity,
                                                   scale=S.mod[:, 8 + fc, r:r + 1], bias=S.mod[:, fc, r:r + 1]),
                     reads=[tps, S.t_mod], writes=[txm])

            def proj(wt, twt, c0):
                ps, tps = k.ps()
                for kc in range(8):
                    k.op("pe", lambda e: e.matmul(ps[:, 0:wd], wt[:, kc, c0:c0 + 128], xm[:, kc, 0:wd],
                                                  start=(kc == 0), stop=(kc == 7)), reads=[twt, txm], writes=[tps])
                return ps, tps

            for oc in range(10):
                ps, tps = proj(w, tw, oc * 128)
                sg, tsg = stg[si % 4]
                si += 1
                if oc % 2 == 0:
                    k.op("dve", lambda e: e.tensor_copy(out=sg[:, 0:wd], in_=ps[:, 0:wd]), reads=[tps], writes=[tsg])
                else:
                    k.op("act", lambda e: e.copy(out=sg[:, 0:wd], in_=ps[:, 0:wd]), reads=[tps], writes=[tsg])
                if oc < 8:
                    k.dma(S.PR[b, oc * 128:(oc + 1) * 128, t0:t0 + wd], sg[:, 0:wd], reads=[tsg], writes=[S.t_PR[b]])
                else:
                    k.dma(S.PS[b, (oc - 8) * 128:(oc - 7) * 128, t0:t0 + wd], sg[:, 0:wd], reads=[tsg], writes=[S.t_PS[b]])
            for qc in range(5):
                ps, tps = proj(w, tw, 1280 + qc * 128)
                sb_, tsb = stgb[si % 4]
                si += 1
                if is_ctx:
                    k.op("act", lambda e: e.copy(out=sb_[:, 0:wd], in_=ps[:, 0:wd]), reads=[tps], writes=[tsb])
                else:
                    ps2, tps2 = proj(ws, tws, qc * 128)
                    ta, tta = tmpa[si % 2]
                    tb_, ttb = tmpb[si % 2]
                    k.op("dve", lambda e: e.tensor_tensor(out=ta[:, 0:wd], in0=ps[:, 0:wd], in1=cosT[:, tl:tl + wd],
                                                          op=ALU.mult), reads=[tps, t_rope], writes=[tta])
                    k.op("dve", lambda e: e.tensor_tensor(out=tb_[:, 0:wd], in0=ps2[:, 0:wd], in1=sinT[:, tl:tl + wd],
                                                          op=ALU.mult), reads=[tps2, t_rope], writes=[ttb])
                    k.op("pool", lambda e: e.tensor_tensor(out=sb_[:, 0:wd], in0=ta[:, 0:wd], in1=tb_[:, 0:wd],
                                                           op=ALU.add), reads=[tta, ttb], writes=[tsb])
                if qc < 4:
                    k.dma(S.QT[b, qc * 128:(qc + 1) * 128, t0:t0 + wd], sb_[:, 0:wd], reads=[tsb], writes=[S.t_QT[b]])
                else:
                    k.dma(S.KT[b, :, t0:t0 + wd], sb_[:, 0:wd], reads=[tsb], writes=[S.t_KT[b]])
            for tb in range(nb):
                ps, tps = k.ps()
                for kc in range(8):
                    k.op("pe", lambda e: e.matmul(ps[:, 0:128], xm[:, kc, tb * 128:(tb + 1) * 128], w[:, kc, 1920:2048],
                                                  start=(kc == 0), stop=(kc == 7)), reads=[tw, txm], writes=[tps])
                sb_, tsb = stgb[si % 4]
                si += 1
                k.op("act", lambda e: e.copy(out=sb_[:, 0:128], in_=ps[:, 0:128]), reads=[tps], writes=[tsb])
                k.dma(S.V[b, t0 + tb * 128:t0 + (tb + 1) * 128, :], sb_[:, 0:128], reads=[tsb], writes=[S.t_V[b]])
    k.barrier()
    st.close()


def build(cfg):
    nc = bass.Bass("TRN2", target_bir_lowering=False)
    k = KB(nc)
    S = State()
    S.k, S.cfg = k, cfg
    NB, C, L, T = cfg.NB, cfg.C, cfg.L, cfg.T
    S.inp = {}
    for name, shp in PARAM_SHAPES.items():
        S.inp[name] = nc.dram_tensor(name, shp, F32, kind="ExternalInput").ap()
    for name, shp in CONST_SHAPES(cfg).items():
        S.inp[name] = nc.dram_tensor(name, shp, F32, kind="ExternalInput").ap()
    S.inp["cc"] = nc.dram_tensor("cc", [NB + 1, D], F32, kind="ExternalInput").ap()
    S.inp["xall"] = nc.dram_tensor("xall", [NB, T, D], F32, kind="ExternalInput").ap()
    S.out = nc.dram_tensor("out", [NB, L, D], F32, kind="ExternalOutput").ap()
    S.t_out = Tr()
    skind = "ExternalOutput" if cfg.dbg else "Internal"
    S.XR = k.dram("XR", [NB, T, D], F32, skind)
    S.PR = k.dram("PR", [NB, 1024, T], F32, skind)
    S.PS = k.dram("PS", [NB, 256, T], F32, skind)
    S.QT = k.dram("QT", [NB, 512, T], BF16, skind)
    S.KT = k.dram("KT", [NB, 128, T], BF16, skind)
    S.V = k.dram("V", [NB, T, 128], BF16, skind)
    S.YT = k.dram("YT", [NB, 1024, T], BF16, skind)
    S.PC = k.dram("PC", [NB, 1024, T], F32, skind)
    S.YD = k.dram("YD", [NB, T, 260], F32, skind)
    S.YS = k.dram("YS", [NB, 2, 256, T], F32, skind)
    _tn = NB * T
    _pmax = ((2 * _tn + EB - 1) // EB + NEXP) * EB
    S.HS = k.dram("HS", [_tn, D], BF16, skind); S.t_HS = Tr()
    S.HSORT = k.dram("HSORT", [_pmax, D], BF16, skind); S.t_HSORT = Tr()
    S.YB = k.dram("YB", [_pmax, D], BF16, skind); S.t_YB = Tr()
    for nm in ("XR", "PR", "PS", "QT", "KT", "V", "YT", "YD", "YS", "PC", "YTa", "YTs"):
        setattr(S, "t_" + nm, [Tr() for _ in range(NB)])
    k.init_psum()
    es = k.es
    S.t_const = Tr()
    S.ident = k.sbuf(es, "ident", [128, 128])
    S.identb = k.sbuf(es, "identb", [128, 128], BF16)
    S.sel3 = k.sbuf(es, "sel3", [3, 384])
    k.dma(S.ident[:], S.inp["ident"][:, :], writes=[S.t_const])
    k.dma(S.identb[:], S.inp["ident"][:, :], writes=[S.t_const], q="pool")
    k.dma(S.sel3[:], S.inp["sel3"][:, :], writes=[S.t_const])
    S.mod = k.sbuf(es, "mod", [128, 48, NB + 1]); S.t_mod = Tr()
    S.gateb = k.sbuf(es, "gateb", [128, 2, NB + 1, D]); S.t_gateb = Tr()
    for b in range(NB):
        for (t0, wd) in tiles(0, T, 512):
            k.dma(S.XR[b, t0:t0 + wd, :], S.inp["xall"][b, t0:t0 + wd, :], writes=[S.t_XR[b]])
    if getattr(cfg, "inject_yt", False):
        ytin = nc.dram_tensor("ytin", [NB, 1024, T], BF16, kind="ExternalInput").ap()
        for b in range(NB):
            k.dma(S.YT[b], ytin[b], writes=[S.t_YT[b]])
    zst = contextlib.ExitStack()
    if stages_has_moe(cfg):
        zt = k.sbuf(zst, "zt", [128, 4, D], BF16); tzt = Tr()
        k.op("pool", lambda e: e.memset(zt[:], 0.0), writes=[tzt])
        for r0 in range(0, _pmax, 512):
            k.dma(S.HSORT[r0:r0 + 512, :].rearrange("(n p) d -> p n d", p=128), zt[:], reads=[tzt], writes=[S.t_HSORT],
                  q=("sp" if (r0 // 512) % 2 == 0 else "act"))
    k.barrier()
    zst.close()
    stages = cfg.stages
    for l in cfg.layers:
        last = (l == DEPTH - 1)
        if stages is None or "mod" in stages:
            stage_mod(S, l)
        if stages is None or "inproj" in stages:
            stage_inproj(S, l)
        if stages is None or "rwkv" in stages:
            stage_rwkv(S, l, last)
        if stages is None or ("s5" in stages and "attn" in stages):
            st_a = contextlib.ExitStack()
            ag = attn_stream(S, l, last, st_a)
            next(ag)
            stage_s5(S, l, last, side=ag)
            for _ in ag:
                pass
            k.barrier()
            st_a.close()
        else:
            if "s5" in stages:
                stage_s5(S, l, last)
            if "attn" in stages:
                stage_attn(S, l, last)
        if stages is None or "outln" in stages:
            stage_outln(S, l, last)
        if stages is None or "moe" in stages:
            stage_moe2(S, l, last)
    k.barrier()
    k.close()
    return nc


_EXPERT_CACHE = {}


def relayout_experts(inputs):
    key = id(inputs["expert_w_gate"])
    if key in _EXPERT_CACHE:
        return _EXPERT_CACHE[key]
    out = {}
    for nm, src, nchunk, width in (("ewg", "expert_w_gate", 8, DE), ("ewu", "expert_w_up", 8, DE), ("ewd", "expert_w_down", 4, D)):
        w = np.asarray(inputs[src], dtype=np.float32).reshape(DEPTH, NEXP, nchunk, 128, width)
        w = w.transpose(0, 1, 3, 2, 4)
        h = nchunk // 2
        out[nm + "_a"] = np.ascontiguousarray(w[:, :, :, :h, :]).reshape(DEPTH * NEXP * 128, 2048)
        out[nm + "_b"] = np.ascontiguousarray(w[:, :, :, h:, :]).reshape(DEPTH * NEXP * 128, 2048)
    _EXPERT_CACHE.clear()
    _EXPERT_CACHE[key] = out
    return out


def stages_has_moe(cfg):
    return (cfg.stages is None or "moe" in cfg.stages) and getattr(cfg, "sparse", True)


def make_in_maps(cfg, inputs, n_cores):
    consts = host_consts(cfg)
    NB = cfg.NB
    maps = []
    for ci in range(n_cores):
        m = {}
        for name in PARAM_SHAPES:
            if name.startswith("ew"):
                continue
            m[name] = np.ascontiguousarray(inputs[name], dtype=np.float32).reshape(PARAM_SHAPES[name])
        m.update(relayout_experts(inputs))
        m.update(consts)
        bs = slice(ci * NB, (ci + 1) * NB)
        m["cc"] = np.ascontiguousarray(np.concatenate([inputs["c"][bs], inputs["c_ctx"][None, :]], axis=0), dtype=np.float32)
        m["xall"] = np.ascontiguousarray(np.concatenate([inputs["ctx"][bs], inputs["x"][bs]], axis=1), dtype=np.float32)
        maps.append(m)
    return maps


def kernel(**inputs):
    cfg = Cfg()
    n = 8
    nc = build(cfg)
    maps = make_in_maps(cfg, inputs, n)
    res = run_bass_kernel_spmd(nc, maps, core_ids=list(range(n)))
    return np.concatenate([np.asarray(r["out"], dtype=np.float32) for r in res.results], axis=0)


def stage_attn(S, l, last):
    st = contextlib.ExitStack()
    for _ in attn_stream(S, l, last, st):
        pass
    S.k.barrier()
    st.close()


def attn_stream(S, l, last, st):
    k, cfg = S.k, S.cfg
    NB, C, L, T = cfg.NB, cfg.C, cfg.L, cfg.T
    ncb = C // 128
    nkb = T // 128
    mskb = k.sbuf(st, "mskb", [128, 4, 128], BF16); t_msk = Tr()
    k.dma(mskb[:].rearrange("p a b -> p (a b)"), S.inp["msk"][:, :], writes=[t_msk], q="pool")
    esk = k.sbuf(st, "esk", [128, AH]); t_esk = Tr()
    k.dma(esk[64:65, :], S.inp["attn_sink"][l:l + 1, :], writes=[t_esk])
    k.op("act", lambda e: e.activation(out=esk[64:65, :], in_=esk[64:65, :], func=AF.Exp), reads=[t_esk], writes=[t_esk])
    onesr = k.sbuf(st, "onesr", [128, 64]); t_on = Tr()
    k.op("dve", lambda e: e.memset(onesr[:], 1.0), writes=[t_on])
    kT = k.sbuf(st, "kT", [64, T], BF16); t_kT = Tr()
    qTs = [(k.sbuf(st, "qT", [64, AG, 128], BF16), Tr()) for _ in range(2)]
    va = k.sbuf(st, "va", [128, nkb, 65], BF16); t_va = Tr()
    pts = [(k.sbuf(st, "pt", [128, 512], BF16), Tr()) for _ in range(3)]
    dens = [(k.sbuf(st, "den", [128, 512]), Tr()) for _ in range(1)]
    rbs = [(k.sbuf(st, "rb", [64, 512]), Tr()) for _ in range(1)]
    obs = [(k.sbuf(st, "ob", [64, 512], BF16), Tr()) for _ in range(2)]
    pi = 0
    oi = 0
    yield
    for b in range(NB):
        for kv in range(AKV):
            k.dma(kT[:], S.KT[b, kv * 64:(kv + 1) * 64, :], reads=[S.t_KT[b]], writes=[t_kT])
            k.op("dve", lambda e: e.memset(va[:, :, 64:65], 1.0), writes=[t_va])
            k.dma(va[:, :, 0:64], S.V[b, :, kv * 64:(kv + 1) * 64].rearrange("(n p) d -> p n d", p=128), reads=[S.t_V[b]],
                  writes=[t_va])
            qblocks = list(range(ncb, nkb)) if last else list(range(nkb))
            for qb in qblocks:
                if qb < ncb:
                    kbl = [(j, None) for j in range(ncb)]
                else:
                    kbl = [(j, None) for j in range(ncb)]
                    if qb - 1 >= ncb:
                        kbl.append((qb - 1, 3))
                    kbl.append((qb, None))
                    if qb + 1 < nkb:
                        kbl.append((qb + 1, 1))
                qT, t_qT = qTs[oi % 2]
                k.dma(qT[:], S.QT[b, kv * 256:(kv + 1) * 256, qb * 128:(qb + 1) * 128].rearrange("(g p) t -> p g t", p=64),
                      reads=[S.t_QT[b]], writes=[t_qT])
                po, tpo = k.ps()
                for i, (kb, m) in enumerate(kbl):
                    ps, tps = k.ps()
                    k.op("pe", lambda e: e.matmul(ps[:, 0:512], kT[:, kb * 128:(kb + 1) * 128], qT[:, :, :], start=True, stop=True),
                         reads=[t_kT, t_qT], writes=[tps])
                    pt, tpt = pts[pi % 3]
                    pi += 1
                    k.op("act", lambda e: e.activation(out=pt[:], in_=ps[:, 0:512], func=AF.Exp, scale=0.125),
                         reads=[tps], writes=[tpt])
                    if m is not None:
                        k.op("pool", lambda e: e.tensor_tensor(
                            out=pt[:].rearrange("p (g q) -> p g q", g=AG), in0=pt[:].rearrange("p (g q) -> p g q", g=AG),
                            in1=mskb[:, m:m + 1, :].to_broadcast([128, AG, 128]), op=ALU.mult),
                            reads=[tpt, t_msk], writes=[tpt])
                    k.op("pe", lambda e: e.matmul(po[0:65, 0:512], va[:, kb, :], pt[:], start=(i == 0),
                                                  stop=(i == len(kbl) - 1)), reads=[t_va, tpt], writes=[tpo])
                den, tden = dens[0]
                rb, trb = rbs[0]
                ob, tob = obs[oi % 2]
                oi += 1
                k.op("dve", lambda e: e.tensor_tensor(
                    out=den[64:65, :].rearrange("p (g q) -> p g q", g=AG), in0=po[64:65, 0:512].rearrange("p (g q) -> p g q", g=AG),
                    in1=esk[64:65, kv * AG:(kv + 1) * AG].unsqueeze(2).to_broadcast([1, AG, 128]), op=ALU.add),
                    reads=[tpo, t_esk], writes=[tden])
                k.op("dve", lambda e: e.reciprocal(out=den[64:65, :], in_=den[64:65, :]), reads=[tden], writes=[tden])
                pb, tpb = k.ps()
                k.op("pe", lambda e: e.matmul(pb[0:64, 0:512], onesr[64:65, 0:64], den[64:65, :], start=True, stop=True),
                     reads=[t_on, tden], writes=[tpb])
                k.op("act", lambda e: e.copy(out=rb[:], in_=pb[0:64, 0:512]), reads=[tpb], writes=[trb])
                k.op("dve", lambda e: e.tensor_tensor(out=ob[:], in0=po[0:64, 0:512], in1=rb[:], op=ALU.mult),
                     reads=[tpo, trb], writes=[tob])
                k.dma(S.YT[b, 512 + kv * 256:512 + (kv + 1) * 256, qb * 128:(qb + 1) * 128].rearrange("(g p) t -> p g t", p=64),
                      ob[:].rearrange("p (g q) -> p g q", g=AG), reads=[tob], writes=[S.t_YTa[b]])
                yield


def ln_block(S, z, tz, lng, lnb, t_ln, sm, tsm):
    k = S.k
    k.op("dve", lambda e: e.bn_stats(out=sm[:, 0:6], in_=z[:, 0:512]), reads=[tz], writes=[tsm])
    k.op("dve", lambda e: e.bn_stats(out=sm[:, 6:12], in_=z[:, 512:1024]), reads=[tz], writes=[tsm])
    k.op("dve", lambda e: e.bn_aggr(out=sm[:, 12:14], in_=sm[:, 0:12].rearrange("p (a b) -> p a b", b=6)),
         reads=[tsm], writes=[tsm])
    rsqrt_eps(k, sm[:, 14:15], sm[:, 13:14], LN_EPS, tsm)
    k.op("dve", lambda e: e.scalar_tensor_tensor(out=sm[:, 15:16], in0=sm[:, 12:13], scalar=-1.0, in1=sm[:, 14:15],
                                                 op0=ALU.mult, op1=ALU.mult), reads=[tsm], writes=[tsm])
    k.op("act", lambda e: e.activation(out=z[:], in_=z[:], func=AF.Identity, scale=sm[:, 14:15], bias=sm[:, 15:16]),
         reads=[tz, tsm], writes=[tz])
    k.op("dve", lambda e: e.tensor_tensor(out=z[:], in0=z[:], in1=lng[:], op=ALU.mult), reads=[tz, t_ln], writes=[tz])
    k.op("pool", lambda e: e.tensor_tensor(out=z[:], in0=z[:], in1=lnb[:], op=ALU.add), reads=[tz, t_ln], writes=[tz])


def rsqrt_eps(k, out, in_, eps, tr):
    k.op("dve", lambda e: e.tensor_scalar(out=out, in0=in_, scalar1=float(eps), scalar2=None, op0=ALU.add), reads=[tr], writes=[tr])
    k.op("act", lambda e: e.activation(out=out, in_=out, func=AF.Sqrt), reads=[tr], writes=[tr])
    k.op("dve", lambda e: e.reciprocal(out=out, in_=out), reads=[tr], writes=[tr])


def load_bcast_row(S, st, name, src_row, t):
    k = S.k
    row = k.sbuf(st, name + "r", [1, D]); trow = Tr()
    k.dma(row[:], src_row, writes=[trow])
    ones = k.sbuf(st, name + "o", [1, 128]); to = Tr()
    k.op("dve", lambda e: e.memset(ones[:], 1.0), writes=[to])
    out = k.sbuf(st, name, [128, D])
    for h in range(2):
        ps, tps = k.ps()
        k.op("pe", lambda e: e.matmul(ps[:, 0:512], ones[0:1, :], row[0:1, h * 512:(h + 1) * 512], start=True, stop=True),
             reads=[trow, to], writes=[tps])
        k.op("act", lambda e: e.copy(out=out[:, h * 512:(h + 1) * 512], in_=ps[:, 0:512]), reads=[tps], writes=[t])
    return out


def stage_outln(S, l, last):
    k, cfg = S.k, S.cfg
    NB, C, L, T = cfg.NB, cfg.C, cfg.L, cfg.T
    st = contextlib.ExitStack()
    wo = k.sbuf(st, "wo", [128, 8, D], BF16); two = Tr()
    for kc in range(8):
        k.dma(wo[:, kc, :], S.inp["w_out"][l, kc * 128:(kc + 1) * 128, :], writes=[two], q="pool")
    t_ln = Tr()
    lng = load_bcast_row(S, st, "lng", S.inp["ln1_g"][l:l + 1, :], t_ln)
    lnb = load_bcast_row(S, st, "lnb", S.inp["ln1_b"][l:l + 1, :], t_ln)
    yts = [(k.sbuf(st, "yt", [128, 8, 128], BF16), Tr()) for _ in range(4)]
    xrs = [(k.sbuf(st, "xr", [128, D]), Tr()) for _ in range(4)]
    zs = [(k.sbuf(st, "z", [128, D]), Tr()) for _ in range(4)]
    sms = [(k.sbuf(st, "sm", [128, 32]), Tr()) for _ in range(4)]
    it = 0
    for b in range(NB):
        for t0 in range(C if last else 0, T, 128):
            r = NB if t0 < C else b
            yt, tyt = yts[it % 4]; xr, txr = xrs[it % 4]; z, tz = zs[it % 4]; sm, tsm = sms[it % 4]
            it += 1
            k.dma(yt[:], S.YT[b, :, t0:t0 + 128].rearrange("(kc p) t -> p kc t", p=128), reads=[S.t_YT[b]], writes=[tyt])
            k.dma(xr[:], S.XR[b, t0:t0 + 128, :], reads=[S.t_XR[b]], writes=[txr], q="act")
            for h in range(2):
                ps, tps = k.ps()
                for kc in range(8):
                    k.op("pe", lambda e: e.matmul(ps[:, 0:512], yt[:, kc, :], wo[:, kc, h * 512:(h + 1) * 512],
                                                  start=(kc == 0), stop=(kc == 7)), reads=[tyt, two], writes=[tps])
                k.op("dve", lambda e: e.tensor_tensor(out=z[:, h * 512:(h + 1) * 512], in0=ps[:, 0:512],
                                                      in1=S.gateb[:, 0, r, h * 512:(h + 1) * 512], op=ALU.mult),
                     reads=[tps, S.t_gateb], writes=[tz])
            k.op("dve", lambda e: e.scalar_tensor_tensor(out=z[:], in0=xr[:], scalar=ALPHA, in1=z[:], op0=ALU.mult,
                                                          op1=ALU.add), reads=[txr, tz], writes=[tz])
            ln_block(S, z, tz, lng, lnb, t_ln, sm, tsm)
            k.dma(S.XR[b, t0:t0 + 128, :], z[:], reads=[tz], writes=[S.t_XR[b]])
    k.barrier()
    st.close()


def stage_s5(S, l, last, side=None):
    k, cfg = S.k, S.cfg
    NB, C, L, T = cfg.NB, cfg.C, cfg.L, cfg.T
    st = contextlib.ExitStack()
    nc = k.nc
    tp = Tr()
    P = lambda name, shape: k.sbuf(st, name, shape)
    halfpi = P("halfpi", [128, 1])
    k.op("dve", lambda e: e.memset(halfpi[:], math.pi / 2), writes=[tp])
    lre = P("lre", [128, 2, 8]); lim = P("lim", [128, 2, 8]); dt = P("dt", [128, 2, 8])
    for d in range(2):
        with nc.allow_non_contiguous_dma(reason="tiny param loads"):
            k.dma(lre[:, d, :], S.inp["s5_lam_re"][l, d].rearrange("(rc g2) p -> g2 p rc", g2=2), writes=[tp])
            k.dma(lim[:, d, :], S.inp["s5_lam_im"][l, d].rearrange("(rc g2) p -> g2 p rc", g2=2), writes=[tp])
            for g2 in range(2):
                k.dma(dt[g2 * 64:(g2 + 1) * 64, d, :],
                      S.inp["s5_log_dt"][l, d:d + 1, :].rearrange("o (rc g2) -> o g2 rc", g2=2)[:, g2, :].to_broadcast([64, 8]),
                      writes=[tp])
    dv = lambda fn: k.op("dve", fn, reads=[tp], writes=[tp])
    ac = lambda fn: k.op("act", fn, reads=[tp], writes=[tp])
    ac(lambda e: e.activation(out=dt[:], in_=dt[:], func=AF.Exp))
    mag = P("mag", [128, 2, 8]); th = P("th", [128, 2, 8]); cs = P("cs", [128, 2, 8]); sn = P("sn", [128, 2, 8])
    t1 = P("t1", [128, 2, 8]); t2 = P("t2", [128, 2, 8])
    dv(lambda e: e.tensor_tensor(out=mag[:], in0=lre[:], in1=dt[:], op=ALU.mult))
    ac(lambda e: e.activation(out=mag[:], in_=mag[:], func=AF.Exp))
    dv(lambda e: e.tensor_tensor(out=th[:], in0=lim[:], in1=dt[:], op=ALU.mult))
    ac(lambda e: e.activation(out=sn[:], in_=th[:], func=AF.Sin, scale=1.0 / 16))
    ac(lambda e: e.activation(out=cs[:], in_=th[:], func=AF.Sin, scale=1.0 / 16, bias=halfpi[:, 0:1]))
    for _ in range(4):
        dv(lambda e: e.tensor_tensor(out=t1[:], in0=cs[:], in1=cs[:], op=ALU.mult))
        dv(lambda e: e.tensor_tensor(out=t2[:], in0=sn[:], in1=sn[:], op=ALU.mult))
        dv(lambda e: e.tensor_tensor(out=sn[:], in0=sn[:], in1=cs[:], op=ALU.mult))
        dv(lambda e: e.tensor_scalar(out=sn[:], in0=sn[:], scalar1=2.0, scalar2=None, op0=ALU.mult))
        dv(lambda e: e.tensor_tensor(out=cs[:], in0=t1[:], in1=t2[:], op=ALU.subtract))
    abr = P("abr", [128, 2, 8]); abi = P("abi", [128, 2, 8]); cre = P("cre", [128, 2, 8]); cim = P("cim", [128, 2, 8])
    den = P("den", [128, 2, 8])
    dv(lambda e: e.tensor_tensor(out=abr[:], in0=mag[:], in1=cs[:], op=ALU.mult))
    dv(lambda e: e.tensor_tensor(out=abi[:], in0=mag[:], in1=sn[:], op=ALU.mult))
    dv(lambda e: e.tensor_scalar(out=t1[:], in0=abr[:], scalar1=-1.0, scalar2=None, op0=ALU.add))
    dv(lambda e: e.tensor_tensor(out=den[:], in0=lre[:], in1=lre[:], op=ALU.mult))
    dv(lambda e: e.tensor_tensor(out=t2[:], in0=lim[:], in1=lim[:], op=ALU.mult))
    dv(lambda e: e.tensor_tensor(out=den[:], in0=den[:], in1=t2[:], op=ALU.add))
    dv(lambda e: e.reciprocal(out=den[:], in_=den[:]))
    dv(lambda e: e.tensor_tensor(out=cre[:], in0=t1[:], in1=lre[:], op=ALU.mult))
    dv(lambda e: e.tensor_tensor(out=t2[:], in0=abi[:], in1=lim[:], op=ALU.mult))
    dv(lambda e: e.tensor_tensor(out=cre[:], in0=cre[:], in1=t2[:], op=ALU.add))
    dv(lambda e: e.tensor_tensor(out=cre[:], in0=cre[:], in1=den[:], op=ALU.mult))
    dv(lambda e: e.tensor_tensor(out=cim[:], in0=abi[:], in1=lre[:], op=ALU.mult))
    dv(lambda e: e.tensor_tensor(out=t2[:], in0=t1[:], in1=lim[:], op=ALU.mult))
    dv(lambda e: e.tensor_tensor(out=cim[:], in0=cim[:], in1=t2[:], op=ALU.subtract))
    dv(lambda e: e.tensor_tensor(out=cim[:], in0=cim[:], in1=den[:], op=ALU.mult))
    bre = P("bre", [128, 8, 16]); bim = P("bim", [128, 8, 16])
    with nc.allow_non_contiguous_dma(reason="param loads"):
        k.dma(bre[:], S.inp["s5_b_re"][l].rearrange("(rc g2) p c -> g2 p rc c", g2=2), writes=[tp])
        k.dma(bim[:], S.inp["s5_b_im"][l].rearrange("(rc g2) p c -> g2 p rc c", g2=2), writes=[tp])
    ctr = P("ctr", [128, 8, 48]); cti = P("cti", [128, 8, 48])
    dv(lambda e: e.memset(ctr[:], 0.0))
    dv(lambda e: e.memset(cti[:], 0.0))
    with nc.allow_non_contiguous_dma(reason="param loads"):
        for g2 in range(2):
            src_r = S.inp["s5_c_re"][l].rearrange("(rc g2) c p -> g2 p rc c", g2=2)[g2]
            src_i = S.inp["s5_c_im"][l].rearrange("(rc g2) c p -> g2 p rc c", g2=2)[g2]
            for rc in range(8):
                k.dma(ctr[g2 * 64:(g2 + 1) * 64, rc, g2 * 32:g2 * 32 + 16], src_r[:, rc, :], writes=[tp])
                k.dma(cti[g2 * 64:(g2 + 1) * 64, rc, g2 * 32:g2 * 32 + 16], src_i[:, rc, :], writes=[tp])
    dv(lambda e: e.tensor_scalar(out=cti[:], in0=cti[:], scalar1=-1.0, scalar2=None, op0=ALU.mult))
    bbp = P("bbp", [128, 8, 48]); tq = P("tq", [128, 8, 16]); tq2 = P("tq2", [128, 8, 16])
    bbT = P("bbT", [48, 2, 2, 8, 128])
    for d in range(2):
        for ri in range(2):
            crb = cre[:, d, :].unsqueeze(2).to_broadcast([128, 8, 16])
            cib = cim[:, d, :].unsqueeze(2).to_broadcast([128, 8, 16])
            if ri == 0:
                dv(lambda e: e.tensor_tensor(out=tq[:], in0=bre[:], in1=crb, op=ALU.mult))
                dv(lambda e: e.tensor_tensor(out=tq2[:], in0=bim[:], in1=cib, op=ALU.mult))
                dv(lambda e: e.tensor_tensor(out=tq[:], in0=tq[:], in1=tq2[:], op=ALU.subtract))
            else:
                dv(lambda e: e.tensor_tensor(out=tq[:], in0=bim[:], in1=crb, op=ALU.mult))
                dv(lambda e: e.tensor_tensor(out=tq2[:], in0=bre[:], in1=cib, op=ALU.mult))
                dv(lambda e: e.tensor_tensor(out=tq[:], in0=tq[:], in1=tq2[:], op=ALU.add))
            dv(lambda e: e.memset(bbp[:], 0.0))
            dv(lambda e: e.tensor_copy(out=bbp[0:64, :, 0:16], in_=tq[0:64, :, :]))
            dv(lambda e: e.tensor_copy(out=bbp[64:128, :, 32:48], in_=tq[64:128, :, :]))
            for rc in range(8):
                ps, tps = k.ps()
                k.op("pe", lambda e: e.transpose(out=ps[0:48, 0:128], in_=bbp[:, rc, :], identity=S.ident[:, :]),
                     reads=[tp, S.t_const], writes=[tps])
                k.op("act", lambda e: e.copy(out=bbT[:, d, ri, rc, :], in_=ps[0:48, 0:128]), reads=[tps], writes=[tp])
    nd = 0
    while (1 << nd) < T:
        nd += 1
    rc_n = P("rcn", [128, 2, 8, nd + 1]); rs_n = P("rsn", [128, 2, 8, nd + 1])
    dv(lambda e: e.tensor_copy(out=rc_n[:, :, :, 0], in_=cs[:]))
    dv(lambda e: e.tensor_copy(out=rs_n[:, :, :, 0], in_=sn[:]))
    for i in range(nd):
        dv(lambda e: e.tensor_tensor(out=t1[:], in0=rc_n[:, :, :, i], in1=rc_n[:, :, :, i], op=ALU.mult))
        dv(lambda e: e.tensor_tensor(out=t2[:], in0=rs_n[:, :, :, i], in1=rs_n[:, :, :, i], op=ALU.mult))
        dv(lambda e: e.tensor_tensor(out=rc_n[:, :, :, i + 1], in0=t1[:], in1=t2[:], op=ALU.subtract))
        dv(lambda e: e.tensor_tensor(out=t1[:], in0=rc_n[:, :, :, i], in1=rs_n[:, :, :, i], op=ALU.mult))
        dv(lambda e: e.tensor_scalar(out=rs_n[:, :, :, i + 1], in0=t1[:], scalar1=2.0, scalar2=None, op0=ALU.mult))
    dskip = P("dskip", [128, 2]); glub = P("glub", [128, 2])
    with nc.allow_non_contiguous_dma(reason="param loads"):
        k.dma(dskip[:], S.inp["s5_d"][l].rearrange("(kc p) -> p kc", p=128), writes=[tp])
        k.dma(glub[:], S.inp["s5_glu_b"][l].rearrange("(kc p) -> p kc", p=128), writes=[tp])
    gluw = k.sbuf(st, "gluw", [128, 2, 256], BF16)
    k.dma(gluw[:], S.inp["s5_glu_w"][l].rearrange("(kc p) n -> p kc n", p=128), writes=[tp], q="pool")

    cosT = P("cosT5", [128, T]); sinT = P("sinT5", [128, T]); t_tab = Tr()
    tabT = P("tabT5", [128, T]); t_tabT = Tr()
    st_main = contextlib.ExitStack()
    PM = lambda name, shape: k.sbuf(st_main, name, shape)
    bufs = []
    for b in range(NB):
        d_ = {}
        for nm in ("zr", "zi", "gr", "gi"):
            d_[nm] = (PM(nm + "5", [128, T]), Tr())
        d_["up"] = (PM("up5", [48, T]), Tr())
        d_["stg"] = [(PM("stg5", [48, 512]), Tr()) for _ in range(1)]
        k.op("dve", lambda e: e.memset(d_["up"][0][:], 0.0), writes=[d_["up"][1]])
        bufs.append(d_)
    ustage = PM("ustage5", [48, T]); t_ust = Tr()
    k.op("dve", lambda e: e.memset(ustage[:], 0.0), writes=[t_ust])

    def body(d, rc, b):
        B_ = bufs[b]
        zr, t_zr = B_["zr"]; zi, t_zi = B_["zi"]; gr, t_gr = B_["gr"]; gi, t_gi = B_["gi"]
        up, t_up = B_["up"]; stg = B_["stg"]
        src = S.PS[b].rearrange("(rc g2 c) t -> rc g2 c t", g2=2, c=16)
        if d == 0:
            k.dma(up[0:16, :], src[rc, 0], reads=[S.t_PS[b]], writes=[t_up])
            k.dma(up[32:48, :], src[rc, 1], reads=[S.t_PS[b]], writes=[t_up])
        else:
            k.dma(ustage[0:16, :], src[rc, 0], reads=[S.t_PS[b]], writes=[t_ust])
            k.dma(ustage[32:48, :], src[rc, 1], reads=[S.t_PS[b]], writes=[t_ust])
            k.op("pool", lambda e: e.tensor_copy(out=up[:, 0:C], in_=ustage[:, 0:C][:, ::-1]), reads=[t_ust], writes=[t_up])
            k.op("pool", lambda e: e.tensor_copy(out=up[:, C:T], in_=ustage[:, C:T][:, ::-1]), reads=[t_ust], writes=[t_up])
        u_, tu_ = up, t_up
        yield
        for (t0, wd) in tiles(0, T, 512):
            pr_, tpr = k.ps()
            pi_, tpi = k.ps()
            k.op("pe", lambda e: e.matmul(pr_[:, 0:wd], bbT[:, d, 0, rc, :], u_[:, t0:t0 + wd], start=True, stop=True),
                 reads=[tp, tu_], writes=[tpr])
            k.op("pe", lambda e: e.matmul(pi_[:, 0:wd], bbT[:, d, 1, rc, :], u_[:, t0:t0 + wd], start=True, stop=True),
                 reads=[tp, tu_], writes=[tpi])
            sl = slice(t0, t0 + wd)
            k.op("dve", lambda e: e.tensor_tensor(out=zr[:, sl], in0=pr_[:, 0:wd], in1=cosT[:, sl], op=ALU.mult),
                 reads=[tpr, t_tab], writes=[t_zr])
            k.op("dve", lambda e: e.tensor_tensor(out=gr[:, sl], in0=pi_[:, 0:wd], in1=sinT[:, sl], op=ALU.mult),
                 reads=[tpi, t_tab], writes=[t_gr])
            k.op("pool", lambda e: e.tensor_tensor(out=zr[:, sl], in0=zr[:, sl], in1=gr[:, sl], op=ALU.add),
                 reads=[t_gr], writes=[t_zr])
            k.op("dve", lambda e: e.tensor_tensor(out=zi[:, sl], in0=pi_[:, 0:wd], in1=cosT[:, sl], op=ALU.mult),
                 reads=[tpi, t_tab], writes=[t_zi])
            k.op("dve", lambda e: e.tensor_tensor(out=gi[:, sl], in0=pr_[:, 0:wd], in1=sinT[:, sl], op=ALU.mult),
                 reads=[tpr, t_tab], writes=[t_gi])
            k.op("pool", lambda e: e.tensor_tensor(out=zi[:, sl], in0=zi[:, sl], in1=gi[:, sl], op=ALU.subtract),
                 reads=[t_gi], writes=[t_zi])
            yield
        mg = mag[:, d, rc:rc + 1].to_broadcast([128, T])
        k.op("dve", lambda e: e.tensor_tensor_scan(out=gr[:], data0=mg, data1=zr[:], initial=0.0, op0=ALU.mult, op1=ALU.add),
             reads=[t_zr, tp], writes=[t_gr])
        yield
        k.op("dve", lambda e: e.tensor_tensor_scan(out=gi[:], data0=mg, data1=zi[:], initial=0.0, op0=ALU.mult, op1=ALU.add),
             reads=[t_zi, tp], writes=[t_gi])
        yield
        k.op("dve", lambda e: e.tensor_tensor(out=zr[:], in0=gr[:], in1=cosT[:], op=ALU.mult), reads=[t_gr, t_tab], writes=[t_zr])
        k.op("pool", lambda e: e.tensor_tensor(out=zi[:], in0=gi[:], in1=sinT[:], op=ALU.mult), reads=[t_gi, t_tab], writes=[t_zi])
        yield
        k.op("dve", lambda e: e.tensor_tensor(out=zr[:], in0=zr[:], in1=zi[:], op=ALU.subtract), reads=[t_zi], writes=[t_zr])
        yield
        k.op("pool", lambda e: e.tensor_tensor(out=zi[:], in0=gi[:], in1=cosT[:], op=ALU.mult), reads=[t_gi, t_tab, t_zr], writes=[t_zi])
        k.op("dve", lambda e: e.tensor_tensor(out=gr[:], in0=gr[:], in1=sinT[:], op=ALU.mult), reads=[t_tab], writes=[t_gr])
        yield
        k.op("pool", lambda e: e.tensor_tensor(out=zi[:], in0=zi[:], in1=gr[:], op=ALU.add), reads=[t_gr], writes=[t_zi])
        yield
        for ti, (t0, wd) in enumerate(tiles(0, T, 512)):
            py, tpy = k.ps()
            k.op("pe", lambda e: e.matmul(py[0:48, 0:wd], ctr[:, rc, :], zr[:, t0:t0 + wd], start=True, stop=False),
                 reads=[tp, t_zr], writes=[tpy])
            k.op("pe", lambda e: e.matmul(py[0:48, 0:wd], cti[:, rc, :], zi[:, t0:t0 + wd], start=False, stop=True),
                 reads=[tp, t_zi], writes=[tpy])
            sg, tsg = stg[0]
            k.op("act", lambda e: e.copy(out=sg[:, 0:wd], in_=py[0:48, 0:wd]), reads=[tpy], writes=[tsg])
            dst = S.YS[b, d].rearrange("(rc g2 c) t -> rc g2 c t", g2=2, c=16)
            k.dma(dst[rc, 0, :, t0:t0 + wd], sg[0:16, 0:wd], reads=[tsg], writes=[S.t_YS[b]])
            k.dma(dst[rc, 1, :, t0:t0 + wd], sg[32:48, 0:wd], reads=[tsg], writes=[S.t_YS[b]])
            yield

    side_alive = [side is not None]
    for d in range(2):
        for rc in range(8):
            k.op("dve", lambda e: e.memset(cosT[:, 0:1], 1.0), reads=[t_tab], writes=[t_tab])
            k.op("dve", lambda e: e.memset(sinT[:, 0:1], 0.0), reads=[t_tab], writes=[t_tab])
            n = 1
            i = 0
            while n < T:
                m = min(n, T - n)
                cn = rc_n[:, d, rc, i:i + 1]; sn_ = rs_n[:, d, rc, i:i + 1]
                k.op("dve", lambda e: e.tensor_scalar(out=tabT[:, 0:m], in0=sinT[:, 0:m], scalar1=sn_, scalar2=None, op0=ALU.mult),
                     reads=[t_tab, tp], writes=[t_tabT])
                k.op("dve", lambda e: e.scalar_tensor_tensor(out=cosT[:, n:n + m], in0=cosT[:, 0:m], scalar=cn, in1=tabT[:, 0:m],
                                                             op0=ALU.mult, op1=ALU.subtract), reads=[t_tab, t_tabT, tp], writes=[t_tab])
                k.op("dve", lambda e: e.tensor_scalar(out=tabT[:, 0:m], in0=cosT[:, 0:m], scalar1=sn_, scalar2=None, op0=ALU.mult),
                     reads=[t_tab, tp], writes=[t_tabT])
                k.op("dve", lambda e: e.scalar_tensor_tensor(out=sinT[:, n:n + m], in0=sinT[:, 0:m], scalar=cn, in1=tabT[:, 0:m],
                                                             op0=ALU.mult, op1=ALU.add), reads=[t_tab, t_tabT, tp], writes=[t_tab])
                n *= 2
                i += 1
            gens = [body(d, rc, b) for b in range(NB)]
            while gens:
                for g_ in list(gens):
                    try:
                        next(g_)
                    except StopIteration:
                        gens.remove(g_)
                if side_alive[0]:
                    try:
                        next(side)
                    except StopIteration:
                        side_alive[0] = False
    k.barrier()
    st_main.close()
    TW = 512
    uu = [(P("uu", [128, 2, TW]), Tr()) for _ in range(2)]
    y0 = [(P("y0", [128, 2, TW]), Tr()) for _ in range(2)]
    y1 = [(P("y1", [128, 2, TW]), Tr()) for _ in range(2)]
    zz = [(P("zz", [128, 2, TW]), Tr()) for _ in range(2)]
    zb = [(k.sbuf(st, "zb", [128, 2, TW], BF16), Tr()) for _ in range(2)]
    ob = [(k.sbuf(st, "ob5", [128, 2, TW], BF16), Tr()) for _ in range(2)]
    it = 0
    GC = 2.0 * math.sqrt(2.0 / math.pi)
    for b in range(NB):
        segs = ([] if last else tiles(0, C, TW)) + tiles(C, T, TW)
        for (t0, wd) in segs:
            seg0, seg1 = (0, C) if t0 < C else (C, T)
            r0 = seg0 + (seg1 - (t0 + wd))
            u_, tu = uu[it % 2]; a0, ta0 = y0[it % 2]; a1, ta1 = y1[it % 2]; z_, tz = zz[it % 2]
            zb_, tzb = zb[it % 2]; ob_, tob = ob[it % 2]
            it += 1
            k.dma(u_[:, :, 0:wd], S.PS[b, :, t0:t0 + wd].rearrange("(kc p) t -> p kc t", p=128), reads=[S.t_PS[b]], writes=[tu])
            k.dma(a0[:, :, 0:wd], S.YS[b, 0, :, t0:t0 + wd].rearrange("(kc p) t -> p kc t", p=128), reads=[S.t_YS[b]], writes=[ta0])
            k.dma(a1[:, :, 0:wd], S.YS[b, 1, :, r0:r0 + wd].rearrange("(kc p) t -> p kc t", p=128), reads=[S.t_YS[b]], writes=[ta1],
                  q="act")
            for kc in range(2):
                k.op("dve", lambda e: e.scalar_tensor_tensor(out=z_[:, kc, 0:wd], in0=u_[:, kc, 0:wd], scalar=dskip[:, kc:kc + 1],
                                                             in1=a0[:, kc, 0:wd], op0=ALU.mult, op1=ALU.add),
                     reads=[tu, ta0, tp], writes=[tz])
            k.op("dve", lambda e: e.tensor_tensor(out=z_[:, :, 0:wd], in0=z_[:, :, 0:wd], in1=a1[:, :, 0:wd][:, :, ::-1], op=ALU.add),
                 reads=[ta1], writes=[tz])
            k.op("pool", lambda e: e.tensor_tensor(out=a0[:, :, 0:wd], in0=z_[:, :, 0:wd], in1=z_[:, :, 0:wd], op=ALU.mult),
                 reads=[tz], writes=[ta0])
            k.op("dve", lambda e: e.tensor_scalar(out=a0[:, :, 0:wd], in0=a0[:, :, 0:wd], scalar1=0.044715, scalar2=1.0,
                                                  op0=ALU.mult, op1=ALU.add), reads=[ta0], writes=[ta0])
            k.op("pool", lambda e: e.tensor_tensor(out=a0[:, :, 0:wd], in0=a0[:, :, 0:wd], in1=z_[:, :, 0:wd], op=ALU.mult),
                 reads=[tz, ta0], writes=[ta0])
            k.op("act", lambda e: e.activation(out=a0[:, :, 0:wd], in_=a0[:, :, 0:wd], func=AF.Sigmoid, scale=GC),
                 reads=[ta0], writes=[ta0])
            k.op("dve", lambda e: e.tensor_tensor(out=z_[:, :, 0:wd], in0=z_[:, :, 0:wd], in1=a0[:, :, 0:wd], op=ALU.mult),
                 reads=[ta0], writes=[tz])
            k.op("pool", lambda e: e.tensor_copy(out=zb_[:, :, 0:wd], in_=z_[:, :, 0:wd]), reads=[tz], writes=[tzb])
            for oc in range(2):
                ps, tps = k.ps()
                for kc in range(2):
                    k.op("pe", lambda e: e.matmul(ps[:, 0:wd], gluw[:, kc, oc * 128:(oc + 1) * 128], zb_[:, kc, 0:wd],
                                                  start=(kc == 0), stop=(kc == 1)), reads=[tp, tzb], writes=[tps])
                k.op("act", lambda e: e.activation(out=a1[:, oc, 0:wd], in_=ps[:, 0:wd], func=AF.Sigmoid, bias=glub[:, oc:oc + 1],
                                                   scale=1.0), reads=[tps, tp], writes=[ta1])
            k.op("dve", lambda e: e.tensor_tensor(out=ob_[:, :, 0:wd], in0=z_[:, :, 0:wd], in1=a1[:, :, 0:wd], op=ALU.mult),
                 reads=[tz, ta1], writes=[tob])
            k.dma(S.YT[b, 256:512, t0:t0 + wd].rearrange("(kc p) t -> p kc t", p=128), ob_[:, :, 0:wd], reads=[tob],
                  writes=[S.t_YT[b]])
    k.barrier()
    st.close()


def stage_rwkv_conv(S, l):
    k, cfg = S.k, S.cfg
    NB, C, L, T = cfg.NB, cfg.C, cfg.L, cfg.T
    st = contextlib.ExitStack()
    nc = k.nc
    cwT = k.sbuf(st, "cwT", [128, 8, 3]); tcw = Tr()
    with nc.allow_non_contiguous_dma(reason="tiny param load"):
        for j in range(3):
            k.dma(cwT[:, :, j], S.inp["rwkv_conv"][l, j].rearrange("(kc p) -> p kc", p=128), writes=[tcw])
    pins = [(k.sbuf(st, "pin", [128, 8, 514]), Tr()) for _ in range(2)]
    pos = [(k.sbuf(st, "po", [128, 8, 512]), Tr()) for _ in range(2)]
    it = 0
    for b in range(NB):
        for (s0, s1) in ((0, C), (C, T)):
            for (t0, wd) in tiles(s0, s1, 512):
                pin, tpin = pins[it % 2]; po, tpo = pos[it % 2]
                it += 1
                lo = max(t0 - 1, s0); hi = min(t0 + wd + 1, s1)
                if t0 == s0:
                    k.op("pool", lambda e: e.memset(pin[:, :, 0:1], 0.0), writes=[tpin])
                if t0 + wd == s1:
                    k.op("pool", lambda e: e.memset(pin[:, :, wd + 1:wd + 2], 0.0), writes=[tpin])
                o0 = lo - (t0 - 1)
                for kc in range(8):
                    k.dma(pin[:, kc, o0:o0 + (hi - lo)], S.PR[b, kc * 128:(kc + 1) * 128, lo:hi], reads=[S.t_PR[b]], writes=[tpin],
                          q=("sp" if kc % 2 == 0 else "act"))
                for kc in range(8):
                    en = "dve"
                    k.op(en, lambda e: e.tensor_scalar(out=po[:, kc, 0:wd], in0=pin[:, kc, 0:wd], scalar1=cwT[:, kc, 0:1], scalar2=None,
                                                       op0=ALU.mult), reads=[tpin, tcw], writes=[tpo])
                    for j in (1, 2):
                        k.op(en, lambda e: e.scalar_tensor_tensor(out=po[:, kc, 0:wd], in0=pin[:, kc, j:j + wd], scalar=cwT[:, kc, j:j + 1],
                                                                  in1=po[:, kc, 0:wd], op0=ALU.mult, op1=ALU.add),
                             reads=[tpin, tcw], writes=[tpo])
                for kc in range(8):
                    k.dma(S.PC[b, kc * 128:(kc + 1) * 128, t0:t0 + wd], po[:, kc, 0:wd], reads=[tpo], writes=[S.t_PC[b]])
    k.barrier()
    st.close()


def stage_rwkv(S, l, last):
    stage_rwkv_conv(S, l)
    k, cfg = S.k, S.cfg
    NB, C, L, T = cfg.NB, cfg.C, cfg.L, cfg.T
    st = contextlib.ExitStack()
    nc = k.nc
    CH = 128
    tp = Tr()
    P = lambda name, shape, dt=F32: k.sbuf(st, name, shape, dt)
    kkp = P("kkp", [64, 4]); kap = P("kap", [64, 4]); omka = P("omka", [64, 4]); rkp = P("rkp", [64, 4])
    a0T = P("a0T", [64, 2, 4])
    with nc.allow_non_contiguous_dma(reason="tiny param loads"):
        k.dma(kkp[:], S.inp["rwkv_k_k"][l].rearrange("(h p) -> p h", p=64), writes=[tp])
        k.dma(kap[:], S.inp["rwkv_k_a"][l].rearrange("(h p) -> p h", p=64), writes=[tp])
        k.dma(rkp[:], S.inp["rwkv_r_k"][l].rearrange("h p -> p h"), writes=[tp])
        for d in range(2):
            k.dma(a0T[:, d, :], S.inp["rwkv_a0"][l, d].rearrange("(h p) -> p h", p=64), writes=[tp])
    k.op("dve", lambda e: e.tensor_scalar(out=omka[:], in0=kap[:], scalar1=-1.0, scalar2=1.0, op0=ALU.mult, op1=ALU.add),
         reads=[tp], writes=[tp])
    w2a = P("w2a", [65, 2, 256]); a2 = P("a2", [64, 2, 256]); g2 = P("g2", [128, 256])
    for d in range(2):
        k.dma(w2a[0:64, d, :], S.inp["rwkv_w2"][l, d], writes=[tp])
        k.dma(w2a[64:65, d, :], S.inp["rwkv_w0"][l, d:d + 1, :], writes=[tp])
        k.dma(a2[:, d, :], S.inp["rwkv_a2"][l, d], writes=[tp])
    k.dma(g2[:], S.inp["rwkv_g2"][l], writes=[tp])
    tri = P("tri", [128, 4, 128]); mskb = P("mskb", [128, 4, 128], BF16)
    k.dma(tri[:].rearrange("p a b -> p (a b)"), S.inp["tri"][:, :], writes=[tp])
    k.dma(mskb[:].rearrange("p a b -> p (a b)"), S.inp["msk"][:, :], writes=[tp], q="pool")
    ones64 = P("ones64", [64, 64])
    k.op("dve", lambda e: e.memset(ones64[:], 1.0), reads=[tp], writes=[tp])
    t_gn = Tr()
    gnw = P("gnw", [128, 256]); gnb = P("gnb", [128, 256])
    for (dst, nm) in ((gnw, "rwkv_gn_w"), (gnb, "rwkv_gn_b")):
        row = P("gnrow", [1, 256]); trow = Tr()
        k.dma(row[:], S.inp[nm][l:l + 1, :], writes=[trow])
        onesr = P("gnones", [1, 128])
        k.op("dve", lambda e: e.memset(onesr[:], 1.0), writes=[trow])
        ps, tps = k.ps()
        k.op("pe", lambda e: e.matmul(ps[:, 0:256], onesr[0:1, :], row[0:1, :], start=True, stop=True), reads=[trow], writes=[tps])
        k.op("act", lambda e: e.copy(out=dst[:], in_=ps[:, 0:256]), reads=[tps], writes=[t_gn])

    NS = max(NB, 1)
    def mk(name, shape, dt=F32):
        return [(k.sbuf(st, name, shape, dt), Tr()) for _ in range(NS)]
    rkv_s = mk("rkv", [64, 3, 4, CH]); wl_s = mk("wl", [65, CH]); al_s = mk("al", [64, CH]); gl_s = mk("gl", [128, CH])
    kq_s = mk("kq", [64, 4, CH]); tA_s = mk("tA", [64, 4, CH]); kk_s = mk("kk", [64, 4, CH])
    lw_s = mk("lw", [128, 256]); Ep_s = mk("Ep", [64, 4, CH]); Em_s = mk("Em", [64, 4, CH]); Ex_s = mk("Ex", [64, 4, CH])
    a_s = mk("a", [64, 4, CH]); kdir_s = mk("kdir", [64, 4, CH]); bv_s = mk("bv", [64, 4, CH])
    kkq_s = mk("kkq", [64, 4, CH]); rq_s = mk("rq", [64, 4, CH]); kd_s = mk("kd", [64, 4, CH]); bd_s = mk("bd", [64, 4, CH])
    kE_s = mk("kE", [64, 4, CH]); bE_s = mk("bE", [64, 4, CH])
    fb_s = mk("fb", [64, 4, 4, CH], BF16)
    tm_s = mk("tm", [128, 3, 4, 64], BF16)
    vtm_s = mk("vtm", [128, 4, 64]); vtb_s = mk("vtb", [128, 4, 64], BF16)
    A_s = mk("A", [128, 5, 4, CH], BF16)
    ivs = [{nm: (k.sbuf(st, "iv" + nm, [128, 4, CH], BF16), Tr()) for nm in
            ("N0", "N0T", "Xa", "Xb", "XTa", "XTb", "P0", "P1", "PT0", "PT1", "W", "W2")} for _ in range(NS)]
    bmskb = P("bmskb", [128, 4, 128], BF16)
    k.dma(bmskb[:].rearrange("p a b -> p (a b)"), S.inp["bmsk"][:, :], writes=[tp], q="pool")
    rhs2_s = mk("rhs2", [128, 4, 128], BF16); mu_s = mk("mu", [128, 4, 128], BF16)
    GT_s = mk("GT", [64, 4, 64]); J_s = mk("J", [64, 4, 64]); RT_s = mk("RT", [64, 4, CH])
    ys_s = mk("ysb", [128, 260]); yd0_s = mk("yd0", [128, 260])
    rk_s = mk("rk", [64, 4, CH])
    yc_s = mk("yc", [128, 4, 64]); y2_s = mk("y2", [128, 4, 64]); st_s = mk("stt", [128, 16]); gt_s = mk("gts", [128, 256])
    ot_s = mk("ot", [128, 2, CH], BF16)
    Hb = [[(P("H", [64, 4, 64]), Tr()) for _ in range(2)] for _ in range(NB * 2)]
    def chain(b, d):
        if True:
            Hs = Hb[b * 2 + d]
            hcur = 0
            k.op("dve", lambda e: e.memset(Hs[0][0][:], 0.0), writes=[Hs[0][1]])
            ctxc = list(range(0, C, CH)); latc = list(range(C, T, CH))
            order = (ctxc + latc) if d == 0 else (ctxc[::-1] + latc[::-1])
            iend = CH - 1 if d == 0 else 0
            m_strict, m_incl, m_nt = (0, 1, 2) if d == 0 else (2, 3, 0)
            tri_i, tri_x = (0, 1) if d == 0 else (2, 3)
            for t0 in order:
                emit = not (last and t0 < C)
                s = b % NS
                yield
                g = lambda lst: lst[s]
                rkv, trkv = g(rkv_s); wl, twl = g(wl_s); al, tal = g(al_s); gl, tgl = g(gl_s)
                for f in range(3):
                    k.dma(rkv[:, f], S.PC[b, f * 256:(f + 1) * 256, t0:t0 + CH].rearrange("(h p) t -> p h t", p=64),
                          reads=[S.t_PC[b]], writes=[trkv], q=("sp" if f != 1 else "act"))
                k.dma(wl[0:64, :], S.PC[b, 768:832, t0:t0 + CH], reads=[S.t_PC[b]], writes=[twl])
                k.dma(al[:], S.PC[b, 832:896, t0:t0 + CH], reads=[S.t_PC[b]], writes=[tal], q="act")
                if emit and d == 1:
                    k.dma(gl[:], S.PC[b, 896:1024, t0:t0 + CH], reads=[S.t_PC[b]], writes=[tgl])
                r_, k_, v_ = rkv[:, 0], rkv[:, 1], rkv[:, 2]
                B3 = lambda ap2: ap2.unsqueeze(2).to_broadcast([64, 4, CH])
                def tt(en, out, in0, in1, op, rd, wr):
                    k.op(en, lambda e: e.tensor_tensor(out=out, in0=in0, in1=in1, op=op), reads=rd, writes=wr)
                kq, tkq = g(kq_s); tA, ttA = g(tA_s); kk, tkk = g(kk_s)
                tt("dve", kq[:], k_, B3(kkp[:, :]), ALU.mult, [trkv, tp], [tkq])
                tt("pool", tA[:], kq[:], kq[:], ALU.mult, [tkq], [ttA])
                ps, tps = k.ps()
                k.op("pe", lambda e: e.matmul(ps[0:64, 0:512], ones64[:, :], tA[:].rearrange("p h t -> p (h t)"), start=True, stop=True),
                     reads=[ttA, tp], writes=[tps])
                k.op("dve", lambda e: e.tensor_scalar(out=tA[:].rearrange("p h t -> p (h t)"), in0=ps[0:64, 0:512], scalar1=1e-12,
                                                      scalar2=None, op0=ALU.add), reads=[tps], writes=[ttA])
                k.op("act", lambda e: e.activation(out=tA[:], in_=tA[:], func=AF.Sqrt), reads=[ttA], writes=[ttA])
                k.op("dve", lambda e: e.reciprocal(out=tA[:], in_=tA[:]), reads=[ttA], writes=[ttA])
                tt("dve", kk[:], kq[:], tA[:], ALU.mult, [tkq, ttA], [tkk])
                k.op("act", lambda e: e.activation(out=wl[0:64, :], in_=wl[0:64, :], func=AF.Tanh), reads=[twl], writes=[twl])
                k.op("dve", lambda e: e.memset(wl[64:65, :], 1.0), reads=[twl], writes=[twl])
                yield
                lw, tlw = g(lw_s)
                ps, tps = k.ps()
                k.op("pe", lambda e: e.matmul(ps[:, 0:256], wl[:, :], w2a[:, d, :], start=True, stop=True), reads=[twl, tp], writes=[tps])
                k.op("act", lambda e: e.activation(out=lw[:], in_=ps[:, 0:256], func=AF.Sigmoid), reads=[tps], writes=[tlw])
                pL, tpL = k.ps()
                pX, tpX = k.ps()
                for h in range(4):
                    k.op("pe", lambda e: e.matmul(pL[0:64, h * CH:(h + 1) * CH], lw[:, h * 64:(h + 1) * 64], tri[:, tri_i, :],
                                                  start=True, stop=True), reads=[tlw, tp], writes=[tpL])
                    k.op("pe", lambda e: e.matmul(pX[0:64, h * CH:(h + 1) * CH], lw[:, h * 64:(h + 1) * 64], tri[:, tri_x, :],
                                                  start=True, stop=True), reads=[tlw, tp], writes=[tpX])
                Ep, tEp = g(Ep_s); Em, tEm = g(Em_s); Ex, tEx = g(Ex_s)
                F2 = lambda t_: t_[:].rearrange("p h t -> p (h t)")
                k.op("act", lambda e: e.activation(out=F2(Ep), in_=pL[0:64, 0:512], func=AF.Exp), reads=[tpL], writes=[tEp])
                k.op("act", lambda e: e.activation(out=F2(Em), in_=pL[0:64, 0:512], func=AF.Exp, scale=-1.0), reads=[tpL], writes=[tEm])
                k.op("act", lambda e: e.activation(out=F2(Ex), in_=pX[0:64, 0:512], func=AF.Exp), reads=[tpX], writes=[tEx])
                a_, ta_ = g(a_s)
                pa, tpa = k.ps()
                for h in range(4):
                    k.op("pe", lambda e: e.matmul(pa[0:64, h * CH:(h + 1) * CH], a2[:, d, h * 64:(h + 1) * 64], al[:, :], start=True, stop=True),
                         reads=[tal, tp], writes=[tpa])
                for h in range(4):
                    k.op("act", lambda e: e.activation(out=a_[:, h, :], in_=pa[0:64, h * CH:(h + 1) * CH], func=AF.Sigmoid,
                                                       bias=a0T[:, d, h:h + 1], scale=1.0), reads=[tpa, tp], writes=[ta_])
                yield
                kdir, tkd_ = g(kdir_s); bv, tbv = g(bv_s)
                tt("dve", kdir[:], a_[:], B3(kap[:, :]), ALU.mult, [ta_, tp], [tkd_])
                tt("pool", kdir[:], kdir[:], B3(omka[:, :]), ALU.add, [tp], [tkd_])
                tt("dve", kdir[:], kdir[:], k_, ALU.mult, [trkv], [tkd_])
                tt("pool", bv[:], kk[:], a_[:], ALU.mult, [tkk, ta_], [tbv])
                kkq, tkkq = g(kkq_s); rq, trq = g(rq_s); kd, tkd = g(kd_s); bd, tbd = g(bd_s); kE, tkE = g(kE_s); bE, tbE = g(bE_s)
                tt("dve", kkq[:], kk[:], Ex[:], ALU.mult, [tkk, tEx], [tkkq])
                tt("pool", rq[:], r_, Ep[:], ALU.mult, [trkv, tEp], [trq])
                tt("dve", kd[:], kdir[:], Em[:], ALU.mult, [tkd_, tEm], [tkd])
                tt("pool", bd[:], bv[:], Em[:], ALU.mult, [tbv, tEm], [tbd])
                wCb = Ep[:, :, iend:iend + 1].to_broadcast([64, 4, CH])
                tt("dve", kE[:], kd[:], wCb, ALU.mult, [tkd, tEp], [tkE])
                tt("pool", bE[:], bd[:], wCb, ALU.mult, [tbd, tEp], [tbE])
                fb, tfb = g(fb_s)
                for i_, (src, tsrc) in enumerate(((kkq, tkkq), (rq, trq), (kd, tkd), (bd, tbd))):
                    k.op("pool" if i_ % 2 else "act", (lambda e: e.tensor_copy(out=fb[:, i_], in_=src[:])) if i_ % 2 else
                         (lambda e: e.copy(out=fb[:, i_], in_=src[:])), reads=[tsrc], writes=[tfb])
                yield
                tm, ttm = g(tm_s); vtm, tvtm = g(vtm_s); vtb, tvtb = g(vtb_s)
                for i_, (src, tsrc) in enumerate(((kkq, tkkq), (kE, tkE), (bE, tbE))):
                    ps, tps = k.ps()
                    for h in range(4):
                        k.op("pe", lambda e: e.transpose(out=ps[:, h * 64:(h + 1) * 64], in_=src[:, h, :], identity=S.ident[0:64, 0:64]),
                             reads=[tsrc, S.t_const], writes=[tps])
                    k.op("act" if i_ % 2 else "dve", (lambda e: e.copy(out=tm[:, i_].rearrange("p h v -> p (h v)"), in_=ps[:, 0:256])) if i_ % 2
                         else (lambda e: e.tensor_copy(out=tm[:, i_].rearrange("p h v -> p (h v)"), in_=ps[:, 0:256])), reads=[tps], writes=[ttm])
                ps, tps = k.ps()
                for h in range(4):
                    k.op("pe", lambda e: e.transpose(out=ps[:, h * 64:(h + 1) * 64], in_=v_[:, h, :], identity=S.ident[0:64, 0:64]),
                         reads=[trkv, S.t_const], writes=[tps])
                k.op("act", lambda e: e.copy(out=vtm[:].rearrange("p h v -> p (h v)"), in_=ps[:, 0:256]), reads=[tps], writes=[tvtm])
                k.op("dve", lambda e: e.tensor_copy(out=vtb[:].rearrange("p h v -> p (h v)"), in_=ps[:, 0:256]), reads=[tps], writes=[tvtb])
                yield
                A, tA5 = g(A_s)
                specs = ((3, 0, m_strict), (0, 3, m_nt), (2, 0, m_strict), (2, 1, m_incl), (3, 1, m_incl))
                for ai, (li, ri, mi) in enumerate(specs):
                    ps, tps = k.ps()
                    for h in range(4):
                        k.op("pe", lambda e: e.matmul(ps[:, h * CH:(h + 1) * CH], fb[:, li, h, :], fb[:, ri, h, :], start=True, stop=True),
                             reads=[tfb], writes=[tps])
                    k.op("dve", lambda e: e.tensor_tensor(out=A[:, ai], in0=ps[:, 0:512].rearrange("p (h t) -> p h t", h=4),
                                                          in1=mskb[:, mi:mi + 1, :].to_broadcast([128, 4, CH]), op=ALU.mult),
                         reads=[tps, tp], writes=[tA5])
                yield
                F3 = lambda t_: t_[:].rearrange("p h t -> p (h t)")
                def mmg(lhs, tl, rhs, tr_):
                    ps_, tps_ = k.ps()
                    for h in range(4):
                        k.op("pe", lambda e: e.matmul(ps_[:, h * CH:(h + 1) * CH], lhs[:, h, :], rhs[:, h, :], start=True, stop=True),
                             reads=[tl, tr_], writes=[tps_])
                    return ps_, tps_
                def bm(i_):
                    return bmskb[:, i_:i_ + 1, :].to_broadcast([128, 4, CH])
                iv = ivs[s]
                N0, tN0 = iv["N0"]; N0T, tN0T = iv["N0T"]
                Xc, tXc = iv["Xa"]; XTc, tXTc = iv["XTa"]
                Xo, tXo = iv["Xb"]; XTo, tXTo = iv["XTb"]
                tt("pool", N0[:], A[:, 0], bm(0), ALU.mult, [tA5, tp], [tN0])
                tt("pool", N0T[:], A[:, 1], bm(0), ALU.mult, [tA5, tp], [tN0T])
                idb = S.identb[:, :].unsqueeze(1).to_broadcast([128, 4, CH])
                tt("dve", Xc[:], idb, N0[:], ALU.subtract, [tN0, S.t_const], [tXc])
                tt("dve", XTc[:], idb, N0T[:], ALU.subtract, [tN0T, S.t_const], [tXTc])
                Pc, tPc, PTc, tPTc = N0, tN0, N0T, tN0T
                for j in range(3):
                    Pn, tPn = iv["P%d" % (j % 2)]; PTn, tPTn = iv["PT%d" % (j % 2)]
                    ps_, tps_ = mmg(PTc, tPTc, Pc, tPc)
                    k.op("act", lambda e: e.copy(out=F3(Pn), in_=ps_[:, 0:512]), reads=[tps_], writes=[tPn])
                    ps_, tps_ = mmg(Pc, tPc, PTc, tPTc)
                    k.op("dve", lambda e: e.tensor_copy(out=F3(PTn), in_=ps_[:, 0:512]), reads=[tps_], writes=[tPTn])
                    ps_, tps_ = mmg(PTn, tPTn, Xc, tXc)
                    k.op("dve", lambda e: e.tensor_tensor(out=F3(Xo), in0=ps_[:, 0:512], in1=F3(Xc), op=ALU.add), reads=[tps_, tXc], writes=[tXo])
                    ps_, tps_ = mmg(Pn, tPn, XTc, tXTc)
                    k.op("dve", lambda e: e.tensor_tensor(out=F3(XTo), in0=ps_[:, 0:512], in1=F3(XTc), op=ALU.add), reads=[tps_, tXTc], writes=[tXTo])
                    Xc, tXc, Xo, tXo = Xo, tXo, Xc, tXc
                    XTc, tXTc, XTo, tXTo = XTo, tXTo, XTc, tXTc
                    Pc, tPc, PTc, tPTc = Pn, tPn, PTn, tPTn
                yield
                for lv in range(3):
                    Np, tNp = iv["P0"]; NpT, tNpT = iv["PT0"]
                    Wt, tWt = iv["W"]; Wt2, tWt2 = iv["W2"]
                    tt("pool", Np[:], A[:, 0], bm(1 + lv), ALU.mult, [tA5, tp], [tNp])
                    tt("pool", NpT[:], A[:, 1], bm(1 + lv), ALU.mult, [tA5, tp], [tNpT])
                    ps_, tps_ = mmg(NpT, tNpT, Xc, tXc)
                    k.op("act", lambda e: e.copy(out=F3(Wt), in_=ps_[:, 0:512]), reads=[tps_], writes=[tWt])
                    if lv < 2:
                        ps_, tps_ = mmg(Np, tNp, XTc, tXTc)
                        k.op("dve", lambda e: e.tensor_copy(out=F3(Wt2), in_=ps_[:, 0:512]), reads=[tps_], writes=[tWt2])
                    ps_, tps_ = mmg(XTc, tXTc, Wt, tWt)
                    k.op("dve", lambda e: e.tensor_tensor(out=F3(Xo), in0=F3(Xc), in1=ps_[:, 0:512], op=ALU.subtract), reads=[tps_, tXc], writes=[tXo])
                    if lv < 2:
                        ps_, tps_ = mmg(Xc, tXc, Wt2, tWt2)
                        k.op("dve", lambda e: e.tensor_tensor(out=F3(XTo), in0=F3(XTc), in1=ps_[:, 0:512], op=ALU.subtract),
                             reads=[tps_, tXTc], writes=[tXTo])
                        XTc, tXTc, XTo, tXTo = XTo, tXTo, XTc, tXTc
                    Xc, tXc, Xo, tXo = Xo, tXo, Xc, tXc
                    yield
                rhs2, trh = g(rhs2_s); mu, tmu = g(mu_s)
                ps, tps = k.ps()
                for h in range(4):
                    k.op("pe", lambda e: e.matmul(ps[:, h * 64:(h + 1) * 64], A[:, 2, h, :], vtb[:, h, :], start=True, stop=True),
                         reads=[tA5, tvtb], writes=[tps])
                k.op("dve", lambda e: e.tensor_scalar(out=rhs2[:, :, 64:128], in0=ps[:, 0:256].rearrange("p (h v) -> p h v", h=4), scalar1=-1.0,
                                                      scalar2=None, op0=ALU.mult), reads=[tps], writes=[trh])
                k.op("pool", lambda e: e.tensor_copy(out=rhs2[:, :, 0:64], in_=tm[:, 0]), reads=[ttm], writes=[trh])
                ps, tps = k.ps()
                for h in range(4):
                    k.op("pe", lambda e: e.matmul(ps[:, h * 128:(h + 1) * 128], Xc[:, h, :], rhs2[:, h, :], start=True, stop=True),
                         reads=[tXc, trh], writes=[tps])
                k.op("act", lambda e: e.copy(out=mu[:].rearrange("p h t -> p (h t)"), in_=ps[:, 0:512]), reads=[tps], writes=[tmu])
                yield
                GT, tGT = g(GT_s); J, tJ = g(J_s); RT, tRT = g(RT_s)
                ps, tps = k.ps()
                for h in range(4):
                    k.op("pe", lambda e: e.matmul(ps[0:64, h * 64:(h + 1) * 64], mu[:, h, 0:64], tm[:, 2, h, :], start=True, stop=True),
                         reads=[tmu, ttm], writes=[tps])
                for h in range(4):
                    k.op("dve", lambda e: e.scalar_tensor_tensor(out=GT[:, h, :], in0=S.ident[0:64, 0:64], scalar=Ep[:, h, iend:iend + 1],
                                                                 in1=ps[0:64, h * 64:(h + 1) * 64], op0=ALU.mult, op1=ALU.subtract),
                         reads=[tps, tEp, S.t_const], writes=[tGT])
                ps, tps = k.ps()
                for h in range(4):
                    k.op("pe", lambda e: e.matmul(ps[0:64, h * 64:(h + 1) * 64], tm[:, 1, h, :], vtb[:, h, :], start=True, stop=False),
                         reads=[ttm, tvtb], writes=[tps])
                    k.op("pe", lambda e: e.matmul(ps[0:64, h * 64:(h + 1) * 64], tm[:, 2, h, :], mu[:, h, 64:128], start=False, stop=True),
                         reads=[ttm, tmu], writes=[tps])
                k.op("act", lambda e: e.copy(out=J[:].rearrange("p h v -> p (h v)"), in_=ps[0:64, 0:256]), reads=[tps], writes=[tJ])
                Hc, tHc = Hs[hcur]
                Hn, tHn = Hs[1 - hcur]
                if emit:
                    ps, tps = k.ps()
                    for h in range(4):
                        k.op("pe", lambda e: e.matmul(ps[0:64, h * CH:(h + 1) * CH], mu[:, h, 0:64], A[:, 4, h, :], start=True, stop=True),
                             reads=[tmu, tA5], writes=[tps])
                    k.op("dve", lambda e: e.tensor_tensor(out=RT[:].rearrange("p h t -> p (h t)"), in0=rq[:].rearrange("p h t -> p (h t)"),
                                                          in1=ps[0:64, 0:512], op=ALU.subtract), reads=[tps, trq], writes=[tRT])
                    py, tpy = k.ps()
                    for h in range(4):
                        k.op("pe", lambda e: e.matmul(py[:, h * 64:(h + 1) * 64], A[:, 3, h, :], vtb[:, h, :], start=True, stop=False),
                             reads=[tA5, tvtb], writes=[tpy])
                        k.op("pe", lambda e: e.matmul(py[:, h * 64:(h + 1) * 64], A[:, 4, h, :], mu[:, h, 64:128], start=False, stop=False),
                             reads=[tA5, tmu], writes=[tpy])
                        k.op("pe", lambda e: e.matmul(py[:, h * 64:(h + 1) * 64], RT[:, h, :], Hc[:, h, :], start=False, stop=True),
                             reads=[tRT, tHc], writes=[tpy])
                    rk, trk = g(rk_s)
                    tt("pool", rk[:], r_, kdir[:], ALU.mult, [trkv, tkd_], [trk])
                    tt("pool", rk[:], rk[:], B3(rkp[:, :]), ALU.mult, [tp], [trk])
                    for h in range(4):
                        k.op("pe", lambda e: e.matmul(py[:, 256 + h:257 + h], rk[:, h, :], ones64[:, 0:1], start=True, stop=True),
                             reads=[trk, tp], writes=[tpy])
                    ysb, tys = g(ys_s)
                    if d == 0:
                        k.op("act", lambda e: e.copy(out=ysb[:, 0:260], in_=py[:, 0:260]), reads=[tpy], writes=[tys])
                        k.dma(S.YD[b, t0:t0 + CH, :], ysb[:, :], reads=[tys], writes=[S.t_YD[b]])
                    else:
                        yd0, tyd0 = g(yd0_s)
                        k.dma(yd0[:], S.YD[b, t0:t0 + CH, :], reads=[S.t_YD[b]], writes=[tyd0])
                        k.op("dve", lambda e: e.tensor_tensor(out=ysb[:, 0:260], in0=py[:, 0:260], in1=yd0[:, 0:260], op=ALU.add),
                             reads=[tpy, tyd0], writes=[tys])
                yield
                ph, tph = k.ps()
                for h in range(4):
                    k.op("pe", lambda e: e.matmul(ph[0:64, h * 64:(h + 1) * 64], GT[:, h, :], Hc[:, h, :], start=True, stop=True),
                         reads=[tGT, tHc], writes=[tph])
                k.op("dve", lambda e: e.tensor_tensor(out=Hn[:].rearrange("p h v -> p (h v)"), in0=ph[0:64, 0:256],
                                                      in1=J[:].rearrange("p h v -> p (h v)"), op=ALU.add), reads=[tph, tJ], writes=[tHn])
                hcur = 1 - hcur
                if not emit:
                    continue
                yield
                ysb, tys = g(ys_s)
                if d == 0:
                    continue
                yc, tyc = g(yc_s); y2, ty2 = g(y2_s); stt, tst = g(st_s); gts, tgts = g(gt_s)
                y3 = ysb[:, 0:256].rearrange("p (h v) -> p h v", h=4)
                k.op("dve", lambda e: e.tensor_reduce(out=stt[:, 0:4], in_=y3, axis=AX.X, op=ALU.add), reads=[tys], writes=[tst])
                k.op("dve", lambda e: e.tensor_scalar(out=stt[:, 0:4], in0=stt[:, 0:4], scalar1=1.0 / 64, scalar2=None, op0=ALU.mult),
                     reads=[tst], writes=[tst])
                k.op("dve", lambda e: e.tensor_tensor(out=yc[:], in0=y3, in1=stt[:, 0:4].unsqueeze(2).to_broadcast([128, 4, 64]),
                                                      op=ALU.subtract), reads=[tys, tst], writes=[tyc])
                k.op("pool", lambda e: e.tensor_tensor(out=y2[:], in0=yc[:], in1=yc[:], op=ALU.mult), reads=[tyc], writes=[ty2])
                k.op("dve", lambda e: e.tensor_reduce(out=stt[:, 4:8], in_=y2[:], axis=AX.X, op=ALU.add), reads=[ty2], writes=[tst])
                k.op("dve", lambda e: e.tensor_scalar(out=stt[:, 4:8], in0=stt[:, 4:8], scalar1=1.0 / 64, scalar2=GN_EPS, op0=ALU.mult,
                                                      op1=ALU.add), reads=[tst], writes=[tst])
                k.op("act", lambda e: e.activation(out=stt[:, 4:8], in_=stt[:, 4:8], func=AF.Sqrt), reads=[tst], writes=[tst])
                k.op("dve", lambda e: e.reciprocal(out=stt[:, 4:8], in_=stt[:, 4:8]), reads=[tst], writes=[tst])
                k.op("dve", lambda e: e.tensor_tensor(out=yc[:], in0=yc[:], in1=stt[:, 4:8].unsqueeze(2).to_broadcast([128, 4, 64]),
                                                      op=ALU.mult), reads=[tst], writes=[tyc])
                ycf = yc[:].rearrange("p h v -> p (h v)")
                k.op("pool", lambda e: e.tensor_tensor(out=ycf, in0=ycf, in1=gnw[:], op=ALU.mult), reads=[t_gn], writes=[tyc])
                k.op("pool", lambda e: e.tensor_tensor(out=ycf, in0=ycf, in1=gnb[:], op=ALU.add), reads=[t_gn], writes=[tyc])
                yield
                k.op("dve", lambda e: e.tensor_tensor(out=y2[:], in0=vtm[:], in1=ysb[:, 256:260].unsqueeze(2).to_broadcast([128, 4, 64]),
                                                      op=ALU.mult), reads=[tvtm, tys], writes=[ty2])
                k.op("dve", lambda e: e.tensor_tensor(out=yc[:], in0=yc[:], in1=y2[:], op=ALU.add), reads=[ty2], writes=[tyc])
                k.op("act", lambda e: e.activation(out=gl[:], in_=gl[:], func=AF.Sigmoid), reads=[tgl], writes=[tgl])
                pg, tpg = k.ps()
                k.op("pe", lambda e: e.matmul(pg[:, 0:256], gl[:, :], g2[:, :], start=True, stop=True), reads=[tgl, tp], writes=[tpg])
                k.op("dve", lambda e: e.tensor_tensor(out=gts[:], in0=pg[:, 0:256], in1=ycf, op=ALU.mult), reads=[tpg, tyc], writes=[tgts])
                ot, tot = g(ot_s)
                ps, tps = k.ps()
                for c2 in range(2):
                    k.op("pe", lambda e: e.transpose(out=ps[:, c2 * CH:(c2 + 1) * CH], in_=gts[:, c2 * 128:(c2 + 1) * 128], identity=S.ident[:, :]),
                         reads=[tgts, S.t_const], writes=[tps])
                k.op("act", lambda e: e.copy(out=ot[:].rearrange("p c t -> p (c t)"), in_=ps[:, 0:256]), reads=[tps], writes=[tot])
                k.dma(S.YT[b, 0:256, t0:t0 + CH].rearrange("(c p) t -> p c t", p=128), ot[:], reads=[tot], writes=[S.t_YT[b]])
    for d in range(2):
        gens = [chain(b, d) for b in range(NB)]
        for gi_, g_ in enumerate(gens[:-1]):
            for _ in range(RWKV_STAGGER * (len(gens) - 1 - gi_)):
                next(g_)
        while gens:
            for g_ in list(gens):
                try:
                    next(g_)
                except StopIteration:
                    gens.remove(g_)
    k.barrier()
    st.close()


def stage_moe2(S, l, last):
    k, cfg = S.k, S.cfg
    NB, C, L, T = cfg.NB, cfg.C, cfg.L, cfg.T
    nc = k.nc
    st = contextlib.ExitStack()
    blocks = [(b, t0) for b in range(NB) for t0 in range(C if last else 0, T, 128)]
    NBK = len(blocks)
    Tn = NBK * 128
    NBLK = (2 * Tn + EB - 1) // EB + NEXP
    PMAX = NBLK * EB
    MAXB = (Tn + EB - 1) // EB
    R = NB + 1
    tp = Tr()
    P = lambda name, shape, dt=F32: k.sbuf(st, name, shape, dt)
    t_ln = Tr()
    lng = load_bcast_row(S, st, "lng2", S.inp["ln2_g"][l:l + 1, :], t_ln)
    lnb = load_bcast_row(S, st, "lnb2", S.inp["ln2_b"][l:l + 1, :], t_ln)
    wr = P("wr", [128, 8, 36])
    k.dma(wr[:, :, 0:4], S.inp["router_group_w"][l].rearrange("(kc p) n -> p kc n", p=128), writes=[tp])
    k.dma(wr[:, :, 4:36], S.inp["router_expert_w"][l].rearrange("(kc p) n -> p kc n", p=128), writes=[tp])
    rbias = P("rbias", [1, 36])
    k.dma(rbias[0:1, 0:4], S.inp["router_group_b"][l:l + 1, :], writes=[tp])
    k.dma(rbias[0:1, 4:36], S.inp["router_expert_b"][l:l + 1, :], writes=[tp])
    ones = P("onesm", [128, 128])
    k.op("dve", lambda e: e.memset(ones[:], 1.0), reads=[tp], writes=[tp])
    tris = P("tris", [128, 128])
    k.dma(tris[:], S.inp["msk"][:, 0:128], writes=[tp])
    iop = P("iop", [128, 1])
    k.dma(iop[:], S.inp["iotap"][:, :], writes=[tp])
    w12 = P("w12", [128, NBK, 2]); t_w12 = Tr()
    dsti = P("dsti", [128, NBK, 2], I32)
    widi = P("widi", [128, NBLK], I32)
    st1 = contextlib.ExitStack()
    P1 = lambda name, shape, dt=F32: k.sbuf(st1, name, shape, dt)
    scb = P1("scb", [128, R, D]); shb = P1("shb", [128, R, D]); dm = [(P1("dm", [128, 128]), Tr()) for _ in range(2)]
    di = 0
    for (dst, base) in ((scb, 32), (shb, 24)):
        for r in range(R):
            for half in range(2):
                ps, tps = k.ps()
                for f4 in range(4):
                    fc = half * 4 + f4
                    dmt, tdm = dm[di % 2]
                    di += 1
                    k.op("dve", lambda e: e.tensor_scalar(out=dmt[:], in0=S.ident[:, :], scalar1=S.mod[:, base + fc, r:r + 1],
                                                          scalar2=None, op0=ALU.mult), reads=[S.t_mod, S.t_const], writes=[tdm])
                    k.op("pe", lambda e: e.matmul(ps[:, f4 * 128:(f4 + 1) * 128], ones[:, :], dmt[:, :], start=True, stop=True),
                         reads=[tdm, tp], writes=[tps])
                k.op("act", lambda e: e.copy(out=dst[:, r, half * 512:(half + 1) * 512], in_=ps[:, 0:512]), reads=[tps], writes=[tp])
    OH = P1("OH", [128, NBK, 2, 32]); t_OH = Tr()
    x1s = [(P1("x1", [128, D]), Tr()) for _ in range(2)]
    hfs = [(P1("hf", [128, 8, 128]), Tr()) for _ in range(2)]
    hts = [(P1("ht", [128, D]), Tr()) for _ in range(2)]
    htb = [(k.sbuf(st1, "htb", [128, D], BF16), Tr()) for _ in range(2)]
    lga = P1("lga", [128, NBK, 36]); t_lga = Tr()
    it = 0
    for j, (b, t0) in enumerate(blocks):
        r = NB if t0 < C else b
        x1, tx1 = x1s[it % 2]; hf, thf = hfs[it % 2]; ht, tht = hts[it % 2]; hb_, thb = htb[it % 2]
        it += 1
        k.dma(x1[:], S.XR[b, t0:t0 + 128, :], reads=[S.t_XR[b]], writes=[tx1])
        k.op("pool", lambda e: e.tensor_tensor(out=ht[:], in0=x1[:], in1=scb[:, r, :], op=ALU.mult), reads=[tx1, tp], writes=[tht])
        k.op("pool", lambda e: e.tensor_tensor(out=hb_[:], in0=ht[:], in1=shb[:, r, :], op=ALU.add), reads=[tht, tp], writes=[thb])
        k.dma(S.HS[j * 128:(j + 1) * 128, :], hb_[:], reads=[thb], writes=[S.t_HS])
        for hh in range(2):
            ps, tps = k.ps()
            for f4 in range(4):
                fc = hh * 4 + f4
                k.op("pe", lambda e: e.transpose(out=ps[:, f4 * 128:(f4 + 1) * 128], in_=x1[:, fc * 128:(fc + 1) * 128],
                                                 identity=S.ident[:, :]), reads=[tx1, S.t_const], writes=[tps])
            for f4 in range(4):
                fc = hh * 4 + f4
                k.op("act", lambda e: e.activation(out=hf[:, fc, :], in_=ps[:, f4 * 128:(f4 + 1) * 128], func=AF.Identity,
                                                   scale=S.mod[:, 32 + fc, r:r + 1], bias=S.mod[:, 24 + fc, r:r + 1]),
                     reads=[tps, S.t_mod], writes=[thf])
        ps, tps = k.ps()
        for fc in range(8):
            k.op("pe", lambda e: e.matmul(ps[:, 0:36], hf[:, fc, :], wr[:, fc, :], start=(fc == 0), stop=False),
                 reads=[thf, tp], writes=[tps])
        k.op("pe", lambda e: e.matmul(ps[:, 0:36], ones[0:1, :], rbias[0:1, :], start=False, stop=True), reads=[tp], writes=[tps])
        k.op("act", lambda e: e.copy(out=lga[:, j, :], in_=ps[:, 0:36]), reads=[tps], writes=[t_lga])
    RB = lambda name, n: P1(name, [128, NBK, n])
    gmx = RB("gmx", 1); gm = RB("gm", 4); ge = RB("ge", 4); gpr = RB("gpr", 1); elm = RB("elm", 32); els = RB("els", 8)
    m1 = RB("m1", 1); mk1 = RB("mk1", 8); el2 = RB("el2", 8); m2 = RB("m2", 1); mk2 = RB("mk2", 8); dd = RB("dd", 1); w1 = RB("w1", 1)
    t_r = Tr()
    rv = lambda fn, rd=(), wrs=(), en="dve": k.op(en, fn, reads=[t_r, t_lga] + list(rd), writes=[t_r] + list(wrs))
    BC = lambda ap, n: ap.to_broadcast([128, NBK, n])
    rv(lambda e: e.tensor_reduce(out=gmx[:, :, 0], in_=lga[:, :, 0:4], axis=AX.X, op=ALU.max))
    rv(lambda e: e.tensor_tensor(out=gm[:], in0=lga[:, :, 0:4], in1=BC(gmx[:, :, 0:1], 4), op=ALU.is_equal))
    rv(lambda e: e.tensor_tensor(out=ge[:], in0=lga[:, :, 0:4], in1=BC(gmx[:, :, 0:1], 4), op=ALU.subtract))
    rv(lambda e: e.activation(out=ge[:], in_=ge[:], func=AF.Exp), en="act")
    rv(lambda e: e.tensor_reduce(out=gpr[:, :, 0], in_=ge[:], axis=AX.X, op=ALU.add))
    rv(lambda e: e.reciprocal(out=gpr[:], in_=gpr[:]))
    for g_ in range(4):
        rv(lambda e: e.tensor_tensor(out=elm[:, :, g_ * 8:(g_ + 1) * 8], in0=lga[:, :, 4 + g_ * 8:12 + g_ * 8], in1=BC(gm[:, :, g_:g_ + 1], 8),
                                     op=ALU.mult))
    rv(lambda e: e.tensor_tensor(out=els[:], in0=elm[:, :, 0:8], in1=elm[:, :, 8:16], op=ALU.add))
    rv(lambda e: e.tensor_tensor(out=els[:], in0=els[:], in1=elm[:, :, 16:24], op=ALU.add))
    rv(lambda e: e.tensor_tensor(out=els[:], in0=els[:], in1=elm[:, :, 24:32], op=ALU.add))
    rv(lambda e: e.tensor_reduce(out=m1[:, :, 0], in_=els[:], axis=AX.X, op=ALU.max))
    rv(lambda e: e.tensor_tensor(out=mk1[:], in0=els[:], in1=BC(m1[:, :, 0:1], 8), op=ALU.is_equal))
    rv(lambda e: e.scalar_tensor_tensor(out=el2[:], in0=mk1[:], scalar=-1e30, in1=els[:], op0=ALU.mult, op1=ALU.add))
    rv(lambda e: e.tensor_reduce(out=m2[:, :, 0], in_=el2[:], axis=AX.X, op=ALU.max))
    rv(lambda e: e.tensor_tensor(out=mk2[:], in0=el2[:], in1=BC(m2[:, :, 0:1], 8), op=ALU.is_equal))
    rv(lambda e: e.tensor_tensor(out=dd[:], in0=m2[:], in1=m1[:], op=ALU.subtract))
    rv(lambda e: e.activation(out=dd[:], in_=dd[:], func=AF.Exp), en="act")
    rv(lambda e: e.tensor_scalar(out=w1[:], in0=dd[:], scalar1=1.0, scalar2=None, op0=ALU.add))
    rv(lambda e: e.reciprocal(out=w1[:], in_=w1[:]))
    rv(lambda e: e.tensor_tensor(out=dd[:], in0=dd[:], in1=w1[:], op=ALU.mult))
    rv(lambda e: e.tensor_tensor(out=w12[:, :, 0:1], in0=w1[:], in1=gpr[:], op=ALU.mult), wrs=[t_w12])
    rv(lambda e: e.tensor_tensor(out=w12[:, :, 1:2], in0=dd[:], in1=gpr[:], op=ALU.mult), wrs=[t_w12])
    for kk_, mk in ((0, mk1), (1, mk2)):
        for g_ in range(4):
            rv(lambda e: e.tensor_tensor(out=OH[:, :, kk_, g_ * 8:(g_ + 1) * 8], in0=mk[:], in1=BC(gm[:, :, g_:g_ + 1], 8), op=ALU.mult),
               wrs=[t_OH])
    OHs = P1("OHs", [128, NBK, 32]); t_OHs = Tr()
    k.op("dve", lambda e: e.tensor_tensor(out=OHs[:], in0=OH[:, :, 0, :], in1=OH[:, :, 1, :], op=ALU.add), reads=[t_OH], writes=[t_OHs])
    pref = P1("pref", [128, NBK, 32]); t_pref = Tr()
    for j in range(NBK):
        ps, tps = k.ps()
        k.op("pe", lambda e: e.matmul(ps[:, 0:32], tris[:, :], OHs[:, j, :], start=True, stop=(j == 0)), reads=[t_OHs, tp], writes=[tps])
        for j2 in range(j):
            k.op("pe", lambda e: e.matmul(ps[:, 0:32], ones[:, :], OHs[:, j2, :], start=False, stop=(j2 == j - 1)),
                 reads=[t_OHs, tp], writes=[tps])
        k.op("act", lambda e: e.copy(out=pref[:, j, :], in_=ps[:, 0:32]), reads=[tps], writes=[t_pref])
    cst = P1("cst", [128, 8, 32]); t_c = Tr()
    ps, tps = k.ps()
    for j in range(NBK):
        k.op("pe", lambda e: e.matmul(ps[:, 0:32], ones[:, :], OHs[:, j, :], start=(j == 0), stop=(j == NBK - 1)),
             reads=[t_OHs, tp], writes=[tps])
    cd = lambda fn, rd=(): k.op("dve", fn, reads=[t_c] + list(rd), writes=[t_c])
    cd(lambda e: e.tensor_copy(out=cst[:, 0, :], in_=ps[:, 0:32]), rd=[tps])
    cd(lambda e: e.memset(cst[:, 1, :], 0.0))
    cd(lambda e: e.memset(cst[:, 6, :], 1.0))
    for m in range(MAXB):
        cd(lambda e: e.tensor_scalar(out=cst[:, 5, :], in0=cst[:, 0, :], scalar1=float(m * EB), scalar2=None, op0=ALU.is_gt))
        cd(lambda e: e.tensor_tensor(out=cst[:, 1, :], in0=cst[:, 1, :], in1=cst[:, 5, :], op=ALU.add))
    cd(lambda e: e.tensor_scalar(out=cst[:, 2, :], in0=cst[:, 1, :], scalar1=float(EB), scalar2=None, op0=ALU.mult))
    cd(lambda e: e.tensor_tensor_scan(out=cst[:, 3, :], data0=cst[:, 6, :], data1=cst[:, 2, :], initial=0.0, op0=ALU.mult, op1=ALU.add))
    cd(lambda e: e.tensor_tensor(out=cst[:, 4, :], in0=cst[:, 3, :], in1=cst[:, 2, :], op=ALU.subtract))
    k.op("dve", lambda e: e.tensor_tensor(out=pref[:], in0=pref[:], in1=cst[:, 4:5, :].to_broadcast([128, NBK, 32]), op=ALU.add),
         reads=[t_c], writes=[t_pref])
    dstf = P1("dstf", [128, NBK, 2]); t_d = Tr()
    tmpo = P1("tmpo", [128, NBK, 32]); t_to = Tr()
    for kk_ in range(2):
        k.op("dve", lambda e: e.tensor_tensor(out=tmpo[:], in0=OH[:, :, kk_, :], in1=pref[:], op=ALU.mult), reads=[t_OH, t_pref], writes=[t_to])
        k.op("dve", lambda e: e.tensor_reduce(out=dstf[:, :, kk_], in_=tmpo[:], axis=AX.X, op=ALU.add), reads=[t_to], writes=[t_d])
    k.op("dve", lambda e: e.tensor_copy(out=dsti[:], in_=dstf[:]), reads=[t_d], writes=[t_d])
    bexp = P1("bexp", [128, NBLK]); t_be = Tr()
    for bk in range(NBLK):
        cd(lambda e: e.tensor_scalar(out=cst[:, 5, :], in0=cst[:, 3, :], scalar1=float(bk * EB), scalar2=None, op0=ALU.is_le))
        k.op("dve", lambda e: e.reduce_sum(out=bexp[:, bk:bk + 1], in_=cst[:, 5, :], axis=AX.X), reads=[t_c], writes=[t_be])
    k.op("dve", lambda e: e.tensor_scalar(out=bexp[:], in0=bexp[:], scalar1=float(NEXP - 1), scalar2=None, op0=ALU.min), reads=[t_be], writes=[t_be])
    widf = P1("widf", [128, NBLK]); t_wi = Tr()
    k.op("dve", lambda e: e.tensor_scalar(out=widf[:], in0=bexp[:], scalar1=128.0, scalar2=float(l * NEXP * 128), op0=ALU.mult, op1=ALU.add),
         reads=[t_be], writes=[t_wi])
    k.op("dve", lambda e: e.tensor_tensor(out=widf[:], in0=widf[:], in1=iop[:, 0:1].to_broadcast([128, NBLK]), op=ALU.add),
         reads=[tp], writes=[t_wi])
    k.op("dve", lambda e: e.tensor_copy(out=widi[:], in_=widf[:]), reads=[t_wi], writes=[t_wi])
    hbs = [(k.sbuf(st1, "hb2", [128, D], BF16), Tr()) for _ in range(2)]
    for j in range(NBK):
        hb_, thb = hbs[j % 2]
        k.dma(hb_[:], S.HS[j * 128:(j + 1) * 128, :], reads=[S.t_HS], writes=[thb])
        for kk_ in range(2):
            k.idma(S.HSORT[:, :], hb_[:], out_off=dsti[:, j, kk_:kk_ + 1], bound=PMAX - 1, reads=[thb, t_d], writes=[S.t_HSORT])
    k.barrier()
    st1.close()
    if getattr(cfg, "moe_stop", 9) < 2:
        st.close()
        return
    st2 = contextlib.ExitStack()
    P2 = lambda name, shape, dt=F32: k.sbuf(st2, name, shape, dt)
    wgs = [(P2("wg", [128, 8, DE], BF16), P2("wu", [128, 8, DE], BF16), P2("wd", [128, 4, D], BF16), Tr()) for _ in range(2)]
    htk = [(P2("htk", [128, 4, D], BF16), Tr()) for _ in range(2)]
    hTs = [(P2("hT", [128, 8, EB], BF16), Tr()) for _ in range(2)]
    sgs = [(P2("sg", [128, 512]), Tr()) for _ in range(2)]
    aTs = [(P2("aT", [128, 4, 512], BF16), Tr()) for _ in range(2)]
    ybs = [(P2("yb", [128, 4, D], BF16), Tr()) for _ in range(2)]
    for bk in range(NBLK):
        wg, wu, wd, twe = wgs[bk % 2]
        bnd = DEPTH * NEXP * 128 - 1
        for (dst, nm, hc) in ((wg, "ewg", 4), (wu, "ewu", 4), (wd, "ewd", 2)):
            k.idma(dst[:, 0:hc, :].rearrange("p a b -> p (a b)"), S.inp[nm + "_a"][:, :], in_off=widi[:, bk:bk + 1], bound=bnd,
                   reads=[t_wi], writes=[twe])
            k.idma(dst[:, hc:2 * hc, :].rearrange("p a b -> p (a b)"), S.inp[nm + "_b"][:, :], in_off=widi[:, bk:bk + 1], bound=bnd,
                   reads=[t_wi], writes=[twe])
        hk, thk = htk[bk % 2]
        hT, thT = hTs[bk % 2]
        k.dma(hk[:], S.HSORT[bk * EB:(bk + 1) * EB, :].rearrange("(n p) d -> p n d", p=128), reads=[S.t_HSORT], writes=[thk])
        for fc in range(8):
            ps, tps = k.ps()
            psb = ps[:].bitcast(BF16)
            for n in range(4):
                k.op("pe", lambda e: e.transpose(out=psb[:, n * 128:(n + 1) * 128], in_=hk[:, n, fc * 128:(fc + 1) * 128],
                                                 identity=S.identb[:, :]), reads=[thk, S.t_const], writes=[tps])
            k.op("act" if fc % 2 else "dve", (lambda e: e.copy(out=hT[:, fc, :], in_=psb[:, 0:512])) if fc % 2 else
                 (lambda e: e.tensor_copy(out=hT[:, fc, :], in_=psb[:, 0:512])), reads=[tps], writes=[thT])
        aT, taT = aTs[bk % 2]
        for f in range(4):
            pg, tpg = k.ps()
            pu, tpu = k.ps()
            for kc in range(8):
                k.op("pe", lambda e: e.matmul(pg[:, 0:EB], wg[:, kc, f * 128:(f + 1) * 128], hT[:, kc, :], start=(kc == 0), stop=(kc == 7)),
                     reads=[twe, thT], writes=[tpg])
            for kc in range(8):
                k.op("pe", lambda e: e.matmul(pu[:, 0:EB], wu[:, kc, f * 128:(f + 1) * 128], hT[:, kc, :], start=(kc == 0), stop=(kc == 7)),
                     reads=[twe, thT], writes=[tpu])
            sg, tsg = sgs[f % 2]
            k.op("act", lambda e: e.activation(out=sg[:], in_=pg[:, 0:EB], func=AF.Silu), reads=[tpg], writes=[tsg])
            k.op("dve", lambda e: e.tensor_tensor(out=aT[:, f, :], in0=pu[:, 0:EB], in1=sg[:], op=ALU.mult), reads=[tpu, tsg], writes=[taT])
        yb, tyb = ybs[bk % 2]
        for jb in range(4):
            for h in range(2):
                py, tpy = k.ps()
                for f in range(4):
                    k.op("pe", lambda e: e.matmul(py[:, 0:512], aT[:, f, jb * 128:(jb + 1) * 128], wd[:, f, h * 512:(h + 1) * 512],
                                                  start=(f == 0), stop=(f == 3)), reads=[taT, twe], writes=[tpy])
                k.op("act" if h else "dve", (lambda e: e.copy(out=yb[:, jb, h * 512:(h + 1) * 512], in_=py[:, 0:512])) if h else
                     (lambda e: e.tensor_copy(out=yb[:, jb, h * 512:(h + 1) * 512], in_=py[:, 0:512])), reads=[tpy], writes=[tyb])
        k.dma(S.YB[bk * EB:(bk + 1) * EB, :].rearrange("(n p) d -> p n d", p=128), yb[:], reads=[tyb], writes=[S.t_YB])
    k.barrier()
    st2.close()
    if getattr(cfg, "moe_stop", 9) < 3:
        st.close()
        return
    NB3 = 4
    x1s = [(P("x1c", [128, D]), Tr()) for _ in range(NB3)]
    g1s = [(k.sbuf(st, "g1", [128, D], BF16), k.sbuf(st, "g2_", [128, D], BF16), Tr()) for _ in range(NB3)]
    zs = [(P("z2", [128, D]), Tr()) for _ in range(NB3)]
    sms = [(P("sm2", [128, 32]), Tr()) for _ in range(NB3)]
    for j, (b, t0) in enumerate(blocks):
        r = NB if t0 < C else b
        x1, tx1 = x1s[j % NB3]; ga, gb, tg = g1s[j % NB3]; z, tz = zs[j % NB3]; sm, tsm = sms[j % NB3]
        k.dma(x1[:], S.XR[b, t0:t0 + 128, :], reads=[S.t_XR[b]], writes=[tx1])
        k.idma(ga[:], S.YB[:, :], in_off=dsti[:, j, 0:1], bound=PMAX - 1, reads=[S.t_YB, t_d], writes=[tg])
        k.idma(gb[:], S.YB[:, :], in_off=dsti[:, j, 1:2], bound=PMAX - 1, reads=[S.t_YB, t_d], writes=[tg])
        k.op("dve", lambda e: e.tensor_scalar(out=z[:], in0=ga[:], scalar1=w12[:, j, 0:1], scalar2=None, op0=ALU.mult),
             reads=[tg, t_w12], writes=[tz])
        k.op("dve", lambda e: e.scalar_tensor_tensor(out=z[:], in0=gb[:], scalar=w12[:, j, 1:2], in1=z[:], op0=ALU.mult, op1=ALU.add),
             reads=[tg, t_w12], writes=[tz])
        k.op("pool", lambda e: e.tensor_tensor(out=z[:], in0=z[:], in1=S.gateb[:, 1, r, :], op=ALU.mult), reads=[S.t_gateb], writes=[tz])
        k.op("dve", lambda e: e.scalar_tensor_tensor(out=z[:], in0=x1[:], scalar=ALPHA, in1=z[:], op0=ALU.mult, op1=ALU.add),
             reads=[tx1], writes=[tz])
        ln_block(S, z, tz, lng, lnb, t_ln, sm, tsm)
        if last:
            k.dma(S.out[b, t0 - C:t0 - C + 128, :], z[:], reads=[tz], writes=[S.t_out])
        else:
            k.dma(S.XR[b, t0:t0 + 128, :], z[:], reads=[tz], writes=[S.t_XR[b]])
    k.barrier()
    st.close()
```

```python
import contextlib
import math
import numpy as np
import concourse.bass as bass
import concourse.mybir as mybir
from concourse.bass_utils import run_bass_kernel_spmd

F32 = mybir.dt.float32
BF16 = mybir.dt.bfloat16
AF = mybir.ActivationFunctionType
ALU = mybir.AluOpType
AX = mybir.AxisListType

D = 1024
DEPTH = 2
HD = 64
RW = 256
NH = 4
S5W = 256
S5G = 16
S5P = 64
S5C = 16
AW = 512
AH = 8
AKV = 2
AG = 4
DIN = 2048
NEXP = 32
DE = 512
ALPHA = (2.0 * DEPTH) ** 0.25
LN_EPS = 1e-5
GN_EPS = 64e-5
GRID_W = 64
SEM_LIMIT = 20000
RWKV_STAGGER = 0
I32 = mybir.dt.int32
EB = 512


class Tr:
    __slots__ = ("w", "r")

    def __init__(self):
        self.w = None
        self.r = {}


class KB:
    def __init__(self, nc):
        self.nc = nc
        self.es = contextlib.ExitStack()
        self.eng = {"pe": nc.tensor, "dve": nc.vector, "act": nc.scalar, "pool": nc.gpsimd, "sp": nc.sync}
        self.sems = {}
        self.cnt = {}
        self.phase = {k: 0 for k in self.eng}
        self.waited = {k: {} for k in self.eng}
        self.ndq = 8
        self.dq_next = {}
        self.n_inst = 0
        self._uid = 0
        self.psring = []
        self.nw = {}
        self.bregs = {}
        self.ps_i = 0

    def sbuf(self, st, name, shape, dt=F32):
        self._uid += 1
        return st.enter_context(self.nc.sbuf_tensor("%s_%d" % (name, self._uid), list(shape), dt))

    def dram(self, name, shape, dt=F32, kind="Internal"):
        return self.nc.dram_tensor(name, list(shape), dt, kind=kind).ap()

    def init_psum(self):
        for i in range(8):
            t = self.es.enter_context(self.nc.psum_tensor("psr%d" % i, [128, 512], F32))
            self.psring.append((t, Tr()))

    def ps(self):
        t = self.psring[self.ps_i]
        self.ps_i = (self.ps_i + 1) % 8
        return t

    def _sem(self, key):
        if key not in self.sems:
            self._uid += 1
            self.sems[key] = self.es.enter_context(self.nc.semaphore("s%d" % self._uid))
            self.cnt[key] = 0
        return self.sems[key]

    def _cur_key(self, e):
        key = (e, self.phase[e])
        self._sem(key)
        if self.cnt[key] >= SEM_LIMIT:
            self.phase[e] += 1
            key = (e, self.phase[e])
            self._sem(key)
        return key

    def _wait(self, e, deps):
        best = {}
        for d in deps:
            if d is None:
                continue
            key, n = d
            if best.get(key, 0) < n:
                best[key] = n
        for key, n in best.items():
            if key[0] == e and e == "pe":
                continue
            if self.waited[e].get(key, 0) >= n:
                continue
            self.eng[e].wait_ge(self._sem(key), n)
            self.waited[e][key] = n

    @staticmethod
    def _deps(reads, writes):
        deps = []
        for t in reads:
            deps.append(t.w)
        for t in writes:
            deps.append(t.w)
            for kk, n in t.r.items():
                deps.append((kk, n))
        return deps

    @staticmethod
    def _mark(reads, writes, me):
        key, n = me
        for t in reads:
            t.r[key] = n
        for t in writes:
            t.w = me
            t.r = {}

    def op(self, e, fn, reads=(), writes=()):
        self._wait(e, self._deps(reads, writes))
        key = self._cur_key(e)
        ins = fn(self.eng[e])
        self.nw[e] = 0
        ins.then_inc(self.sems[key], 1)
        self.cnt[key] += 1
        me = (key, self.cnt[key])
        self._mark(reads, writes, me)
        self.n_inst += 1
        return me

    def dma(self, out, in_, reads=(), writes=(), q="sp", **kw):
        i = self.dq_next.get(q, 0)
        self.dq_next[q] = (i + 1) % self.ndq
        key = ("dq" + q, i)
        self._sem(key)
        deps = self._deps(reads, writes)
        if self.cnt[key] > 0:
            deps.append((key, self.cnt[key]))
        self._wait(q, deps)
        ins = self.eng[q].dma_start(out=out, in_=in_, **kw)
        self.nw[q] = 0
        ins.then_inc(self.sems[key], 16)
        self.cnt[key] += 16
        me = (key, self.cnt[key])
        self._mark(reads, writes, me)
        self.n_inst += 1
        return me

    def idma(self, out, in_, in_off=None, out_off=None, bound=0, reads=(), writes=()):
        q = "pool"
        i = self.dq_next.get("ind", 0)
        self.dq_next["ind"] = (i + 1) % self.ndq
        key = ("dqind", i)
        self._sem(key)
        deps = self._deps(reads, writes)
        if self.cnt[key] > 0:
            deps.append((key, self.cnt[key]))
        self._wait(q, deps)
        if bound not in self.bregs:
            rg = self.nc.gpsimd.alloc_register("bnd%d" % len(self.bregs))
            self.nc.gpsimd.reg_mov(rg, int(bound))
            self.bregs[bound] = rg
        ins = self.nc.gpsimd.indirect_dma_start(
            out=out, out_offset=(bass.IndirectOffsetOnAxis(ap=out_off, axis=0) if out_off is not None else None),
            in_=in_, in_offset=(bass.IndirectOffsetOnAxis(ap=in_off, axis=0) if in_off is not None else None),
            bounds_check=self.bregs[bound], oob_is_err=False)
        self.nw[q] = 0
        ins.then_inc(self.sems[key], 16)
        self.cnt[key] += 16
        me = (key, self.cnt[key])
        self._mark(reads, writes, me)
        self.n_inst += 1
        return me

    def barrier(self):
        allk = [(key, c) for key, c in self.cnt.items() if c > 0]
        for e in self.eng:
            self._wait(e, allk)

    def close(self):
        self.es.close()


class Cfg:
    def __init__(self, NB=2, C=256, L=2048, layers=(0, 1), dbg=False, stages=None):
        self.NB, self.C, self.L = NB, C, L
        self.T = C + L
        self.layers = layers
        self.dbg = dbg
        self.stages = stages


def tiles(t0, t1, w):
    out = []
    t = t0
    while t < t1:
        ww = min(w, t1 - t)
        out.append((t, ww))
        t += ww
    return out


def host_consts(cfg):
    L = cfg.L
    c = {}
    c["ident"] = np.eye(128, dtype=np.float32)
    rows = L // GRID_W
    rid, cid = np.meshgrid(np.arange(rows, dtype=np.float32), np.arange(GRID_W, dtype=np.float32), indexing="ij")
    nf = HD // 4
    inv = (10000.0 ** (-np.arange(nf, dtype=np.float32) / nf)).astype(np.float32)
    ang = np.concatenate([rid.reshape(-1, 1) * inv, cid.reshape(-1, 1) * inv], axis=-1).astype(np.float32)
    cos = np.cos(ang).astype(np.float32).T
    sin = np.sin(ang).astype(np.float32).T
    c["rope_cos"] = np.ascontiguousarray(np.concatenate([cos, cos, cos, cos], axis=0))
    c["rope_sin"] = np.ascontiguousarray(np.concatenate([-sin, sin, -sin, sin], axis=0))
    sel = np.zeros((3, 3, 128), np.float32)
    for r in range(3):
        sel[r, r, :] = 1.0
    c["sel3"] = sel.reshape(3, 3 * 128)
    s = np.arange(128)[:, None]
    t = np.arange(128)[None, :]
    cw = -math.exp(-0.5)
    c["tri"] = np.stack([
        np.where(s <= t, cw, 0.0), np.where(s < t, cw, 0.0),
        np.where(s >= t, cw, 0.0), np.where(s > t, cw, 0.0),
    ]).astype(np.float32).transpose(1, 0, 2).reshape(128, 4 * 128)
    c["msk"] = np.stack([s < t, s <= t, s > t, s >= t]).astype(np.float32).transpose(1, 0, 2).reshape(128, 4 * 128)
    c["iotap"] = np.arange(128, dtype=np.float32).reshape(128, 1)
    c["blkoff"] = np.ascontiguousarray(np.broadcast_to((np.arange(128, dtype=np.float32) * EB)[None, :], (128, 128)))
    bi = np.arange(128)
    bmk = lambda B: (bi[:, None] // B == bi[None, :] // B).astype(np.float32)
    c["bmsk"] = np.stack([bmk(16), bmk(32) - bmk(16), bmk(64) - bmk(32), bmk(128) - bmk(64)]).transpose(1, 0, 2).reshape(128, 512)
    return c


CONST_SHAPES = lambda cfg: {
    "ident": [128, 128], "rope_cos": [128, cfg.L], "rope_sin": [128, cfg.L], "sel3": [3, 384],
    "tri": [128, 512], "msk": [128, 512], "bmsk": [128, 512], "iotap": [128, 1], "blkoff": [128, 128],
}

PARAM_SHAPES = {
    "w_mod": [DEPTH, D, 6 * D], "b_mod": [DEPTH, 6 * D], "w_in": [DEPTH, D, DIN], "rwkv_conv": [DEPTH, 3, 1024],
    "rwkv_w0": [DEPTH, 2, RW], "rwkv_w2": [DEPTH, 2, 64, RW], "rwkv_a0": [DEPTH, 2, RW], "rwkv_a2": [DEPTH, 2, 64, RW],
    "rwkv_g2": [DEPTH, 128, RW], "rwkv_k_k": [DEPTH, RW], "rwkv_k_a": [DEPTH, RW], "rwkv_r_k": [DEPTH, NH, HD],
    "rwkv_gn_w": [DEPTH, RW], "rwkv_gn_b": [DEPTH, RW],
    "s5_lam_re": [DEPTH, 2, S5G, S5P], "s5_lam_im": [DEPTH, 2, S5G, S5P], "s5_log_dt": [DEPTH, 2, S5G],
    "s5_b_re": [DEPTH, S5G, S5P, S5C], "s5_b_im": [DEPTH, S5G, S5P, S5C], "s5_c_re": [DEPTH, S5G, S5C, S5P],
    "s5_c_im": [DEPTH, S5G, S5C, S5P], "s5_d": [DEPTH, S5W], "s5_glu_w": [DEPTH, S5W, S5W], "s5_glu_b": [DEPTH, S5W],
    "attn_sink": [DEPTH, AH], "w_out": [DEPTH, D, D], "ln1_g": [DEPTH, D], "ln1_b": [DEPTH, D], "ln2_g": [DEPTH, D],
    "ln2_b": [DEPTH, D], "router_group_w": [DEPTH, D, 4], "router_group_b": [DEPTH, 4],
    "router_expert_w": [DEPTH, D, NEXP], "router_expert_b": [DEPTH, NEXP],
    "ewg_a": [DEPTH * NEXP * 128, 2048], "ewg_b": [DEPTH * NEXP * 128, 2048], "ewu_a": [DEPTH * NEXP * 128, 2048],
    "ewu_b": [DEPTH * NEXP * 128, 2048], "ewd_a": [DEPTH * NEXP * 128, 2048], "ewd_b": [DEPTH * NEXP * 128, 2048],
}


class State:
    pass


def R3(ap, pat, **kw):
    return ap.rearrange(pat, **kw)


def stage_mod(S, l):
    k, cfg = S.k, S.cfg
    R = cfg.NB + 1
    st = contextlib.ExitStack()
    cc = k.sbuf(st, "cc", [R, D]); t_cc = Tr()
    k.dma(cc[:], S.inp["cc"][:, :], writes=[t_cc])
    k.op("act", lambda e: e.activation(out=cc[:], in_=cc[:], func=AF.Silu), reads=[t_cc], writes=[t_cc])
    siluT = k.sbuf(st, "siluT", [128, 8, R]); t_sT = Tr()
    ps, tps = k.ps()
    for kc in range(8):
        k.op("pe", lambda e: e.transpose(out=ps[:, kc * R:(kc + 1) * R], in_=cc[:, kc * 128:(kc + 1) * 128],
                                         identity=S.ident[0:R, 0:R]), reads=[t_cc, S.t_const], writes=[tps])
    k.op("dve", lambda e: e.tensor_copy(out=siluT[:].rearrange("p a b -> p (a b)"), in_=ps[:, 0:8 * R]),
         reads=[tps], writes=[t_sT])
    bmod = k.sbuf(st, "bmod", [1, 6 * D]); t_bm = Tr()
    k.dma(bmod[:], S.inp["b_mod"][l:l + 1, :], writes=[t_bm])
    ones = k.sbuf(st, "ones", [1, 128]); t_on = Tr()
    k.op("dve", lambda e: e.memset(ones[:], 1.0), writes=[t_on])
    gaterow = k.sbuf(st, "gaterow", [R, 2, D]); t_gr = Tr()
    wms = [(k.sbuf(st, "wm", [128, 8, 1024]), Tr()) for _ in range(2)]
    psm, tpsm = k.ps()
    wsrc = S.inp["w_mod"]
    for g in range(6):
        wm, twm = wms[g % 2]
        for kc in range(8):
            k.dma(wm[:, kc, :], wsrc[l, kc * 128:(kc + 1) * 128, g * 1024:(g + 1) * 1024], writes=[twm],
                  q=("sp" if kc % 2 == 0 else "act"))
        for oc in range(8):
            col = (g * 8 + oc) * R
            for kc in range(8):
                k.op("pe", lambda e: e.matmul(psm[:, col:col + R], wm[:, kc, oc * 128:(oc + 1) * 128], siluT[:, kc, :],
                                              start=(kc == 0), stop=False), reads=[twm, t_sT], writes=[tpsm])
            k.op("pe", lambda e: e.matmul(psm[:, col:col + R], bmod[0:1, (g * 8 + oc) * 128:(g * 8 + oc + 1) * 128],
                                          ones[0:1, 0:R], start=False, stop=True), reads=[t_bm, t_on], writes=[tpsm])
        if g in (2, 5):
            gi = 0 if g == 2 else 1
            for half in range(2):
                pg, tpg = k.ps()
                for kc in range(8):
                    k.op("pe", lambda e: e.matmul(pg[0:R, 0:512], siluT[:, kc, :], wm[:, kc, half * 512:(half + 1) * 512],
                                                  start=(kc == 0), stop=False), reads=[twm, t_sT], writes=[tpg])
                k.op("pe", lambda e: e.matmul(pg[0:R, 0:512], ones[0:1, 0:R],
                                              bmod[0:1, g * 1024 + half * 512:g * 1024 + (half + 1) * 512],
                                              start=False, stop=True), reads=[t_bm, t_on], writes=[tpg])
                k.op("act", lambda e: e.copy(out=gaterow[:, gi, half * 512:(half + 1) * 512], in_=pg[0:R, 0:512]),
                     reads=[tpg], writes=[t_gr])
    k.op("dve", lambda e: e.tensor_copy(out=S.mod[:].rearrange("p a b -> p (a b)"), in_=psm[:, 0:48 * R]),
         reads=[tpsm], writes=[S.t_mod])
    for base in (8, 32):
        k.op("dve", lambda e: e.tensor_scalar_add(out=S.mod[:, base:base + 8, :], in0=S.mod[:, base:base + 8, :],
                                                  scalar1=1.0), reads=[S.t_mod], writes=[S.t_mod])
    for gi in range(2):
        for r in range(R):
            for half in range(2):
                pg, tpg = k.ps()
                k.op("pe", lambda e: e.matmul(pg[:, 0:512], S.sel3[0:R, r * 128:(r + 1) * 128],
                                              gaterow[0:R, gi, half * 512:(half + 1) * 512], start=True, stop=True),
                     reads=[t_gr, S.t_const], writes=[tpg])
                k.op("act", lambda e: e.copy(out=S.gateb[:, gi, r, half * 512:(half + 1) * 512], in_=pg[:, 0:512]),
                     reads=[tpg], writes=[S.t_gateb])
    k.barrier()
    st.close()


def stage_inproj(S, l):
    k, cfg = S.k, S.cfg
    NB, C, L, T = cfg.NB, cfg.C, cfg.L, cfg.T
    st = contextlib.ExitStack()
    w = k.sbuf(st, "win", [128, 8, DIN], BF16); tw = Tr()
    ws = k.sbuf(st, "wsw", [128, 8, 640], BF16); tws = Tr()
    win = S.inp["w_in"]
    for kc in range(8):
        k.dma(w[:, kc, :], win[l, kc * 128:(kc + 1) * 128, :], writes=[tw], q="pool")
        src = win[l, kc * 128:(kc + 1) * 128, 1280:1920].rearrange("p (h two j) -> p h two j", two=2, j=32)
        dst = ws[:, kc, :].rearrange("p (h two j) -> p h two j", two=2, j=32)
        for half in range(2):
            k.dma(dst[:, :, 1 - half, :], src[:, :, half, :], writes=[tws], q="pool")
    cosT = k.sbuf(st, "cosT", [128, L]); sinT = k.sbuf(st, "sinT", [128, L]); t_rope = Tr()
    k.dma(cosT[:], S.inp["rope_cos"][:, :], writes=[t_rope])
    k.dma(sinT[:], S.inp["rope_sin"][:, :], writes=[t_rope])
    xins = [[(k.sbuf(st, "xin", [128, D]), Tr()) for _ in range(4)] for _ in range(2)]
    xms = [(k.sbuf(st, "xm", [128, 8, 512], BF16), Tr()) for _ in range(2)]
    stg = [(k.sbuf(st, "stg", [128, 512]), Tr()) for _ in range(4)]
    stgb = [(k.sbuf(st, "stgb", [128, 512], BF16), Tr()) for _ in range(4)]
    tmpa = [(k.sbuf(st, "tmpa", [128, 512]), Tr()) for _ in range(2)]
    tmpb = [(k.sbuf(st, "tmpb", [128, 512]), Tr()) for _ in range(2)]
    it = 0
    si = 0
    for b in range(NB):
        for (t0, wd) in tiles(0, C, 512) + tiles(C, T, 512):
            is_ctx = t0 < C
            r = NB if is_ctx else b
            tl = t0 - C
            nb = wd // 128
            xin = xins[it % 2]
            xm, txm = xms[it % 2]
            it += 1
            for tb in range(nb):
                k.dma(xin[tb][0][:], S.XRin[b, t0 + tb * 128:t0 + (tb + 1) * 128, :], reads=[S.t_XR[b]], writes=[xin[tb][1]],
                      q=("sp" if tb % 2 == 0 else "act"))
            for fc in range(8):
                ps, tps = k.ps()
                for tb in range(nb):
                    k.op("pe", lambda e: e.transpose(out=ps[:, tb * 128:(tb + 1) * 128],
                                                     in_=xin[tb][0][:, fc * 128:(fc + 1) * 128], identity=S.ident[:, :]),
                         reads=[xin[tb][1], S.t_const], writes=[tps])
                k.op("act", lambda e: e.activation(out=xm[:, fc, 0:wd], in_=ps[:, 0:wd], func=AF.Identity,
                                                   scale=S.mod[:, 8 + fc, r:r + 1], bias=S.mod[:, fc, r:r + 1]),
                     reads=[tps, S.t_mod], writes=[txm])

            def proj(wt, twt, c0):
                ps, tps = k.ps()
                for kc in range(8):
                    k.op("pe", lambda e: e.matmul(ps[:, 0:wd], wt[:, kc, c0:c0 + 128], xm[:, kc, 0:wd],
                                                  start=(kc == 0), stop=(kc == 7)), reads=[twt, txm], writes=[tps])
                return ps, tps

            for oc in range(10):
                ps, tps = proj(w, tw, oc * 128)
                sg, tsg = stg[si % 4]
                si += 1
                if oc % 2 == 0:
                    k.op("dve", lambda e: e.tensor_copy(out=sg[:, 0:wd], in_=ps[:, 0:wd]), reads=[tps], writes=[tsg])
                else:
                    k.op("act", lambda e: e.copy(out=sg[:, 0:wd], in_=ps[:, 0:wd]), reads=[tps], writes=[tsg])
                if oc < 8:
                    k.dma(S.PR[b, oc * 128:(oc + 1) * 128, t0:t0 + wd], sg[:, 0:wd], reads=[tsg], writes=[S.t_PR[b]])
                else:
                    k.dma(S.PS[b, (oc - 8) * 128:(oc - 7) * 128, t0:t0 + wd], sg[:, 0:wd], reads=[tsg], writes=[S.t_PS[b]])
            for qc in range(5):
                ps, tps = proj(w, tw, 1280 + qc * 128)
                sb_, tsb = stgb[si % 4]
                si += 1
                if is_ctx:
                    k.op("act", lambda e: e.copy(out=sb_[:, 0:wd], in_=ps[:, 0:wd]), reads=[tps], writes=[tsb])
                else:
                    ps2, tps2 = proj(ws, tws, qc * 128)
                    ta, tta = tmpa[si % 2]
                    tb_, ttb = tmpb[si % 2]
                    k.op("dve", lambda e: e.tensor_tensor(out=ta[:, 0:wd], in0=ps[:, 0:wd], in1=cosT[:, tl:tl + wd],
                                                          op=ALU.mult), reads=[tps, t_rope], writes=[tta])
                    k.op("dve", lambda e: e.tensor_tensor(out=tb_[:, 0:wd], in0=ps2[:, 0:wd], in1=sinT[:, tl:tl + wd],
                                                          op=ALU.mult), reads=[tps2, t_rope], writes=[ttb])
                    k.op("pool", lambda e: e.tensor_tensor(out=sb_[:, 0:wd], in0=ta[:, 0:wd], in1=tb_[:, 0:wd],
                                                           op=ALU.add), reads=[tta, ttb], writes=[tsb])
                if qc < 4:
                    k.dma(S.QT[b, qc * 128:(qc + 1) * 128, t0:t0 + wd], sb_[:, 0:wd], reads=[tsb], writes=[S.t_QT[b]])
                else:
                    k.dma(S.KT[b, :, t0:t0 + wd], sb_[:, 0:wd], reads=[tsb], writes=[S.t_KT[b]])
            for tb in range(nb):
                ps, tps = k.ps()
                for kc in range(8):
                    k.op("pe", lambda e: e.matmul(ps[:, 0:128], xm[:, kc, tb * 128:(tb + 1) * 128], w[:, kc, 1920:2048],
                                                  start=(kc == 0), stop=(kc == 7)), reads=[tw, txm], writes=[tps])
                sb_, tsb = stgb[si % 4]
                si += 1
                k.op("act", lambda e: e.copy(out=sb_[:, 0:128], in_=ps[:, 0:128]), reads=[tps], writes=[tsb])
                k.dma(S.V[b, t0 + tb * 128:t0 + (tb + 1) * 128, :], sb_[:, 0:128], reads=[tsb], writes=[S.t_V[b]])
    k.barrier()
    st.close()


def build(cfg):
    nc = bass.Bass("TRN2", target_bir_lowering=False)
    k = KB(nc)
    S = State()
    S.k, S.cfg = k, cfg
    NB, C, L, T = cfg.NB, cfg.C, cfg.L, cfg.T
    S.inp = {}
    for name, shp in PARAM_SHAPES.items():
        S.inp[name] = nc.dram_tensor(name, shp, F32, kind="ExternalInput").ap()
    for name, shp in CONST_SHAPES(cfg).items():
        S.inp[name] = nc.dram_tensor(name, shp, F32, kind="ExternalInput").ap()
    S.inp["cc"] = nc.dram_tensor("cc", [NB + 1, D], F32, kind="ExternalInput").ap()
    S.inp["xall"] = nc.dram_tensor("xall", [NB, T, D], F32, kind="ExternalInput").ap()
    S.out = nc.dram_tensor("out", [NB, L, D], F32, kind="ExternalOutput").ap()
    S.t_out = Tr()
    skind = "ExternalOutput" if cfg.dbg else "Internal"
    S.XR = k.dram("XR", [NB, T, D], F32, skind)
    S.PR = k.dram("PR", [NB, 1024, T], F32, skind)
    S.PS = k.dram("PS", [NB, 256, T], F32, skind)
    S.QT = k.dram("QT", [NB, 512, T], BF16, skind)
    S.KT = k.dram("KT", [NB, 128, T], BF16, skind)
    S.V = k.dram("V", [NB, T, 128], BF16, skind)
    S.YT = k.dram("YT", [NB, 1024, T], BF16, skind)
    S.PC = k.dram("PC", [NB, 1024, T], F32, skind)
    S.YD = k.dram("YD", [NB, T, 260], F32, skind)
    S.YS = k.dram("YS", [NB, 2, 256, T], F32, skind)
    _tn = NB * T
    _pmax = ((2 * _tn + EB - 1) // EB + NEXP) * EB
    S.HS = k.dram("HS", [_tn, D], BF16, skind); S.t_HS = Tr()
    S.HSORT = k.dram("HSORT", [_pmax, D], BF16, skind); S.t_HSORT = Tr()
    S.YB = k.dram("YB", [_pmax, D], BF16, skind); S.t_YB = Tr()
    for nm in ("XR", "PR", "PS", "QT", "KT", "V", "YT", "YD", "YS", "PC", "YTa", "YTs"):
        setattr(S, "t_" + nm, [Tr() for _ in range(NB)])
    k.init_psum()
    es = k.es
    S.t_const = Tr()
    S.ident = k.sbuf(es, "ident", [128, 128])
    S.identb = k.sbuf(es, "identb", [128, 128], BF16)
    S.sel3 = k.sbuf(es, "sel3", [3, 384])
    k.dma(S.ident[:], S.inp["ident"][:, :], writes=[S.t_const])
    k.dma(S.identb[:], S.inp["ident"][:, :], writes=[S.t_const], q="pool")
    k.dma(S.sel3[:], S.inp["sel3"][:, :], writes=[S.t_const])
    S.mod = k.sbuf(es, "mod", [128, 48, NB + 1]); S.t_mod = Tr()
    S.gateb = k.sbuf(es, "gateb", [128, 2, NB + 1, D]); S.t_gateb = Tr()
    if getattr(cfg, "inject_yt", False):
        ytin = nc.dram_tensor("ytin", [NB, 1024, T], BF16, kind="ExternalInput").ap()
        for b in range(NB):
            k.dma(S.YT[b], ytin[b], writes=[S.t_YT[b]])
    zst = contextlib.ExitStack()
    if stages_has_moe(cfg):
        zt = k.sbuf(zst, "zt", [128, 4, D], BF16); tzt = Tr()
        k.op("pool", lambda e: e.memset(zt[:], 0.0), writes=[tzt])
        for r0 in range(0, _pmax, 512):
            k.dma(S.HSORT[r0:r0 + 512, :].rearrange("(n p) d -> p n d", p=128), zt[:], reads=[tzt], writes=[S.t_HSORT],
                  q=("sp" if (r0 // 512) % 2 == 0 else "act"))
    k.barrier()
    zst.close()
    stages = cfg.stages
    for l in cfg.layers:
        last = (l == DEPTH - 1)
        S.XRin = S.inp["xall"] if l == cfg.layers[0] else S.XR
        if stages is None or "mod" in stages:
            stage_mod(S, l)
        if stages is None or "inproj" in stages:
            stage_inproj(S, l)
        if stages is None or "rwkv" in stages:
            stage_rwkv(S, l, last)
        if stages is None or ("s5" in stages and "attn" in stages):
            st_a = contextlib.ExitStack()
            ag = attn_stream(S, l, last, st_a)
            next(ag)
            stage_s5(S, l, last, side=ag)
            for _ in ag:
                pass
            k.barrier()
            st_a.close()
        else:
            if "s5" in stages:
                stage_s5(S, l, last)
            if "attn" in stages:
                stage_attn(S, l, last)
        if stages is None or "outln" in stages:
            stage_outln(S, l, last)
        if stages is None or "moe" in stages:
            stage_moe2(S, l, last)
    k.barrier()
    k.close()
    return nc


_EXPERT_CACHE = {}


def relayout_experts(inputs):
    key = id(inputs["expert_w_gate"])
    if key in _EXPERT_CACHE:
        return _EXPERT_CACHE[key]
    out = {}
    for nm, src, nchunk, width in (("ewg", "expert_w_gate", 8, DE), ("ewu", "expert_w_up", 8, DE), ("ewd", "expert_w_down", 4, D)):
        w = np.asarray(inputs[src], dtype=np.float32).reshape(DEPTH, NEXP, nchunk, 128, width)
        w = w.transpose(0, 1, 3, 2, 4)
        h = nchunk // 2
        out[nm + "_a"] = np.ascontiguousarray(w[:, :, :, :h, :]).reshape(DEPTH * NEXP * 128, 2048)
        out[nm + "_b"] = np.ascontiguousarray(w[:, :, :, h:, :]).reshape(DEPTH * NEXP * 128, 2048)
    _EXPERT_CACHE.clear()
    _EXPERT_CACHE[key] = out
    return out


def stages_has_moe(cfg):
    return (cfg.stages is None or "moe" in cfg.stages) and getattr(cfg, "sparse", True)


def make_in_maps(cfg, inputs, n_cores):
    consts = host_consts(cfg)
    NB = cfg.NB
    maps = []
    for ci in range(n_cores):
        m = {}
        for name in PARAM_SHAPES:
            if name.startswith("ew"):
                continue
            m[name] = np.ascontiguousarray(inputs[name], dtype=np.float32).reshape(PARAM_SHAPES[name])
        m.update(relayout_experts(inputs))
        m.update(consts)
        bs = slice(ci * NB, (ci + 1) * NB)
        m["cc"] = np.ascontiguousarray(np.concatenate([inputs["c"][bs], inputs["c_ctx"][None, :]], axis=0), dtype=np.float32)
        m["xall"] = np.ascontiguousarray(np.concatenate([inputs["ctx"][bs], inputs["x"][bs]], axis=1), dtype=np.float32)
        maps.append(m)
    return maps


def kernel(**inputs):
    cfg = Cfg()
    n = 8
    nc = build(cfg)
    maps = make_in_maps(cfg, inputs, n)
    res = run_bass_kernel_spmd(nc, maps, core_ids=list(range(n)))
    return np.concatenate([np.asarray(r["out"], dtype=np.float32) for r in res.results], axis=0)


def stage_attn(S, l, last):
    st = contextlib.ExitStack()
    for _ in attn_stream(S, l, last, st):
        pass
    S.k.barrier()
    st.close()


def attn_stream(S, l, last, st):
    k, cfg = S.k, S.cfg
    NB, C, L, T = cfg.NB, cfg.C, cfg.L, cfg.T
    ncb = C // 128
    nkb = T // 128
    mskb = k.sbuf(st, "mskb", [128, 4, 128], BF16); t_msk = Tr()
    k.dma(mskb[:].rearrange("p a b -> p (a b)"), S.inp["msk"][:, :], writes=[t_msk], q="pool")
    esk = k.sbuf(st, "esk", [128, AH]); t_esk = Tr()
    k.dma(esk[64:65, :], S.inp["attn_sink"][l:l + 1, :], writes=[t_esk])
    k.op("act", lambda e: e.activation(out=esk[64:65, :], in_=esk[64:65, :], func=AF.Exp), reads=[t_esk], writes=[t_esk])
    onesr = k.sbuf(st, "onesr", [128, 64]); t_on = Tr()
    k.op("dve", lambda e: e.memset(onesr[:], 1.0), writes=[t_on])
    kT = k.sbuf(st, "kT", [64, T], BF16); t_kT = Tr()
    qTs = [(k.sbuf(st, "qT", [64, AG, 128], BF16), Tr()) for _ in range(2)]
    va = k.sbuf(st, "va", [128, nkb, 65], BF16); t_va = Tr()
    pts = [(k.sbuf(st, "pt", [128, 512], BF16), Tr()) for _ in range(3)]
    dens = [(k.sbuf(st, "den", [128, 512]), Tr()) for _ in range(1)]
    rbs = [(k.sbuf(st, "rb", [64, 512]), Tr()) for _ in range(1)]
    obs = [(k.sbuf(st, "ob", [64, 512], BF16), Tr()) for _ in range(2)]
    pi = 0
    oi = 0
    yield
    for b in range(NB):
        for kv in range(AKV):
            k.dma(kT[:], S.KT[b, kv * 64:(kv + 1) * 64, :], reads=[S.t_KT[b]], writes=[t_kT])
            k.op("dve", lambda e: e.memset(va[:, :, 64:65], 1.0), writes=[t_va])
            k.dma(va[:, :, 0:64], S.V[b, :, kv * 64:(kv + 1) * 64].rearrange("(n p) d -> p n d", p=128), reads=[S.t_V[b]],
                  writes=[t_va])
            qblocks = list(range(ncb, nkb)) if last else list(range(nkb))
            for qb in qblocks:
                if qb < ncb:
                    kbl = [(j, None) for j in range(ncb)]
                else:
                    kbl = [(j, None) for j in range(ncb)]
                    if qb - 1 >= ncb:
                        kbl.append((qb - 1, 3))
                    kbl.append((qb, None))
                    if qb + 1 < nkb:
                        kbl.append((qb + 1, 1))
                qT, t_qT = qTs[oi % 2]
                k.dma(qT[:], S.QT[b, kv * 256:(kv + 1) * 256, qb * 128:(qb + 1) * 128].rearrange("(g p) t -> p g t", p=64),
                      reads=[S.t_QT[b]], writes=[t_qT])
                po, tpo = k.ps()
                for i, (kb, m) in enumerate(kbl):
                    ps, tps = k.ps()
                    k.op("pe", lambda e: e.matmul(ps[:, 0:512], kT[:, kb * 128:(kb + 1) * 128], qT[:, :, :], start=True, stop=True),
                         reads=[t_kT, t_qT], writes=[tps])
                    pt, tpt = pts[pi % 3]
                    pi += 1
                    k.op("act", lambda e: e.activation(out=pt[:], in_=ps[:, 0:512], func=AF.Exp, scale=0.125),
                         reads=[tps], writes=[tpt])
                    if m is not None:
                        k.op("pool", lambda e: e.tensor_tensor(
                            out=pt[:].rearrange("p (g q) -> p g q", g=AG), in0=pt[:].rearrange("p (g q) -> p g q", g=AG),
                            in1=mskb[:, m:m + 1, :].to_broadcast([128, AG, 128]), op=ALU.mult),
                            reads=[tpt, t_msk], writes=[tpt])
                    k.op("pe", lambda e: e.matmul(po[0:65, 0:512], va[:, kb, :], pt[:], start=(i == 0),
                                                  stop=(i == len(kbl) - 1)), reads=[t_va, tpt], writes=[tpo])
                den, tden = dens[0]
                rb, trb = rbs[0]
                ob, tob = obs[oi % 2]
                oi += 1
                k.op("dve", lambda e: e.tensor_tensor(
                    out=den[64:65, :].rearrange("p (g q) -> p g q", g=AG), in0=po[64:65, 0:512].rearrange("p (g q) -> p g q", g=AG),
                    in1=esk[64:65, kv * AG:(kv + 1) * AG].unsqueeze(2).to_broadcast([1, AG, 128]), op=ALU.add),
                    reads=[tpo, t_esk], writes=[tden])
                k.op("dve", lambda e: e.reciprocal(out=den[64:65, :], in_=den[64:65, :]), reads=[tden], writes=[tden])
                pb, tpb = k.ps()
                k.op("pe", lambda e: e.matmul(pb[0:64, 0:512], onesr[64:65, 0:64], den[64:65, :], start=True, stop=True),
                     reads=[t_on, tden], writes=[tpb])
                k.op("act", lambda e: e.copy(out=rb[:], in_=pb[0:64, 0:512]), reads=[tpb], writes=[trb])
                k.op("dve", lambda e: e.tensor_tensor(out=ob[:], in0=po[0:64, 0:512], in1=rb[:], op=ALU.mult),
                     reads=[tpo, trb], writes=[tob])
                k.dma(S.YT[b, 512 + kv * 256:512 + (kv + 1) * 256, qb * 128:(qb + 1) * 128].rearrange("(g p) t -> p g t", p=64),
                      ob[:].rearrange("p (g q) -> p g q", g=AG), reads=[tob], writes=[S.t_YTa[b]])
                yield


def ln_block(S, z, tz, lng, lnb, t_ln, sm, tsm):
    k = S.k
    k.op("dve", lambda e: e.bn_stats(out=sm[:, 0:6], in_=z[:, 0:512]), reads=[tz], writes=[tsm])
    k.op("dve", lambda e: e.bn_stats(out=sm[:, 6:12], in_=z[:, 512:1024]), reads=[tz], writes=[tsm])
    k.op("dve", lambda e: e.bn_aggr(out=sm[:, 12:14], in_=sm[:, 0:12].rearrange("p (a b) -> p a b", b=6)),
         reads=[tsm], writes=[tsm])
    rsqrt_eps(k, sm[:, 14:15], sm[:, 13:14], LN_EPS, tsm)
    k.op("dve", lambda e: e.scalar_tensor_tensor(out=sm[:, 15:16], in0=sm[:, 12:13], scalar=-1.0, in1=sm[:, 14:15],
                                                 op0=ALU.mult, op1=ALU.mult), reads=[tsm], writes=[tsm])
    k.op("act", lambda e: e.activation(out=z[:], in_=z[:], func=AF.Identity, scale=sm[:, 14:15], bias=sm[:, 15:16]),
         reads=[tz, tsm], writes=[tz])
    k.op("dve", lambda e: e.tensor_tensor(out=z[:], in0=z[:], in1=lng[:], op=ALU.mult), reads=[tz, t_ln], writes=[tz])
    k.op("pool", lambda e: e.tensor_tensor(out=z[:], in0=z[:], in1=lnb[:], op=ALU.add), reads=[tz, t_ln], writes=[tz])


def rsqrt_eps(k, out, in_, eps, tr):
    k.op("dve", lambda e: e.tensor_scalar(out=out, in0=in_, scalar1=float(eps), scalar2=None, op0=ALU.add), reads=[tr], writes=[tr])
    k.op("act", lambda e: e.activation(out=out, in_=out, func=AF.Sqrt), reads=[tr], writes=[tr])
    k.op("dve", lambda e: e.reciprocal(out=out, in_=out), reads=[tr], writes=[tr])


def load_bcast_row(S, st, name, src_row, t):
    k = S.k
    row = k.sbuf(st, name + "r", [1, D]); trow = Tr()
    k.dma(row[:], src_row, writes=[trow])
    ones = k.sbuf(st, name + "o", [1, 128]); to = Tr()
    k.op("dve", lambda e: e.memset(ones[:], 1.0), writes=[to])
    out = k.sbuf(st, name, [128, D])
    for h in range(2):
        ps, tps = k.ps()
        k.op("pe", lambda e: e.matmul(ps[:, 0:512], ones[0:1, :], row[0:1, h * 512:(h + 1) * 512], start=True, stop=True),
             reads=[trow, to], writes=[tps])
        k.op("act", lambda e: e.copy(out=out[:, h * 512:(h + 1) * 512], in_=ps[:, 0:512]), reads=[tps], writes=[t])
    return out


def stage_outln(S, l, last):
    k, cfg = S.k, S.cfg
    NB, C, L, T = cfg.NB, cfg.C, cfg.L, cfg.T
    st = contextlib.ExitStack()
    wo = k.sbuf(st, "wo", [128, 8, D], BF16); two = Tr()
    for kc in range(8):
        k.dma(wo[:, kc, :], S.inp["w_out"][l, kc * 128:(kc + 1) * 128, :], writes=[two], q="pool")
    t_ln = Tr()
    lng = load_bcast_row(S, st, "lng", S.inp["ln1_g"][l:l + 1, :], t_ln)
    lnb = load_bcast_row(S, st, "lnb", S.inp["ln1_b"][l:l + 1, :], t_ln)
    yts = [(k.sbuf(st, "yt", [128, 8, 128], BF16), Tr()) for _ in range(4)]
    xrs = [(k.sbuf(st, "xr", [128, D]), Tr()) for _ in range(4)]
    zs = [(k.sbuf(st, "z", [128, D]), Tr()) for _ in range(4)]
    sms = [(k.sbuf(st, "sm", [128, 32]), Tr()) for _ in range(4)]
    it = 0
    for b in range(NB):
        for t0 in range(C if last else 0, T, 128):
            r = NB if t0 < C else b
            yt, tyt = yts[it % 4]; xr, txr = xrs[it % 4]; z, tz = zs[it % 4]; sm, tsm = sms[it % 4]
            it += 1
            k.dma(yt[:], S.YT[b, :, t0:t0 + 128].rearrange("(kc p) t -> p kc t", p=128), reads=[S.t_YT[b]], writes=[tyt])
            k.dma(xr[:], S.XRin[b, t0:t0 + 128, :], reads=[S.t_XR[b]], writes=[txr], q="act")
            for h in range(2):
                ps, tps = k.ps()
                for kc in range(8):
                    k.op("pe", lambda e: e.matmul(ps[:, 0:512], yt[:, kc, :], wo[:, kc, h * 512:(h + 1) * 512],
                                                  start=(kc == 0), stop=(kc == 7)), reads=[tyt, two], writes=[tps])
                k.op("dve", lambda e: e.tensor_tensor(out=z[:, h * 512:(h + 1) * 512], in0=ps[:, 0:512],
                                                      in1=S.gateb[:, 0, r, h * 512:(h + 1) * 512], op=ALU.mult),
                     reads=[tps, S.t_gateb], writes=[tz])
            k.op("dve", lambda e: e.scalar_tensor_tensor(out=z[:], in0=xr[:], scalar=ALPHA, in1=z[:], op0=ALU.mult,
                                                          op1=ALU.add), reads=[txr, tz], writes=[tz])
            ln_block(S, z, tz, lng, lnb, t_ln, sm, tsm)
            k.dma(S.XR[b, t0:t0 + 128, :], z[:], reads=[tz], writes=[S.t_XR[b]])
    k.barrier()
    st.close()


def stage_s5(S, l, last, side=None):
    k, cfg = S.k, S.cfg
    NB, C, L, T = cfg.NB, cfg.C, cfg.L, cfg.T
    st = contextlib.ExitStack()
    nc = k.nc
    tp = Tr()
    P = lambda name, shape: k.sbuf(st, name, shape)
    halfpi = P("halfpi", [128, 1])
    k.op("dve", lambda e: e.memset(halfpi[:], math.pi / 2), writes=[tp])
    lre = P("lre", [128, 2, 8]); lim = P("lim", [128, 2, 8]); dt = P("dt", [128, 2, 8])
    for d in range(2):
        with nc.allow_non_contiguous_dma(reason="tiny param loads"):
            k.dma(lre[:, d, :], S.inp["s5_lam_re"][l, d].rearrange("(rc g2) p -> g2 p rc", g2=2), writes=[tp])
            k.dma(lim[:, d, :], S.inp["s5_lam_im"][l, d].rearrange("(rc g2) p -> g2 p rc", g2=2), writes=[tp])
            for g2 in range(2):
                k.dma(dt[g2 * 64:(g2 + 1) * 64, d, :],
                      S.inp["s5_log_dt"][l, d:d + 1, :].rearrange("o (rc g2) -> o g2 rc", g2=2)[:, g2, :].to_broadcast([64, 8]),
                      writes=[tp])
    dv = lambda fn: k.op("dve", fn, reads=[tp], writes=[tp])
    ac = lambda fn: k.op("act", fn, reads=[tp], writes=[tp])
    ac(lambda e: e.activation(out=dt[:], in_=dt[:], func=AF.Exp))
    mag = P("mag", [128, 2, 8]); th = P("th", [128, 2, 8]); cs = P("cs", [128, 2, 8]); sn = P("sn", [128, 2, 8])
    t1 = P("t1", [128, 2, 8]); t2 = P("t2", [128, 2, 8])
    dv(lambda e: e.tensor_tensor(out=mag[:], in0=lre[:], in1=dt[:], op=ALU.mult))
    ac(lambda e: e.activation(out=mag[:], in_=mag[:], func=AF.Exp))
    dv(lambda e: e.tensor_tensor(out=th[:], in0=lim[:], in1=dt[:], op=ALU.mult))
    ac(lambda e: e.activation(out=sn[:], in_=th[:], func=AF.Sin, scale=1.0 / 16))
    ac(lambda e: e.activation(out=cs[:], in_=th[:], func=AF.Sin, scale=1.0 / 16, bias=halfpi[:, 0:1]))
    for _ in range(4):
        dv(lambda e: e.tensor_tensor(out=t1[:], in0=cs[:], in1=cs[:], op=ALU.mult))
        dv(lambda e: e.tensor_tensor(out=t2[:], in0=sn[:], in1=sn[:], op=ALU.mult))
        dv(lambda e: e.tensor_tensor(out=sn[:], in0=sn[:], in1=cs[:], op=ALU.mult))
        dv(lambda e: e.tensor_scalar(out=sn[:], in0=sn[:], scalar1=2.0, scalar2=None, op0=ALU.mult))
        dv(lambda e: e.tensor_tensor(out=cs[:], in0=t1[:], in1=t2[:], op=ALU.subtract))
    abr = P("abr", [128, 2, 8]); abi = P("abi", [128, 2, 8]); cre = P("cre", [128, 2, 8]); cim = P("cim", [128, 2, 8])
    den = P("den", [128, 2, 8])
    dv(lambda e: e.tensor_tensor(out=abr[:], in0=mag[:], in1=cs[:], op=ALU.mult))
    dv(lambda e: e.tensor_tensor(out=abi[:], in0=mag[:], in1=sn[:], op=ALU.mult))
    dv(lambda e: e.tensor_scalar(out=t1[:], in0=abr[:], scalar1=-1.0, scalar2=None, op0=ALU.add))
    dv(lambda e: e.tensor_tensor(out=den[:], in0=lre[:], in1=lre[:], op=ALU.mult))
    dv(lambda e: e.tensor_tensor(out=t2[:], in0=lim[:], in1=lim[:], op=ALU.mult))
    dv(lambda e: e.tensor_tensor(out=den[:], in0=den[:], in1=t2[:], op=ALU.add))
    dv(lambda e: e.reciprocal(out=den[:], in_=den[:]))
    dv(lambda e: e.tensor_tensor(out=cre[:], in0=t1[:], in1=lre[:], op=ALU.mult))
    dv(lambda e: e.tensor_tensor(out=t2[:], in0=abi[:], in1=lim[:], op=ALU.mult))
    dv(lambda e: e.tensor_tensor(out=cre[:], in0=cre[:], in1=t2[:], op=ALU.add))
    dv(lambda e: e.tensor_tensor(out=cre[:], in0=cre[:], in1=den[:], op=ALU.mult))
    dv(lambda e: e.tensor_tensor(out=cim[:], in0=abi[:], in1=lre[:], op=ALU.mult))
    dv(lambda e: e.tensor_tensor(out=t2[:], in0=t1[:], in1=lim[:], op=ALU.mult))
    dv(lambda e: e.tensor_tensor(out=cim[:], in0=cim[:], in1=t2[:], op=ALU.subtract))
    dv(lambda e: e.tensor_tensor(out=cim[:], in0=cim[:], in1=den[:], op=ALU.mult))
    bre = P("bre", [128, 8, 16]); bim = P("bim", [128, 8, 16])
    with nc.allow_non_contiguous_dma(reason="param loads"):
        k.dma(bre[:], S.inp["s5_b_re"][l].rearrange("(rc g2) p c -> g2 p rc c", g2=2), writes=[tp])
        k.dma(bim[:], S.inp["s5_b_im"][l].rearrange("(rc g2) p c -> g2 p rc c", g2=2), writes=[tp])
    ctr = P("ctr", [128, 8, 48]); cti = P("cti", [128, 8, 48])
    dv(lambda e: e.memset(ctr[:], 0.0))
    dv(lambda e: e.memset(cti[:], 0.0))
    with nc.allow_non_contiguous_dma(reason="param loads"):
        for g2 in range(2):
            src_r = S.inp["s5_c_re"][l].rearrange("(rc g2) c p -> g2 p rc c", g2=2)[g2]
            src_i = S.inp["s5_c_im"][l].rearrange("(rc g2) c p -> g2 p rc c", g2=2)[g2]
            for rc in range(8):
                k.dma(ctr[g2 * 64:(g2 + 1) * 64, rc, g2 * 32:g2 * 32 + 16], src_r[:, rc, :], writes=[tp])
                k.dma(cti[g2 * 64:(g2 + 1) * 64, rc, g2 * 32:g2 * 32 + 16], src_i[:, rc, :], writes=[tp])
    dv(lambda e: e.tensor_scalar(out=cti[:], in0=cti[:], scalar1=-1.0, scalar2=None, op0=ALU.mult))
    bbp = P("bbp", [128, 8, 48]); tq = P("tq", [128, 8, 16]); tq2 = P("tq2", [128, 8, 16])
    bbT = P("bbT", [48, 2, 2, 8, 128])
    for d in range(2):
        for ri in range(2):
            crb = cre[:, d, :].unsqueeze(2).to_broadcast([128, 8, 16])
            cib = cim[:, d, :].unsqueeze(2).to_broadcast([128, 8, 16])
            if ri == 0:
                dv(lambda e: e.tensor_tensor(out=tq[:], in0=bre[:], in1=crb, op=ALU.mult))
                dv(lambda e: e.tensor_tensor(out=tq2[:], in0=bim[:], in1=cib, op=ALU.mult))
                dv(lambda e: e.tensor_tensor(out=tq[:], in0=tq[:], in1=tq2[:], op=ALU.subtract))
            else:
                dv(lambda e: e.tensor_tensor(out=tq[:], in0=bim[:], in1=crb, op=ALU.mult))
                dv(lambda e: e.tensor_tensor(out=tq2[:], in0=bre[:], in1=cib, op=ALU.mult))
                dv(lambda e: e.tensor_tensor(out=tq[:], in0=tq[:], in1=tq2[:], op=ALU.add))
            dv(lambda e: e.memset(bbp[:], 0.0))
            dv(lambda e: e.tensor_copy(out=bbp[0:64, :, 0:16], in_=tq[0:64, :, :]))
            dv(lambda e: e.tensor_copy(out=bbp[64:128, :, 32:48], in_=tq[64:128, :, :]))
            for rc in range(8):
                ps, tps = k.ps()
                k.op("pe", lambda e: e.transpose(out=ps[0:48, 0:128], in_=bbp[:, rc, :], identity=S.ident[:, :]),
                     reads=[tp, S.t_const], writes=[tps])
                k.op("act", lambda e: e.copy(out=bbT[:, d, ri, rc, :], in_=ps[0:48, 0:128]), reads=[tps], writes=[tp])
    nd = 0
    while (1 << nd) < T:
        nd += 1
    rc_n = P("rcn", [128, 2, 8, nd + 1]); rs_n = P("rsn", [128, 2, 8, nd + 1])
    dv(lambda e: e.tensor_copy(out=rc_n[:, :, :, 0], in_=cs[:]))
    dv(lambda e: e.tensor_copy(out=rs_n[:, :, :, 0], in_=sn[:]))
    for i in range(nd):
        dv(lambda e: e.tensor_tensor(out=t1[:], in0=rc_n[:, :, :, i], in1=rc_n[:, :, :, i], op=ALU.mult))
        dv(lambda e: e.tensor_tensor(out=t2[:], in0=rs_n[:, :, :, i], in1=rs_n[:, :, :, i], op=ALU.mult))
        dv(lambda e: e.tensor_tensor(out=rc_n[:, :, :, i + 1], in0=t1[:], in1=t2[:], op=ALU.subtract))
        dv(lambda e: e.tensor_tensor(out=t1[:], in0=rc_n[:, :, :, i], in1=rs_n[:, :, :, i], op=ALU.mult))
        dv(lambda e: e.tensor_scalar(out=rs_n[:, :, :, i + 1], in0=t1[:], scalar1=2.0, scalar2=None, op0=ALU.mult))
    dskip = P("dskip", [128, 2]); glub = P("glub", [128, 2])
    with nc.allow_non_contiguous_dma(reason="param loads"):
        k.dma(dskip[:], S.inp["s5_d"][l].rearrange("(kc p) -> p kc", p=128), writes=[tp])
        k.dma(glub[:], S.inp["s5_glu_b"][l].rearrange("(kc p) -> p kc", p=128), writes=[tp])
    gluw = k.sbuf(st, "gluw", [128, 2, 256], BF16)
    k.dma(gluw[:], S.inp["s5_glu_w"][l].rearrange("(kc p) n -> p kc n", p=128), writes=[tp], q="pool")

    cosT = P("cosT5", [128, T]); sinT = P("sinT5", [128, T]); t_tab = Tr()
    tabT = P("tabT5", [128, T]); t_tabT = Tr()
    st_main = contextlib.ExitStack()
    PM = lambda name, shape: k.sbuf(st_main, name, shape)
    bufs = []
    for b in range(NB):
        d_ = {}
        for nm in ("zr", "zi", "gr", "gi"):
            d_[nm] = (PM(nm + "5", [128, T]), Tr())
        d_["up"] = (PM("up5", [48, T]), Tr())
        d_["stg"] = [(PM("stg5", [48, 512]), Tr()) for _ in range(1)]
        k.op("dve", lambda e: e.memset(d_["up"][0][:], 0.0), writes=[d_["up"][1]])
        bufs.append(d_)
    ustage = PM("ustage5", [48, T]); t_ust = Tr()
    k.op("dve", lambda e: e.memset(ustage[:], 0.0), writes=[t_ust])

    def body(d, rc, b):
        B_ = bufs[b]
        zr, t_zr = B_["zr"]; zi, t_zi = B_["zi"]; gr, t_gr = B_["gr"]; gi, t_gi = B_["gi"]
        up, t_up = B_["up"]; stg = B_["stg"]
        src = S.PS[b].rearrange("(rc g2 c) t -> rc g2 c t", g2=2, c=16)
        if d == 0:
            k.dma(up[0:16, :], src[rc, 0], reads=[S.t_PS[b]], writes=[t_up])
            k.dma(up[32:48, :], src[rc, 1], reads=[S.t_PS[b]], writes=[t_up])
        else:
            k.dma(ustage[0:16, :], src[rc, 0], reads=[S.t_PS[b]], writes=[t_ust])
            k.dma(ustage[32:48, :], src[rc, 1], reads=[S.t_PS[b]], writes=[t_ust])
            k.op("pool", lambda e: e.tensor_copy(out=up[:, 0:C], in_=ustage[:, 0:C][:, ::-1]), reads=[t_ust], writes=[t_up])
            k.op("pool", lambda e: e.tensor_copy(out=up[:, C:T], in_=ustage[:, C:T][:, ::-1]), reads=[t_ust], writes=[t_up])
        u_, tu_ = up, t_up
        yield
        for (t0, wd) in tiles(0, T, 512):
            pr_, tpr = k.ps()
            pi_, tpi = k.ps()
            k.op("pe", lambda e: e.matmul(pr_[:, 0:wd], bbT[:, d, 0, rc, :], u_[:, t0:t0 + wd], start=True, stop=True),
                 reads=[tp, tu_], writes=[tpr])
            k.op("pe", lambda e: e.matmul(pi_[:, 0:wd], bbT[:, d, 1, rc, :], u_[:, t0:t0 + wd], start=True, stop=True),
                 reads=[tp, tu_], writes=[tpi])
            sl = slice(t0, t0 + wd)
            k.op("dve", lambda e: e.tensor_tensor(out=zr[:, sl], in0=pr_[:, 0:wd], in1=cosT[:, sl], op=ALU.mult),
                 reads=[tpr, t_tab], writes=[t_zr])
            k.op("dve", lambda e: e.tensor_tensor(out=gr[:, sl], in0=pi_[:, 0:wd], in1=sinT[:, sl], op=ALU.mult),
                 reads=[tpi, t_tab], writes=[t_gr])
            k.op("pool", lambda e: e.tensor_tensor(out=zr[:, sl], in0=zr[:, sl], in1=gr[:, sl], op=ALU.add),
                 reads=[t_gr], writes=[t_zr])
            k.op("dve", lambda e: e.tensor_tensor(out=zi[:, sl], in0=pi_[:, 0:wd], in1=cosT[:, sl], op=ALU.mult),
                 reads=[tpi, t_tab], writes=[t_zi])
            k.op("dve", lambda e: e.tensor_tensor(out=gi[:, sl], in0=pr_[:, 0:wd], in1=sinT[:, sl], op=ALU.mult),
                 reads=[tpr, t_tab], writes=[t_gi])
            k.op("pool", lambda e: e.tensor_tensor(out=zi[:, sl], in0=zi[:, sl], in1=gi[:, sl], op=ALU.subtract),
                 reads=[t_gi], writes=[t_zi])
            yield
        mg = mag[:, d, rc:rc + 1].to_broadcast([128, T])
        k.op("dve", lambda e: e.tensor_tensor_scan(out=gr[:], data0=mg, data1=zr[:], initial=0.0, op0=ALU.mult, op1=ALU.add),
             reads=[t_zr, tp], writes=[t_gr])
        yield
        k.op("dve", lambda e: e.tensor_tensor_scan(out=gi[:], data0=mg, data1=zi[:], initial=0.0, op0=ALU.mult, op1=ALU.add),
             reads=[t_zi, tp], writes=[t_gi])
        yield
        k.op("dve", lambda e: e.tensor_tensor(out=zr[:], in0=gr[:], in1=cosT[:], op=ALU.mult), reads=[t_gr, t_tab], writes=[t_zr])
        k.op("pool", lambda e: e.tensor_tensor(out=zi[:], in0=gi[:], in1=sinT[:], op=ALU.mult), reads=[t_gi, t_tab], writes=[t_zi])
        yield
        k.op("dve", lambda e: e.tensor_tensor(out=zr[:], in0=zr[:], in1=zi[:], op=ALU.subtract), reads=[t_zi], writes=[t_zr])
        yield
        k.op("pool", lambda e: e.tensor_tensor(out=zi[:], in0=gi[:], in1=cosT[:], op=ALU.mult), reads=[t_gi, t_tab, t_zr], writes=[t_zi])
        k.op("dve", lambda e: e.tensor_tensor(out=gr[:], in0=gr[:], in1=sinT[:], op=ALU.mult), reads=[t_tab], writes=[t_gr])
        yield
        k.op("pool", lambda e: e.tensor_tensor(out=zi[:], in0=zi[:], in1=gr[:], op=ALU.add), reads=[t_gr], writes=[t_zi])
        yield
        for ti, (t0, wd) in enumerate(tiles(0, T, 512)):
            py, tpy = k.ps()
            k.op("pe", lambda e: e.matmul(py[0:48, 0:wd], ctr[:, rc, :], zr[:, t0:t0 + wd], start=True, stop=False),
                 reads=[tp, t_zr], writes=[tpy])
            k.op("pe", lambda e: e.matmul(py[0:48, 0:wd], cti[:, rc, :], zi[:, t0:t0 + wd], start=False, stop=True),
                 reads=[tp, t_zi], writes=[tpy])
            sg, tsg = stg[0]
            k.op("act", lambda e: e.copy(out=sg[:, 0:wd], in_=py[0:48, 0:wd]), reads=[tpy], writes=[tsg])
            dst = S.YS[b, d].rearrange("(rc g2 c) t -> rc g2 c t", g2=2, c=16)
            k.dma(dst[rc, 0, :, t0:t0 + wd], sg[0:16, 0:wd], reads=[tsg], writes=[S.t_YS[b]])
            k.dma(dst[rc, 1, :, t0:t0 + wd], sg[32:48, 0:wd], reads=[tsg], writes=[S.t_YS[b]])
            yield

    side_alive = [side is not None]
    for d in range(2):
        for rc in range(8):
            k.op("dve", lambda e: e.memset(cosT[:, 0:1], 1.0), reads=[t_tab], writes=[t_tab])
            k.op("dve", lambda e: e.memset(sinT[:, 0:1], 0.0), reads=[t_tab], writes=[t_tab])
            n = 1
            i = 0
            while n < T:
                m = min(n, T - n)
                cn = rc_n[:, d, rc, i:i + 1]; sn_ = rs_n[:, d, rc, i:i + 1]
                k.op("dve", lambda e: e.tensor_scalar(out=tabT[:, 0:m], in0=sinT[:, 0:m], scalar1=sn_, scalar2=None, op0=ALU.mult),
                     reads=[t_tab, tp], writes=[t_tabT])
                k.op("dve", lambda e: e.scalar_tensor_tensor(out=cosT[:, n:n + m], in0=cosT[:, 0:m], scalar=cn, in1=tabT[:, 0:m],
                                                             op0=ALU.mult, op1=ALU.subtract), reads=[t_tab, t_tabT, tp], writes=[t_tab])
                k.op("dve", lambda e: e.tensor_scalar(out=tabT[:, 0:m], in0=cosT[:, 0:m], scalar1=sn_, scalar2=None, op0=ALU.mult),
                     reads=[t_tab, tp], writes=[t_tabT])
                k.op("dve", lambda e: e.scalar_tensor_tensor(out=sinT[:, n:n + m], in0=sinT[:, 0:m], scalar=cn, in1=tabT[:, 0:m],
                                                             op0=ALU.mult, op1=ALU.add), reads=[t_tab, t_tabT, tp], writes=[t_tab])
                n *= 2
                i += 1
            gens = [body(d, rc, b) for b in range(NB)]
            while gens:
                for g_ in list(gens):
                    try:
                        next(g_)
                    except StopIteration:
                        gens.remove(g_)
                if side_alive[0]:
                    try:
                        next(side)
                    except StopIteration:
                        side_alive[0] = False
    k.barrier()
    st_main.close()
    TW = 512
    uu = [(P("uu", [128, 2, TW]), Tr()) for _ in range(2)]
    y0 = [(P("y0", [128, 2, TW]), Tr()) for _ in range(2)]
    y1 = [(P("y1", [128, 2, TW]), Tr()) for _ in range(2)]
    zz = [(P("zz", [128, 2, TW]), Tr()) for _ in range(2)]
    zb = [(k.sbuf(st, "zb", [128, 2, TW], BF16), Tr()) for _ in range(2)]
    ob = [(k.sbuf(st, "ob5", [128, 2, TW], BF16), Tr()) for _ in range(2)]
    it = 0
    GC = 2.0 * math.sqrt(2.0 / math.pi)
    for b in range(NB):
        segs = ([] if last else tiles(0, C, TW)) + tiles(C, T, TW)
        for (t0, wd) in segs:
            seg0, seg1 = (0, C) if t0 < C else (C, T)
            r0 = seg0 + (seg1 - (t0 + wd))
            u_, tu = uu[it % 2]; a0, ta0 = y0[it % 2]; a1, ta1 = y1[it % 2]; z_, tz = zz[it % 2]
            zb_, tzb = zb[it % 2]; ob_, tob = ob[it % 2]
            it += 1
            k.dma(u_[:, :, 0:wd], S.PS[b, :, t0:t0 + wd].rearrange("(kc p) t -> p kc t", p=128), reads=[S.t_PS[b]], writes=[tu])
            k.dma(a0[:, :, 0:wd], S.YS[b, 0, :, t0:t0 + wd].rearrange("(kc p) t -> p kc t", p=128), reads=[S.t_YS[b]], writes=[ta0])
            k.dma(a1[:, :, 0:wd], S.YS[b, 1, :, r0:r0 + wd].rearrange("(kc p) t -> p kc t", p=128), reads=[S.t_YS[b]], writes=[ta1],
                  q="act")
            for kc in range(2):
                k.op("dve", lambda e: e.scalar_tensor_tensor(out=z_[:, kc, 0:wd], in0=u_[:, kc, 0:wd], scalar=dskip[:, kc:kc + 1],
                                                             in1=a0[:, kc, 0:wd], op0=ALU.mult, op1=ALU.add),
                     reads=[tu, ta0, tp], writes=[tz])
            k.op("dve", lambda e: e.tensor_tensor(out=z_[:, :, 0:wd], in0=z_[:, :, 0:wd], in1=a1[:, :, 0:wd][:, :, ::-1], op=ALU.add),
                 reads=[ta1], writes=[tz])
            k.op("pool", lambda e: e.tensor_tensor(out=a0[:, :, 0:wd], in0=z_[:, :, 0:wd], in1=z_[:, :, 0:wd], op=ALU.mult),
                 reads=[tz], writes=[ta0])
            k.op("dve", lambda e: e.tensor_scalar(out=a0[:, :, 0:wd], in0=a0[:, :, 0:wd], scalar1=0.044715, scalar2=1.0,
                                                  op0=ALU.mult, op1=ALU.add), reads=[ta0], writes=[ta0])
            k.op("pool", lambda e: e.tensor_tensor(out=a0[:, :, 0:wd], in0=a0[:, :, 0:wd], in1=z_[:, :, 0:wd], op=ALU.mult),
                 reads=[tz, ta0], writes=[ta0])
            k.op("act", lambda e: e.activation(out=a0[:, :, 0:wd], in_=a0[:, :, 0:wd], func=AF.Sigmoid, scale=GC),
                 reads=[ta0], writes=[ta0])
            k.op("dve", lambda e: e.tensor_tensor(out=z_[:, :, 0:wd], in0=z_[:, :, 0:wd], in1=a0[:, :, 0:wd], op=ALU.mult),
                 reads=[ta0], writes=[tz])
            k.op("pool", lambda e: e.tensor_copy(out=zb_[:, :, 0:wd], in_=z_[:, :, 0:wd]), reads=[tz], writes=[tzb])
            for oc in range(2):
                ps, tps = k.ps()
                for kc in range(2):
                    k.op("pe", lambda e: e.matmul(ps[:, 0:wd], gluw[:, kc, oc * 128:(oc + 1) * 128], zb_[:, kc, 0:wd],
                                                  start=(kc == 0), stop=(kc == 1)), reads=[tp, tzb], writes=[tps])
                k.op("act", lambda e: e.activation(out=a1[:, oc, 0:wd], in_=ps[:, 0:wd], func=AF.Sigmoid, bias=glub[:, oc:oc + 1],
                                                   scale=1.0), reads=[tps, tp], writes=[ta1])
            k.op("dve", lambda e: e.tensor_tensor(out=ob_[:, :, 0:wd], in0=z_[:, :, 0:wd], in1=a1[:, :, 0:wd], op=ALU.mult),
                 reads=[tz, ta1], writes=[tob])
            k.dma(S.YT[b, 256:512, t0:t0 + wd].rearrange("(kc p) t -> p kc t", p=128), ob_[:, :, 0:wd], reads=[tob],
                  writes=[S.t_YT[b]])
    k.barrier()
    st.close()


def stage_rwkv_conv(S, l):
    k, cfg = S.k, S.cfg
    NB, C, L, T = cfg.NB, cfg.C, cfg.L, cfg.T
    st = contextlib.ExitStack()
    nc = k.nc
    cwT = k.sbuf(st, "cwT", [128, 8, 3]); tcw = Tr()
    with nc.allow_non_contiguous_dma(reason="tiny param load"):
        for j in range(3):
            k.dma(cwT[:, :, j], S.inp["rwkv_conv"][l, j].rearrange("(kc p) -> p kc", p=128), writes=[tcw])
    pins = [(k.sbuf(st, "pin", [128, 8, 514]), Tr()) for _ in range(2)]
    pos = [(k.sbuf(st, "po", [128, 8, 512]), Tr()) for _ in range(2)]
    it = 0
    for b in range(NB):
        for (s0, s1) in ((0, C), (C, T)):
            for (t0, wd) in tiles(s0, s1, 512):
                pin, tpin = pins[it % 2]; po, tpo = pos[it % 2]
                it += 1
                lo = max(t0 - 1, s0); hi = min(t0 + wd + 1, s1)
                if t0 == s0:
                    k.op("pool", lambda e: e.memset(pin[:, :, 0:1], 0.0), writes=[tpin])
                if t0 + wd == s1:
                    k.op("pool", lambda e: e.memset(pin[:, :, wd + 1:wd + 2], 0.0), writes=[tpin])
                o0 = lo - (t0 - 1)
                for kc in range(8):
                    k.dma(pin[:, kc, o0:o0 + (hi - lo)], S.PR[b, kc * 128:(kc + 1) * 128, lo:hi], reads=[S.t_PR[b]], writes=[tpin],
                          q=("sp" if kc % 2 == 0 else "act"))
                for kc in range(8):
                    en = "dve"
                    k.op(en, lambda e: e.tensor_scalar(out=po[:, kc, 0:wd], in0=pin[:, kc, 0:wd], scalar1=cwT[:, kc, 0:1], scalar2=None,
                                                       op0=ALU.mult), reads=[tpin, tcw], writes=[tpo])
                    for j in (1, 2):
                        k.op(en, lambda e: e.scalar_tensor_tensor(out=po[:, kc, 0:wd], in0=pin[:, kc, j:j + wd], scalar=cwT[:, kc, j:j + 1],
                                                                  in1=po[:, kc, 0:wd], op0=ALU.mult, op1=ALU.add),
                             reads=[tpin, tcw], writes=[tpo])
                for kc in range(8):
                    k.dma(S.PC[b, kc * 128:(kc + 1) * 128, t0:t0 + wd], po[:, kc, 0:wd], reads=[tpo], writes=[S.t_PC[b]])
    k.barrier()
    st.close()


def stage_rwkv(S, l, last):
    stage_rwkv_conv(S, l)
    k, cfg = S.k, S.cfg
    NB, C, L, T = cfg.NB, cfg.C, cfg.L, cfg.T
    st = contextlib.ExitStack()
    nc = k.nc
    CH = 128
    tp = Tr()
    P = lambda name, shape, dt=F32: k.sbuf(st, name, shape, dt)
    kkp = P("kkp", [64, 4]); kap = P("kap", [64, 4]); omka = P("omka", [64, 4]); rkp = P("rkp", [64, 4])
    a0T = P("a0T", [64, 2, 4])
    with nc.allow_non_contiguous_dma(reason="tiny param loads"):
        k.dma(kkp[:], S.inp["rwkv_k_k"][l].rearrange("(h p) -> p h", p=64), writes=[tp])
        k.dma(kap[:], S.inp["rwkv_k_a"][l].rearrange("(h p) -> p h", p=64), writes=[tp])
        k.dma(rkp[:], S.inp["rwkv_r_k"][l].rearrange("h p -> p h"), writes=[tp])
        for d in range(2):
            k.dma(a0T[:, d, :], S.inp["rwkv_a0"][l, d].rearrange("(h p) -> p h", p=64), writes=[tp])
    k.op("dve", lambda e: e.tensor_scalar(out=omka[:], in0=kap[:], scalar1=-1.0, scalar2=1.0, op0=ALU.mult, op1=ALU.add),
         reads=[tp], writes=[tp])
    w2a = P("w2a", [65, 2, 256]); a2 = P("a2", [64, 2, 256]); g2 = P("g2", [128, 256])
    for d in range(2):
        k.dma(w2a[0:64, d, :], S.inp["rwkv_w2"][l, d], writes=[tp])
        k.dma(w2a[64:65, d, :], S.inp["rwkv_w0"][l, d:d + 1, :], writes=[tp])
        k.dma(a2[:, d, :], S.inp["rwkv_a2"][l, d], writes=[tp])
    k.dma(g2[:], S.inp["rwkv_g2"][l], writes=[tp])
    tri = P("tri", [128, 4, 128]); mskb = P("mskb", [128, 4, 128], BF16)
    k.dma(tri[:].rearrange("p a b -> p (a b)"), S.inp["tri"][:, :], writes=[tp])
    k.dma(mskb[:].rearrange("p a b -> p (a b)"), S.inp["msk"][:, :], writes=[tp], q="pool")
    ones64 = P("ones64", [64, 64])
    k.op("dve", lambda e: e.memset(ones64[:], 1.0), reads=[tp], writes=[tp])
    t_gn = Tr()
    gnw = P("gnw", [128, 256]); gnb = P("gnb", [128, 256])
    for (dst, nm) in ((gnw, "rwkv_gn_w"), (gnb, "rwkv_gn_b")):
        row = P("gnrow", [1, 256]); trow = Tr()
        k.dma(row[:], S.inp[nm][l:l + 1, :], writes=[trow])
        onesr = P("gnones", [1, 128])
        k.op("dve", lambda e: e.memset(onesr[:], 1.0), writes=[trow])
        ps, tps = k.ps()
        k.op("pe", lambda e: e.matmul(ps[:, 0:256], onesr[0:1, :], row[0:1, :], start=True, stop=True), reads=[trow], writes=[tps])
        k.op("act", lambda e: e.copy(out=dst[:], in_=ps[:, 0:256]), reads=[tps], writes=[t_gn])

    NS = max(NB, 1)
    def mk(name, shape, dt=F32):
        return [(k.sbuf(st, name, shape, dt), Tr()) for _ in range(NS)]
    rkv_s = mk("rkv", [64, 3, 4, CH]); wl_s = mk("wl", [65, CH]); al_s = mk("al", [64, CH]); gl_s = mk("gl", [128, CH])
    kq_s = mk("kq", [64, 4, CH]); tA_s = mk("tA", [64, 4, CH]); kk_s = mk("kk", [64, 4, CH])
    lw_s = mk("lw", [128, 256]); Ep_s = mk("Ep", [64, 4, CH]); Em_s = mk("Em", [64, 4, CH]); Ex_s = mk("Ex", [64, 4, CH])
    a_s = mk("a", [64, 4, CH]); kdir_s = mk("kdir", [64, 4, CH]); bv_s = mk("bv", [64, 4, CH])
    kkq_s = mk("kkq", [64, 4, CH]); rq_s = mk("rq", [64, 4, CH]); kd_s = mk("kd", [64, 4, CH]); bd_s = mk("bd", [64, 4, CH])
    kE_s = mk("kE", [64, 4, CH]); bE_s = mk("bE", [64, 4, CH])
    fb_s = mk("fb", [64, 4, 4, CH], BF16)
    tm_s = mk("tm", [128, 3, 4, 64], BF16)
    vtm_s = mk("vtm", [128, 4, 64]); vtb_s = mk("vtb", [128, 4, 64], BF16)
    A_s = mk("A", [128, 5, 4, CH], BF16)
    ivs = [{nm: (k.sbuf(st, "iv" + nm, [128, 4, CH], BF16), Tr()) for nm in
            ("N0", "N0T", "Xa", "Xb", "XTa", "XTb", "P0", "P1", "PT0", "PT1", "W", "W2")} for _ in range(NS)]
    bmskb = P("bmskb", [128, 4, 128], BF16)
    k.dma(bmskb[:].rearrange("p a b -> p (a b)"), S.inp["bmsk"][:, :], writes=[tp], q="pool")
    rhs2_s = mk("rhs2", [128, 4, 128], BF16); mu_s = mk("mu", [128, 4, 128], BF16)
    GT_s = mk("GT", [64, 4, 64]); J_s = mk("J", [64, 4, 64]); RT_s = mk("RT", [64, 4, CH])
    ys_s = mk("ysb", [128, 260]); yd0_s = mk("yd0", [128, 260])
    rk_s = mk("rk", [64, 4, CH])
    yc_s = mk("yc", [128, 4, 64]); y2_s = mk("y2", [128, 4, 64]); st_s = mk("stt", [128, 16]); gt_s = mk("gts", [128, 256])
    ot_s = mk("ot", [128, 2, CH], BF16)
    Hb = [[(P("H", [64, 4, 64]), Tr()) for _ in range(2)] for _ in range(NB * 2)]
    def chain(b, d):
        if True:
            Hs = Hb[b * 2 + d]
            hcur = 0
            k.op("dve", lambda e: e.memset(Hs[0][0][:], 0.0), writes=[Hs[0][1]])
            ctxc = list(range(0, C, CH)); latc = list(range(C, T, CH))
            order = (ctxc + latc) if d == 0 else (ctxc[::-1] + latc[::-1])
            iend = CH - 1 if d == 0 else 0
            m_strict, m_incl, m_nt = (0, 1, 2) if d == 0 else (2, 3, 0)
            tri_i, tri_x = (0, 1) if d == 0 else (2, 3)
            for t0 in order:
                emit = not (last and t0 < C)
                s = b % NS
                yield
                g = lambda lst: lst[s]
                rkv, trkv = g(rkv_s); wl, twl = g(wl_s); al, tal = g(al_s); gl, tgl = g(gl_s)
                for f in range(3):
                    k.dma(rkv[:, f], S.PC[b, f * 256:(f + 1) * 256, t0:t0 + CH].rearrange("(h p) t -> p h t", p=64),
                          reads=[S.t_PC[b]], writes=[trkv], q=("sp" if f != 1 else "act"))
                k.dma(wl[0:64, :], S.PC[b, 768:832, t0:t0 + CH], reads=[S.t_PC[b]], writes=[twl])
                k.dma(al[:], S.PC[b, 832:896, t0:t0 + CH], reads=[S.t_PC[b]], writes=[tal], q="act")
                if emit and d == 1:
                    k.dma(gl[:], S.PC[b, 896:1024, t0:t0 + CH], reads=[S.t_PC[b]], writes=[tgl])
                r_, k_, v_ = rkv[:, 0], rkv[:, 1], rkv[:, 2]
                B3 = lambda ap2: ap2.unsqueeze(2).to_broadcast([64, 4, CH])
                def tt(en, out, in0, in1, op, rd, wr):
                    k.op(en, lambda e: e.tensor_tensor(out=out, in0=in0, in1=in1, op=op), reads=rd, writes=wr)
                kq, tkq = g(kq_s); tA, ttA = g(tA_s); kk, tkk = g(kk_s)
                tt("dve", kq[:], k_, B3(kkp[:, :]), ALU.mult, [trkv, tp], [tkq])
                tt("pool", tA[:], kq[:], kq[:], ALU.mult, [tkq], [ttA])
                ps, tps = k.ps()
                k.op("pe", lambda e: e.matmul(ps[0:64, 0:512], ones64[:, :], tA[:].rearrange("p h t -> p (h t)"), start=True, stop=True),
                     reads=[ttA, tp], writes=[tps])
                k.op("dve", lambda e: e.tensor_scalar(out=tA[:].rearrange("p h t -> p (h t)"), in0=ps[0:64, 0:512], scalar1=1e-12,
                                                      scalar2=None, op0=ALU.add), reads=[tps], writes=[ttA])
                k.op("act", lambda e: e.activation(out=tA[:], in_=tA[:], func=AF.Sqrt), reads=[ttA], writes=[ttA])
                k.op("dve", lambda e: e.reciprocal(out=tA[:], in_=tA[:]), reads=[ttA], writes=[ttA])
                tt("dve", kk[:], kq[:], tA[:], ALU.mult, [tkq, ttA], [tkk])
                k.op("act", lambda e: e.activation(out=wl[0:64, :], in_=wl[0:64, :], func=AF.Tanh), reads=[twl], writes=[twl])
                k.op("dve", lambda e: e.memset(wl[64:65, :], 1.0), reads=[twl], writes=[twl])
                yield
                lw, tlw = g(lw_s)
                ps, tps = k.ps()
                k.op("pe", lambda e: e.matmul(ps[:, 0:256], wl[:, :], w2a[:, d, :], start=True, stop=True), reads=[twl, tp], writes=[tps])
                k.op("act", lambda e: e.activation(out=lw[:], in_=ps[:, 0:256], func=AF.Sigmoid), reads=[tps], writes=[tlw])
                pL, tpL = k.ps()
                pX, tpX = k.ps()
                for h in range(4):
                    k.op("pe", lambda e: e.matmul(pL[0:64, h * CH:(h + 1) * CH], lw[:, h * 64:(h + 1) * 64], tri[:, tri_i, :],
                                                  start=True, stop=True), reads=[tlw, tp], writes=[tpL])
                    k.op("pe", lambda e: e.matmul(pX[0:64, h * CH:(h + 1) * CH], lw[:, h * 64:(h + 1) * 64], tri[:, tri_x, :],
                                                  start=True, stop=True), reads=[tlw, tp], writes=[tpX])
                Ep, tEp = g(Ep_s); Em, tEm = g(Em_s); Ex, tEx = g(Ex_s)
                F2 = lambda t_: t_[:].rearrange("p h t -> p (h t)")
                k.op("act", lambda e: e.activation(out=F2(Ep), in_=pL[0:64, 0:512], func=AF.Exp), reads=[tpL], writes=[tEp])
                k.op("act", lambda e: e.activation(out=F2(Em), in_=pL[0:64, 0:512], func=AF.Exp, scale=-1.0), reads=[tpL], writes=[tEm])
                k.op("act", lambda e: e.activation(out=F2(Ex), in_=pX[0:64, 0:512], func=AF.Exp), reads=[tpX], writes=[tEx])
                a_, ta_ = g(a_s)
                pa, tpa = k.ps()
                for h in range(4):
                    k.op("pe", lambda e: e.matmul(pa[0:64, h * CH:(h + 1) * CH], a2[:, d, h * 64:(h + 1) * 64], al[:, :], start=True, stop=True),
                         reads=[tal, tp], writes=[tpa])
                for h in range(4):
                    k.op("act", lambda e: e.activation(out=a_[:, h, :], in_=pa[0:64, h * CH:(h + 1) * CH], func=AF.Sigmoid,
                                                       bias=a0T[:, d, h:h + 1], scale=1.0), reads=[tpa, tp], writes=[ta_])
                yield
                kdir, tkd_ = g(kdir_s); bv, tbv = g(bv_s)
                tt("dve", kdir[:], a_[:], B3(kap[:, :]), ALU.mult, [ta_, tp], [tkd_])
                tt("pool", kdir[:], kdir[:], B3(omka[:, :]), ALU.add, [tp], [tkd_])
                tt("dve", kdir[:], kdir[:], k_, ALU.mult, [trkv], [tkd_])
                tt("pool", bv[:], kk[:], a_[:], ALU.mult, [tkk, ta_], [tbv])
                kkq, tkkq = g(kkq_s); rq, trq = g(rq_s); kd, tkd = g(kd_s); bd, tbd = g(bd_s); kE, tkE = g(kE_s); bE, tbE = g(bE_s)
                tt("dve", kkq[:], kk[:], Ex[:], ALU.mult, [tkk, tEx], [tkkq])
                tt("pool", rq[:], r_, Ep[:], ALU.mult, [trkv, tEp], [trq])
                tt("dve", kd[:], kdir[:], Em[:], ALU.mult, [tkd_, tEm], [tkd])
                tt("pool", bd[:], bv[:], Em[:], ALU.mult, [tbv, tEm], [tbd])
                wCb = Ep[:, :, iend:iend + 1].to_broadcast([64, 4, CH])
                tt("dve", kE[:], kd[:], wCb, ALU.mult, [tkd, tEp], [tkE])
                tt("pool", bE[:], bd[:], wCb, ALU.mult, [tbd, tEp], [tbE])
                fb, tfb = g(fb_s)
                for i_, (src, tsrc) in enumerate(((kkq, tkkq), (rq, trq), (kd, tkd), (bd, tbd))):
                    k.op("pool" if i_ % 2 else "act", (lambda e: e.tensor_copy(out=fb[:, i_], in_=src[:])) if i_ % 2 else
                         (lambda e: e.copy(out=fb[:, i_], in_=src[:])), reads=[tsrc], writes=[tfb])
                yield
                tm, ttm = g(tm_s); vtm, tvtm = g(vtm_s); vtb, tvtb = g(vtb_s)
                for i_, (src, tsrc) in enumerate(((kkq, tkkq), (kE, tkE), (bE, tbE))):
                    ps, tps = k.ps()
                    for h in range(4):
                        k.op("pe", lambda e: e.transpose(out=ps[:, h * 64:(h + 1) * 64], in_=src[:, h, :], identity=S.ident[0:64, 0:64]),
                             reads=[tsrc, S.t_const], writes=[tps])
                    k.op("act" if i_ % 2 else "dve", (lambda e: e.copy(out=tm[:, i_].rearrange("p h v -> p (h v)"), in_=ps[:, 0:256])) if i_ % 2
                         else (lambda e: e.tensor_copy(out=tm[:, i_].rearrange("p h v -> p (h v)"), in_=ps[:, 0:256])), reads=[tps], writes=[ttm])
                ps, tps = k.ps()
                for h in range(4):
                    k.op("pe", lambda e: e.transpose(out=ps[:, h * 64:(h + 1) * 64], in_=v_[:, h, :], identity=S.ident[0:64, 0:64]),
                         reads=[trkv, S.t_const], writes=[tps])
                k.op("act", lambda e: e.copy(out=vtm[:].rearrange("p h v -> p (h v)"), in_=ps[:, 0:256]), reads=[tps], writes=[tvtm])
                k.op("dve", lambda e: e.tensor_copy(out=vtb[:].rearrange("p h v -> p (h v)"), in_=ps[:, 0:256]), reads=[tps], writes=[tvtb])
                yield
                A, tA5 = g(A_s)
                specs = ((3, 0, m_strict), (0, 3, m_nt), (2, 0, m_strict), (2, 1, m_incl), (3, 1, m_incl))
                for ai, (li, ri, mi) in enumerate(specs):
                    ps, tps = k.ps()
                    for h in range(4):
                        k.op("pe", lambda e: e.matmul(ps[:, h * CH:(h + 1) * CH], fb[:, li, h, :], fb[:, ri, h, :], start=True, stop=True),
                             reads=[tfb], writes=[tps])
                    k.op("dve", lambda e: e.tensor_tensor(out=A[:, ai], in0=ps[:, 0:512].rearrange("p (h t) -> p h t", h=4),
                                                          in1=mskb[:, mi:mi + 1, :].to_broadcast([128, 4, CH]), op=ALU.mult),
                         reads=[tps, tp], writes=[tA5])
                yield
                F3 = lambda t_: t_[:].rearrange("p h t -> p (h t)")
                def mmg(lhs, tl, rhs, tr_):
                    ps_, tps_ = k.ps()
                    for h in range(4):
                        k.op("pe", lambda e: e.matmul(ps_[:, h * CH:(h + 1) * CH], lhs[:, h, :], rhs[:, h, :], start=True, stop=True),
                             reads=[tl, tr_], writes=[tps_])
                    return ps_, tps_
                def bm(i_):
                    return bmskb[:, i_:i_ + 1, :].to_broadcast([128, 4, CH])
                iv = ivs[s]
                N0, tN0 = iv["N0"]; N0T, tN0T = iv["N0T"]
                Xc, tXc = iv["Xa"]; XTc, tXTc = iv["XTa"]
                Xo, tXo = iv["Xb"]; XTo, tXTo = iv["XTb"]
                tt("pool", N0[:], A[:, 0], bm(0), ALU.mult, [tA5, tp], [tN0])
                tt("pool", N0T[:], A[:, 1], bm(0), ALU.mult, [tA5, tp], [tN0T])
                idb = S.identb[:, :].unsqueeze(1).to_broadcast([128, 4, CH])
                tt("dve", Xc[:], idb, N0[:], ALU.subtract, [tN0, S.t_const], [tXc])
                tt("dve", XTc[:], idb, N0T[:], ALU.subtract, [tN0T, S.t_const], [tXTc])
                Pc, tPc, PTc, tPTc = N0, tN0, N0T, tN0T
                for j in range(3):
                    Pn, tPn = iv["P%d" % (j % 2)]; PTn, tPTn = iv["PT%d" % (j % 2)]
                    ps_, tps_ = mmg(PTc, tPTc, Pc, tPc)
                    k.op("act", lambda e: e.copy(out=F3(Pn), in_=ps_[:, 0:512]), reads=[tps_], writes=[tPn])
                    ps_, tps_ = mmg(Pc, tPc, PTc, tPTc)
                    k.op("dve", lambda e: e.tensor_copy(out=F3(PTn), in_=ps_[:, 0:512]), reads=[tps_], writes=[tPTn])
                    ps_, tps_ = mmg(PTn, tPTn, Xc, tXc)
                    k.op("dve", lambda e: e.tensor_tensor(out=F3(Xo), in0=ps_[:, 0:512], in1=F3(Xc), op=ALU.add), reads=[tps_, tXc], writes=[tXo])
                    ps_, tps_ = mmg(Pn, tPn, XTc, tXTc)
                    k.op("dve", lambda e: e.tensor_tensor(out=F3(XTo), in0=ps_[:, 0:512], in1=F3(XTc), op=ALU.add), reads=[tps_, tXTc], writes=[tXTo])
                    Xc, tXc, Xo, tXo = Xo, tXo, Xc, tXc
                    XTc, tXTc, XTo, tXTo = XTo, tXTo, XTc, tXTc
                    Pc, tPc, PTc, tPTc = Pn, tPn, PTn, tPTn
                yield
                for lv in range(3):
                    Np, tNp = iv["P0"]; NpT, tNpT = iv["PT0"]
                    Wt, tWt = iv["W"]; Wt2, tWt2 = iv["W2"]
                    tt("pool", Np[:], A[:, 0], bm(1 + lv), ALU.mult, [tA5, tp], [tNp])
                    tt("pool", NpT[:], A[:, 1], bm(1 + lv), ALU.mult, [tA5, tp], [tNpT])
                    ps_, tps_ = mmg(NpT, tNpT, Xc, tXc)
                    k.op("act", lambda e: e.copy(out=F3(Wt), in_=ps_[:, 0:512]), reads=[tps_], writes=[tWt])
                    if lv < 2:
                        ps_, tps_ = mmg(Np, tNp, XTc, tXTc)
                        k.op("dve", lambda e: e.tensor_copy(out=F3(Wt2), in_=ps_[:, 0:512]), reads=[tps_], writes=[tWt2])
                    ps_, tps_ = mmg(XTc, tXTc, Wt, tWt)
                    k.op("dve", lambda e: e.tensor_tensor(out=F3(Xo), in0=F3(Xc), in1=ps_[:, 0:512], op=ALU.subtract), reads=[tps_, tXc], writes=[tXo])
                    if lv < 2:
                        ps_, tps_ = mmg(Xc, tXc, Wt2, tWt2)
                        k.op("dve", lambda e: e.tensor_tensor(out=F3(XTo), in0=F3(XTc), in1=ps_[:, 0:512], op=ALU.subtract),
                             reads=[tps_, tXTc], writes=[tXTo])
                        XTc, tXTc, XTo, tXTo = XTo, tXTo, XTc, tXTc
                    Xc, tXc, Xo, tXo = Xo, tXo, Xc, tXc
                    yield
                rhs2, trh = g(rhs2_s); mu, tmu = g(mu_s)
                ps, tps = k.ps()
                for h in range(4):
                    k.op("pe", lambda e: e.matmul(ps[:, h * 64:(h + 1) * 64], A[:, 2, h, :], vtb[:, h, :], start=True, stop=True),
                         reads=[tA5, tvtb], writes=[tps])
                k.op("dve", lambda e: e.tensor_scalar(out=rhs2[:, :, 64:128], in0=ps[:, 0:256].rearrange("p (h v) -> p h v", h=4), scalar1=-1.0,
                                                      scalar2=None, op0=ALU.mult), reads=[tps], writes=[trh])
                k.op("pool", lambda e: e.tensor_copy(out=rhs2[:, :, 0:64], in_=tm[:, 0]), reads=[ttm], writes=[trh])
                ps, tps = k.ps()
                for h in range(4):
                    k.op("pe", lambda e: e.matmul(ps[:, h * 128:(h + 1) * 128], Xc[:, h, :], rhs2[:, h, :], start=True, stop=True),
                         reads=[tXc, trh], writes=[tps])
                k.op("act", lambda e: e.copy(out=mu[:].rearrange("p h t -> p (h t)"), in_=ps[:, 0:512]), reads=[tps], writes=[tmu])
                yield
                GT, tGT = g(GT_s); J, tJ = g(J_s); RT, tRT = g(RT_s)
                ps, tps = k.ps()
                for h in range(4):
                    k.op("pe", lambda e: e.matmul(ps[0:64, h * 64:(h + 1) * 64], mu[:, h, 0:64], tm[:, 2, h, :], start=True, stop=True),
                         reads=[tmu, ttm], writes=[tps])
                for h in range(4):
                    k.op("dve", lambda e: e.scalar_tensor_tensor(out=GT[:, h, :], in0=S.ident[0:64, 0:64], scalar=Ep[:, h, iend:iend + 1],
                                                                 in1=ps[0:64, h * 64:(h + 1) * 64], op0=ALU.mult, op1=ALU.subtract),
                         reads=[tps, tEp, S.t_const], writes=[tGT])
                ps, tps = k.ps()
                for h in range(4):
                    k.op("pe", lambda e: e.matmul(ps[0:64, h * 64:(h + 1) * 64], tm[:, 1, h, :], vtb[:, h, :], start=True, stop=False),
                         reads=[ttm, tvtb], writes=[tps])
                    k.op("pe", lambda e: e.matmul(ps[0:64, h * 64:(h + 1) * 64], tm[:, 2, h, :], mu[:, h, 64:128], start=False, stop=True),
                         reads=[ttm, tmu], writes=[tps])
                k.op("act", lambda e: e.copy(out=J[:].rearrange("p h v -> p (h v)"), in_=ps[0:64, 0:256]), reads=[tps], writes=[tJ])
                Hc, tHc = Hs[hcur]
                Hn, tHn = Hs[1 - hcur]
                if emit:
                    ps, tps = k.ps()
                    for h in range(4):
                        k.op("pe", lambda e: e.matmul(ps[0:64, h * CH:(h + 1) * CH], mu[:, h, 0:64], A[:, 4, h, :], start=True, stop=True),
                             reads=[tmu, tA5], writes=[tps])
                    k.op("dve", lambda e: e.tensor_tensor(out=RT[:].rearrange("p h t -> p (h t)"), in0=rq[:].rearrange("p h t -> p (h t)"),
                                                          in1=ps[0:64, 0:512], op=ALU.subtract), reads=[tps, trq], writes=[tRT])
                    py, tpy = k.ps()
                    for h in range(4):
                        k.op("pe", lambda e: e.matmul(py[:, h * 64:(h + 1) * 64], A[:, 3, h, :], vtb[:, h, :], start=True, stop=False),
                             reads=[tA5, tvtb], writes=[tpy])
                        k.op("pe", lambda e: e.matmul(py[:, h * 64:(h + 1) * 64], A[:, 4, h, :], mu[:, h, 64:128], start=False, stop=False),
                             reads=[tA5, tmu], writes=[tpy])
                        k.op("pe", lambda e: e.matmul(py[:, h * 64:(h + 1) * 64], RT[:, h, :], Hc[:, h, :], start=False, stop=True),
                             reads=[tRT, tHc], writes=[tpy])
                    rk, trk = g(rk_s)
                    tt("pool", rk[:], r_, kdir[:], ALU.mult, [trkv, tkd_], [trk])
                    tt("pool", rk[:], rk[:], B3(rkp[:, :]), ALU.mult, [tp], [trk])
                    for h in range(4):
                        k.op("pe", lambda e: e.matmul(py[:, 256 + h:257 + h], rk[:, h, :], ones64[:, 0:1], start=True, stop=True),
                             reads=[trk, tp], writes=[tpy])
                    ysb, tys = g(ys_s)
                    if d == 0:
                        k.op("act", lambda e: e.copy(out=ysb[:, 0:260], in_=py[:, 0:260]), reads=[tpy], writes=[tys])
                        k.dma(S.YD[b, t0:t0 + CH, :], ysb[:, :], reads=[tys], writes=[S.t_YD[b]])
                    else:
                        yd0, tyd0 = g(yd0_s)
                        k.dma(yd0[:], S.YD[b, t0:t0 + CH, :], reads=[S.t_YD[b]], writes=[tyd0])
                        k.op("dve", lambda e: e.tensor_tensor(out=ysb[:, 0:260], in0=py[:, 0:260], in1=yd0[:, 0:260], op=ALU.add),
                             reads=[tpy, tyd0], writes=[tys])
                yield
                ph, tph = k.ps()
                for h in range(4):
                    k.op("pe", lambda e: e.matmul(ph[0:64, h * 64:(h + 1) * 64], GT[:, h, :], Hc[:, h, :], start=True, stop=True),
                         reads=[tGT, tHc], writes=[tph])
                k.op("dve", lambda e: e.tensor_tensor(out=Hn[:].rearrange("p h v -> p (h v)"), in0=ph[0:64, 0:256],
                                                      in1=J[:].rearrange("p h v -> p (h v)"), op=ALU.add), reads=[tph, tJ], writes=[tHn])
                hcur = 1 - hcur
                if not emit:
                    continue
                yield
                ysb, tys = g(ys_s)
                if d == 0:
                    continue
                yc, tyc = g(yc_s); y2, ty2 = g(y2_s); stt, tst = g(st_s); gts, tgts = g(gt_s)
                y3 = ysb[:, 0:256].rearrange("p (h v) -> p h v", h=4)
                k.op("dve", lambda e: e.tensor_reduce(out=stt[:, 0:4], in_=y3, axis=AX.X, op=ALU.add), reads=[tys], writes=[tst])
                k.op("dve", lambda e: e.tensor_scalar(out=stt[:, 0:4], in0=stt[:, 0:4], scalar1=1.0 / 64, scalar2=None, op0=ALU.mult),
                     reads=[tst], writes=[tst])
                k.op("dve", lambda e: e.tensor_tensor(out=yc[:], in0=y3, in1=stt[:, 0:4].unsqueeze(2).to_broadcast([128, 4, 64]),
                                                      op=ALU.subtract), reads=[tys, tst], writes=[tyc])
                k.op("pool", lambda e: e.tensor_tensor(out=y2[:], in0=yc[:], in1=yc[:], op=ALU.mult), reads=[tyc], writes=[ty2])
                k.op("dve", lambda e: e.tensor_reduce(out=stt[:, 4:8], in_=y2[:], axis=AX.X, op=ALU.add), reads=[ty2], writes=[tst])
                k.op("dve", lambda e: e.tensor_scalar(out=stt[:, 4:8], in0=stt[:, 4:8], scalar1=1.0 / 64, scalar2=GN_EPS, op0=ALU.mult,
                                                      op1=ALU.add), reads=[tst], writes=[tst])
                k.op("act", lambda e: e.activation(out=stt[:, 4:8], in_=stt[:, 4:8], func=AF.Sqrt), reads=[tst], writes=[tst])
                k.op("dve", lambda e: e.reciprocal(out=stt[:, 4:8], in_=stt[:, 4:8]), reads=[tst], writes=[tst])
                k.op("dve", lambda e: e.tensor_tensor(out=yc[:], in0=yc[:], in1=stt[:, 4:8].unsqueeze(2).to_broadcast([128, 4, 64]),
                                                      op=ALU.mult), reads=[tst], writes=[tyc])
                ycf = yc[:].rearrange("p h v -> p (h v)")
                k.op("pool", lambda e: e.tensor_tensor(out=ycf, in0=ycf, in1=gnw[:], op=ALU.mult), reads=[t_gn], writes=[tyc])
                k.op("pool", lambda e: e.tensor_tensor(out=ycf, in0=ycf, in1=gnb[:], op=ALU.add), reads=[t_gn], writes=[tyc])
                yield
                k.op("dve", lambda e: e.tensor_tensor(out=y2[:], in0=vtm[:], in1=ysb[:, 256:260].unsqueeze(2).to_broadcast([128, 4, 64]),
                                                      op=ALU.mult), reads=[tvtm, tys], writes=[ty2])
                k.op("dve", lambda e: e.tensor_tensor(out=yc[:], in0=yc[:], in1=y2[:], op=ALU.add), reads=[ty2], writes=[tyc])
                k.op("act", lambda e: e.activation(out=gl[:], in_=gl[:], func=AF.Sigmoid), reads=[tgl], writes=[tgl])
                pg, tpg = k.ps()
                k.op("pe", lambda e: e.matmul(pg[:, 0:256], gl[:, :], g2[:, :], start=True, stop=True), reads=[tgl, tp], writes=[tpg])
                k.op("dve", lambda e: e.tensor_tensor(out=gts[:], in0=pg[:, 0:256], in1=ycf, op=ALU.mult), reads=[tpg, tyc], writes=[tgts])
                ot, tot = g(ot_s)
                ps, tps = k.ps()
                for c2 in range(2):
                    k.op("pe", lambda e: e.transpose(out=ps[:, c2 * CH:(c2 + 1) * CH], in_=gts[:, c2 * 128:(c2 + 1) * 128], identity=S.ident[:, :]),
                         reads=[tgts, S.t_const], writes=[tps])
                k.op("act", lambda e: e.copy(out=ot[:].rearrange("p c t -> p (c t)"), in_=ps[:, 0:256]), reads=[tps], writes=[tot])
                k.dma(S.YT[b, 0:256, t0:t0 + CH].rearrange("(c p) t -> p c t", p=128), ot[:], reads=[tot], writes=[S.t_YT[b]])
    for d in range(2):
        gens = [chain(b, d) for b in range(NB)]
        for gi_, g_ in enumerate(gens[:-1]):
            for _ in range(RWKV_STAGGER * (len(gens) - 1 - gi_)):
                next(g_)
        while gens:
            for g_ in list(gens):
                try:
                    next(g_)
                except StopIteration:
                    gens.remove(g_)
    k.barrier()
    st.close()


def stage_moe2(S, l, last):
    k, cfg = S.k, S.cfg
    NB, C, L, T = cfg.NB, cfg.C, cfg.L, cfg.T
    nc = k.nc
    st = contextlib.ExitStack()
    blocks = [(b, t0) for b in range(NB) for t0 in range(C if last else 0, T, 128)]
    NBK = len(blocks)
    Tn = NBK * 128
    NBLK = (2 * Tn + EB - 1) // EB + NEXP
    PMAX = NBLK * EB
    MAXB = (Tn + EB - 1) // EB
    R = NB + 1
    tp = Tr()
    P = lambda name, shape, dt=F32: k.sbuf(st, name, shape, dt)
    t_ln = Tr()
    lng = load_bcast_row(S, st, "lng2", S.inp["ln2_g"][l:l + 1, :], t_ln)
    lnb = load_bcast_row(S, st, "lnb2", S.inp["ln2_b"][l:l + 1, :], t_ln)
    wr = P("wr", [128, 8, 36])
    k.dma(wr[:, :, 0:4], S.inp["router_group_w"][l].rearrange("(kc p) n -> p kc n", p=128), writes=[tp])
    k.dma(wr[:, :, 4:36], S.inp["router_expert_w"][l].rearrange("(kc p) n -> p kc n", p=128), writes=[tp])
    rbias = P("rbias", [1, 36])
    k.dma(rbias[0:1, 0:4], S.inp["router_group_b"][l:l + 1, :], writes=[tp])
    k.dma(rbias[0:1, 4:36], S.inp["router_expert_b"][l:l + 1, :], writes=[tp])
    ones = P("onesm", [128, 128])
    k.op("dve", lambda e: e.memset(ones[:], 1.0), reads=[tp], writes=[tp])
    tris = P("tris", [128, 128])
    k.dma(tris[:], S.inp["msk"][:, 0:128], writes=[tp])
    iop = P("iop", [128, 1])
    k.dma(iop[:], S.inp["iotap"][:, :], writes=[tp])
    w12 = P("w12", [128, NBK, 2]); t_w12 = Tr()
    dsti = P("dsti", [128, NBK, 2], I32)
    widi = P("widi", [128, NBLK], I32)
    st1 = contextlib.ExitStack()
    P1 = lambda name, shape, dt=F32: k.sbuf(st1, name, shape, dt)
    scb = P1("scb", [128, R, D]); shb = P1("shb", [128, R, D]); dm = [(P1("dm", [128, 128]), Tr()) for _ in range(2)]
    di = 0
    for (dst, base) in ((scb, 32), (shb, 24)):
        for r in range(R):
            for half in range(2):
                ps, tps = k.ps()
                for f4 in range(4):
                    fc = half * 4 + f4
                    dmt, tdm = dm[di % 2]
                    di += 1
                    k.op("dve", lambda e: e.tensor_scalar(out=dmt[:], in0=S.ident[:, :], scalar1=S.mod[:, base + fc, r:r + 1],
                                                          scalar2=None, op0=ALU.mult), reads=[S.t_mod, S.t_const], writes=[tdm])
                    k.op("pe", lambda e: e.matmul(ps[:, f4 * 128:(f4 + 1) * 128], ones[:, :], dmt[:, :], start=True, stop=True),
                         reads=[tdm, tp], writes=[tps])
                k.op("act", lambda e: e.copy(out=dst[:, r, half * 512:(half + 1) * 512], in_=ps[:, 0:512]), reads=[tps], writes=[tp])
    OH = P1("OH", [128, NBK, 2, 32]); t_OH = Tr()
    x1s = [(P1("x1", [128, D]), Tr()) for _ in range(2)]
    hfs = [(P1("hf", [128, 8, 128]), Tr()) for _ in range(2)]
    hts = [(P1("ht", [128, D]), Tr()) for _ in range(2)]
    htb = [(k.sbuf(st1, "htb", [128, D], BF16), Tr()) for _ in range(2)]
    lga = P1("lga", [128, NBK, 36]); t_lga = Tr()
    it = 0
    for j, (b, t0) in enumerate(blocks):
        r = NB if t0 < C else b
        x1, tx1 = x1s[it % 2]; hf, thf = hfs[it % 2]; ht, tht = hts[it % 2]; hb_, thb = htb[it % 2]
        it += 1
        k.dma(x1[:], S.XR[b, t0:t0 + 128, :], reads=[S.t_XR[b]], writes=[tx1])
        k.op("pool", lambda e: e.tensor_tensor(out=ht[:], in0=x1[:], in1=scb[:, r, :], op=ALU.mult), reads=[tx1, tp], writes=[tht])
        k.op("pool", lambda e: e.tensor_tensor(out=hb_[:], in0=ht[:], in1=shb[:, r, :], op=ALU.add), reads=[tht, tp], writes=[thb])
        k.dma(S.HS[j * 128:(j + 1) * 128, :], hb_[:], reads=[thb], writes=[S.t_HS])
        for hh in range(2):
            ps, tps = k.ps()
            for f4 in range(4):
                fc = hh * 4 + f4
                k.op("pe", lambda e: e.transpose(out=ps[:, f4 * 128:(f4 + 1) * 128], in_=x1[:, fc * 128:(fc + 1) * 128],
                                                 identity=S.ident[:, :]), reads=[tx1, S.t_const], writes=[tps])
            for f4 in range(4):
                fc = hh * 4 + f4
                k.op("act", lambda e: e.activation(out=hf[:, fc, :], in_=ps[:, f4 * 128:(f4 + 1) * 128], func=AF.Identity,
                                                   scale=S.mod[:, 32 + fc, r:r + 1], bias=S.mod[:, 24 + fc, r:r + 1]),
                     reads=[tps, S.t_mod], writes=[thf])
        ps, tps = k.ps()
        for fc in range(8):
            k.op("pe", lambda e: e.matmul(ps[:, 0:36], hf[:, fc, :], wr[:, fc, :], start=(fc == 0), stop=False),
                 reads=[thf, tp], writes=[tps])
        k.op("pe", lambda e: e.matmul(ps[:, 0:36], ones[0:1, :], rbias[0:1, :], start=False, stop=True), reads=[tp], writes=[tps])
        k.op("act", lambda e: e.copy(out=lga[:, j, :], in_=ps[:, 0:36]), reads=[tps], writes=[t_lga])
    RB = lambda name, n: P1(name, [128, NBK, n])
    gmx = RB("gmx", 1); gm = RB("gm", 4); ge = RB("ge", 4); gpr = RB("gpr", 1); elm = RB("elm", 32); els = RB("els", 8)
    m1 = RB("m1", 1); mk1 = RB("mk1", 8); el2 = RB("el2", 8); m2 = RB("m2", 1); mk2 = RB("mk2", 8); dd = RB("dd", 1); w1 = RB("w1", 1)
    t_r = Tr()
    rv = lambda fn, rd=(), wrs=(), en="dve": k.op(en, fn, reads=[t_r, t_lga] + list(rd), writes=[t_r] + list(wrs))
    BC = lambda ap, n: ap.to_broadcast([128, NBK, n])
    rv(lambda e: e.tensor_reduce(out=gmx[:, :, 0], in_=lga[:, :, 0:4], axis=AX.X, op=ALU.max))
    rv(lambda e: e.tensor_tensor(out=gm[:], in0=lga[:, :, 0:4], in1=BC(gmx[:, :, 0:1], 4), op=ALU.is_equal))
    rv(lambda e: e.tensor_tensor(out=ge[:], in0=lga[:, :, 0:4], in1=BC(gmx[:, :, 0:1], 4), op=ALU.subtract))
    rv(lambda e: e.activation(out=ge[:], in_=ge[:], func=AF.Exp), en="act")
    rv(lambda e: e.tensor_reduce(out=gpr[:, :, 0], in_=ge[:], axis=AX.X, op=ALU.add))
    rv(lambda e: e.reciprocal(out=gpr[:], in_=gpr[:]))
    for g_ in range(4):
        rv(lambda e: e.tensor_tensor(out=elm[:, :, g_ * 8:(g_ + 1) * 8], in0=lga[:, :, 4 + g_ * 8:12 + g_ * 8], in1=BC(gm[:, :, g_:g_ + 1], 8),
                                     op=ALU.mult))
    rv(lambda e: e.tensor_tensor(out=els[:], in0=elm[:, :, 0:8], in1=elm[:, :, 8:16], op=ALU.add))
    rv(lambda e: e.tensor_tensor(out=els[:], in0=els[:], in1=elm[:, :, 16:24], op=ALU.add))
    rv(lambda e: e.tensor_tensor(out=els[:], in0=els[:], in1=elm[:, :, 24:32], op=ALU.add))
    rv(lambda e: e.tensor_reduce(out=m1[:, :, 0], in_=els[:], axis=AX.X, op=ALU.max))
    rv(lambda e: e.tensor_tensor(out=mk1[:], in0=els[:], in1=BC(m1[:, :, 0:1], 8), op=ALU.is_equal))
    rv(lambda e: e.scalar_tensor_tensor(out=el2[:], in0=mk1[:], scalar=-1e30, in1=els[:], op0=ALU.mult, op1=ALU.add))
    rv(lambda e: e.tensor_reduce(out=m2[:, :, 0], in_=el2[:], axis=AX.X, op=ALU.max))
    rv(lambda e: e.tensor_tensor(out=mk2[:], in0=el2[:], in1=BC(m2[:, :, 0:1], 8), op=ALU.is_equal))
    rv(lambda e: e.tensor_tensor(out=dd[:], in0=m2[:], in1=m1[:], op=ALU.subtract))
    rv(lambda e: e.activation(out=dd[:], in_=dd[:], func=AF.Exp), en="act")
    rv(lambda e: e.tensor_scalar(out=w1[:], in0=dd[:], scalar1=1.0, scalar2=None, op0=ALU.add))
    rv(lambda e: e.reciprocal(out=w1[:], in_=w1[:]))
    rv(lambda e: e.tensor_tensor(out=dd[:], in0=dd[:], in1=w1[:], op=ALU.mult))
    rv(lambda e: e.tensor_tensor(out=w12[:, :, 0:1], in0=w1[:], in1=gpr[:], op=ALU.mult), wrs=[t_w12])
    rv(lambda e: e.tensor_tensor(out=w12[:, :, 1:2], in0=dd[:], in1=gpr[:], op=ALU.mult), wrs=[t_w12])
    for kk_, mk in ((0, mk1), (1, mk2)):
        for g_ in range(4):
            rv(lambda e: e.tensor_tensor(out=OH[:, :, kk_, g_ * 8:(g_ + 1) * 8], in0=mk[:], in1=BC(gm[:, :, g_:g_ + 1], 8), op=ALU.mult),
               wrs=[t_OH])
    OHs = P1("OHs", [128, NBK, 32]); t_OHs = Tr()
    k.op("dve", lambda e: e.tensor_tensor(out=OHs[:], in0=OH[:, :, 0, :], in1=OH[:, :, 1, :], op=ALU.add), reads=[t_OH], writes=[t_OHs])
    pref = P1("pref", [128, NBK, 32]); t_pref = Tr()
    for j in range(NBK):
        ps, tps = k.ps()
        k.op("pe", lambda e: e.matmul(ps[:, 0:32], tris[:, :], OHs[:, j, :], start=True, stop=(j == 0)), reads=[t_OHs, tp], writes=[tps])
        for j2 in range(j):
            k.op("pe", lambda e: e.matmul(ps[:, 0:32], ones[:, :], OHs[:, j2, :], start=False, stop=(j2 == j - 1)),
                 reads=[t_OHs, tp], writes=[tps])
        k.op("act", lambda e: e.copy(out=pref[:, j, :], in_=ps[:, 0:32]), reads=[tps], writes=[t_pref])
    cst = P1("cst", [128, 8, 32]); t_c = Tr()
    ps, tps = k.ps()
    for j in range(NBK):
        k.op("pe", lambda e: e.matmul(ps[:, 0:32], ones[:, :], OHs[:, j, :], start=(j == 0), stop=(j == NBK - 1)),
             reads=[t_OHs, tp], writes=[tps])
    cd = lambda fn, rd=(): k.op("dve", fn, reads=[t_c] + list(rd), writes=[t_c])
    cd(lambda e: e.tensor_copy(out=cst[:, 0, :], in_=ps[:, 0:32]), rd=[tps])
    cd(lambda e: e.memset(cst[:, 1, :], 0.0))
    cd(lambda e: e.memset(cst[:, 6, :], 1.0))
    for m in range(MAXB):
        cd(lambda e: e.tensor_scalar(out=cst[:, 5, :], in0=cst[:, 0, :], scalar1=float(m * EB), scalar2=None, op0=ALU.is_gt))
        cd(lambda e: e.tensor_tensor(out=cst[:, 1, :], in0=cst[:, 1, :], in1=cst[:, 5, :], op=ALU.add))
    cd(lambda e: e.tensor_scalar(out=cst[:, 2, :], in0=cst[:, 1, :], scalar1=float(EB), scalar2=None, op0=ALU.mult))
    cd(lambda e: e.tensor_tensor_scan(out=cst[:, 3, :], data0=cst[:, 6, :], data1=cst[:, 2, :], initial=0.0, op0=ALU.mult, op1=ALU.add))
    cd(lambda e: e.tensor_tensor(out=cst[:, 4, :], in0=cst[:, 3, :], in1=cst[:, 2, :], op=ALU.subtract))
    k.op("dve", lambda e: e.tensor_tensor(out=pref[:], in0=pref[:], in1=cst[:, 4:5, :].to_broadcast([128, NBK, 32]), op=ALU.add),
         reads=[t_c], writes=[t_pref])
    dstf = P1("dstf", [128, NBK, 2]); t_d = Tr()
    tmpo = P1("tmpo", [128, NBK, 32]); t_to = Tr()
    for kk_ in range(2):
        k.op("dve", lambda e: e.tensor_tensor(out=tmpo[:], in0=OH[:, :, kk_, :], in1=pref[:], op=ALU.mult), reads=[t_OH, t_pref], writes=[t_to])
        k.op("dve", lambda e: e.tensor_reduce(out=dstf[:, :, kk_], in_=tmpo[:], axis=AX.X, op=ALU.add), reads=[t_to], writes=[t_d])
    k.op("dve", lambda e: e.tensor_copy(out=dsti[:], in_=dstf[:]), reads=[t_d], writes=[t_d])
    bexp = P1("bexp", [128, NBLK]); t_be = Tr()
    for bk in range(NBLK):
        cd(lambda e: e.tensor_scalar(out=cst[:, 5, :], in0=cst[:, 3, :], scalar1=float(bk * EB), scalar2=None, op0=ALU.is_le))
        k.op("dve", lambda e: e.reduce_sum(out=bexp[:, bk:bk + 1], in_=cst[:, 5, :], axis=AX.X), reads=[t_c], writes=[t_be])
    k.op("dve", lambda e: e.tensor_scalar(out=bexp[:], in0=bexp[:], scalar1=float(NEXP - 1), scalar2=None, op0=ALU.min), reads=[t_be], writes=[t_be])
    widf = P1("widf", [128, NBLK]); t_wi = Tr()
    k.op("dve", lambda e: e.tensor_scalar(out=widf[:], in0=bexp[:], scalar1=128.0, scalar2=float(l * NEXP * 128), op0=ALU.mult, op1=ALU.add),
         reads=[t_be], writes=[t_wi])
    k.op("dve", lambda e: e.tensor_tensor(out=widf[:], in0=widf[:], in1=iop[:, 0:1].to_broadcast([128, NBLK]), op=ALU.add),
         reads=[tp], writes=[t_wi])
    k.op("dve", lambda e: e.tensor_copy(out=widi[:], in_=widf[:]), reads=[t_wi], writes=[t_wi])
    hbs = [(k.sbuf(st1, "hb2", [128, D], BF16), Tr()) for _ in range(2)]
    for j in range(NBK):
        hb_, thb = hbs[j % 2]
        k.dma(hb_[:], S.HS[j * 128:(j + 1) * 128, :], reads=[S.t_HS], writes=[thb])
        for kk_ in range(2):
            k.idma(S.HSORT[:, :], hb_[:], out_off=dsti[:, j, kk_:kk_ + 1], bound=PMAX - 1, reads=[thb, t_d], writes=[S.t_HSORT])
    k.barrier()
    st1.close()
    if getattr(cfg, "moe_stop", 9) < 2:
        st.close()
        return
    st2 = contextlib.ExitStack()
    P2 = lambda name, shape, dt=F32: k.sbuf(st2, name, shape, dt)
    wgs = [(P2("wg", [128, 8, DE], BF16), P2("wu", [128, 8, DE], BF16), P2("wd", [128, 4, D], BF16), Tr()) for _ in range(2)]
    htk = [(P2("htk", [128, 4, D], BF16), Tr()) for _ in range(2)]
    hTs = [(P2("hT", [128, 8, EB], BF16), Tr()) for _ in range(2)]
    sgs = [(P2("sg", [128, 512]), Tr()) for _ in range(2)]
    aTs = [(P2("aT", [128, 4, 512], BF16), Tr()) for _ in range(2)]
    ybs = [(P2("yb", [128, 4, D], BF16), Tr()) for _ in range(2)]
    for bk in range(NBLK):
        wg, wu, wd, twe = wgs[bk % 2]
        bnd = DEPTH * NEXP * 128 - 1
        for (dst, nm, hc) in ((wg, "ewg", 4), (wu, "ewu", 4), (wd, "ewd", 2)):
            k.idma(dst[:, 0:hc, :].rearrange("p a b -> p (a b)"), S.inp[nm + "_a"][:, :], in_off=widi[:, bk:bk + 1], bound=bnd,
                   reads=[t_wi], writes=[twe])
            k.idma(dst[:, hc:2 * hc, :].rearrange("p a b -> p (a b)"), S.inp[nm + "_b"][:, :], in_off=widi[:, bk:bk + 1], bound=bnd,
                   reads=[t_wi], writes=[twe])
        hk, thk = htk[bk % 2]
        hT, thT = hTs[bk % 2]
        k.dma(hk[:], S.HSORT[bk * EB:(bk + 1) * EB, :].rearrange("(n p) d -> p n d", p=128), reads=[S.t_HSORT], writes=[thk])
        for fc in range(8):
            ps, tps = k.ps()
            psb = ps[:].bitcast(BF16)
            for n in range(4):
                k.op("pe", lambda e: e.transpose(out=psb[:, n * 128:(n + 1) * 128], in_=hk[:, n, fc * 128:(fc + 1) * 128],
                                                 identity=S.identb[:, :]), reads=[thk, S.t_const], writes=[tps])
            k.op("act" if fc % 2 else "dve", (lambda e: e.copy(out=hT[:, fc, :], in_=psb[:, 0:512])) if fc % 2 else
                 (lambda e: e.tensor_copy(out=hT[:, fc, :], in_=psb[:, 0:512])), reads=[tps], writes=[thT])
        aT, taT = aTs[bk % 2]
        for f in range(4):
            pg, tpg = k.ps()
            pu, tpu = k.ps()
            for kc in range(8):
                k.op("pe", lambda e: e.matmul(pg[:, 0:EB], wg[:, kc, f * 128:(f + 1) * 128], hT[:, kc, :], start=(kc == 0), stop=(kc == 7)),
                     reads=[twe, thT], writes=[tpg])
            for kc in range(8):
                k.op("pe", lambda e: e.matmul(pu[:, 0:EB], wu[:, kc, f * 128:(f + 1) * 128], hT[:, kc, :], start=(kc == 0), stop=(kc == 7)),
                     reads=[twe, thT], writes=[tpu])
            sg, tsg = sgs[f % 2]
            k.op("act", lambda e: e.activation(out=sg[:], in_=pg[:, 0:EB], func=AF.Silu), reads=[tpg], writes=[tsg])
            k.op("dve", lambda e: e.tensor_tensor(out=aT[:, f, :], in0=pu[:, 0:EB], in1=sg[:], op=ALU.mult), reads=[tpu, tsg], writes=[taT])
        yb, tyb = ybs[bk % 2]
        for jb in range(4):
            for h in range(2):
                py, tpy = k.ps()
                for f in range(4):
                    k.op("pe", lambda e: e.matmul(py[:, 0:512], aT[:, f, jb * 128:(jb + 1) * 128], wd[:, f, h * 512:(h + 1) * 512],
                                                  start=(f == 0), stop=(f == 3)), reads=[taT, twe], writes=[tpy])
                k.op("act" if h else "dve", (lambda e: e.copy(out=yb[:, jb, h * 512:(h + 1) * 512], in_=py[:, 0:512])) if h else
                     (lambda e: e.tensor_copy(out=yb[:, jb, h * 512:(h + 1) * 512], in_=py[:, 0:512])), reads=[tpy], writes=[tyb])
        k.dma(S.YB[bk * EB:(bk + 1) * EB, :].rearrange("(n p) d -> p n d", p=128), yb[:], reads=[tyb], writes=[S.t_YB])
    k.barrier()
    st2.close()
    if getattr(cfg, "moe_stop", 9) < 3:
        st.close()
        return
    NB3 = 4
    x1s = [(P("x1c", [128, D]), Tr()) for _ in range(NB3)]
    g1s = [(k.sbuf(st, "g1", [128, D], BF16), k.sbuf(st, "g2_", [128, D], BF16), Tr()) for _ in range(NB3)]
    zs = [(P("z2", [128, D]), Tr()) for _ in range(NB3)]
    sms = [(P("sm2", [128, 32]), Tr()) for _ in range(NB3)]
    for j, (b, t0) in enumerate(blocks):
        r = NB if t0 < C else b
        x1, tx1 = x1s[j % NB3]; ga, gb, tg = g1s[j % NB3]; z, tz = zs[j % NB3]; sm, tsm = sms[j % NB3]
        k.dma(x1[:], S.XR[b, t0:t0 + 128, :], reads=[S.t_XR[b]], writes=[tx1])
        k.idma(ga[:], S.YB[:, :], in_off=dsti[:, j, 0:1], bound=PMAX - 1, reads=[S.t_YB, t_d], writes=[tg])
        k.idma(gb[:], S.YB[:, :], in_off=dsti[:, j, 1:2], bound=PMAX - 1, reads=[S.t_YB, t_d], writes=[tg])
        k.op("dve", lambda e: e.tensor_scalar(out=z[:], in0=ga[:], scalar1=w12[:, j, 0:1], scalar2=None, op0=ALU.mult),
             reads=[tg, t_w12], writes=[tz])
        k.op("dve", lambda e: e.scalar_tensor_tensor(out=z[:], in0=gb[:], scalar=w12[:, j, 1:2], in1=z[:], op0=ALU.mult, op1=ALU.add),
             reads=[tg, t_w12], writes=[tz])
        k.op("pool", lambda e: e.tensor_tensor(out=z[:], in0=z[:], in1=S.gateb[:, 1, r, :], op=ALU.mult), reads=[S.t_gateb], writes=[tz])
        k.op("dve", lambda e: e.scalar_tensor_tensor(out=z[:], in0=x1[:], scalar=ALPHA, in1=z[:], op0=ALU.mult, op1=ALU.add),
             reads=[tx1], writes=[tz])
        ln_block(S, z, tz, lng, lnb, t_ln, sm, tsm)
        if last:
            k.dma(S.out[b, t0 - C:t0 - C + 128, :], z[:], reads=[tz], writes=[S.t_out])
        else:
            k.dma(S.XR[b, t0:t0 + 128, :], z[:], reads=[tz], writes=[S.t_XR[b]])
    k.barrier()
    st.close()
```
